# Optimizing a Trainium2 kernel written in Bass

```python
import jax, jax.numpy as jnp
from jax import lax
import numpy as np

D_MODEL = 1024
BATCH = 4
SEQ = 4096
DEPTH = 2

GRID_W = 64
PLE_DIM = 256
HEAD_DIM = 64
ATTN_HEADS = 8
ATTN_KV_HEADS = 2
GLA_HEADS = 4
MLSTM_HEADS = 4
ATTN_WIDTH = ATTN_HEADS * HEAD_DIM
KV_WIDTH = ATTN_KV_HEADS * HEAD_DIM
GLA_WIDTH = GLA_HEADS * HEAD_DIM
MLSTM_WIDTH = MLSTM_HEADS * HEAD_DIM
MIX_WIDTH = ATTN_WIDTH + GLA_WIDTH + MLSTM_WIDTH
GLA_RANK = 16
GLA_TAU = 16.0
CHUNK = 64
Q_BLOCK = 128
ROPE_THETA = 10000.0
ROPE_AXIS_DIM = HEAD_DIM // 2
CONV_W = 3
N_GROUPS = 4
EXPERTS_PER_GROUP = 4
N_EXPERTS = N_GROUPS * EXPERTS_PER_GROUP
TOP_K = 2
D_EXPERT = 512
EPS = 1e-6
NEG = -1e30
IN_SIZES = (ATTN_WIDTH, KV_WIDTH, KV_WIDTH,
            GLA_WIDTH, GLA_WIDTH, GLA_WIDTH, GLA_WIDTH, 2 * GLA_RANK,
            MLSTM_WIDTH, MLSTM_WIDTH, MLSTM_WIDTH, MLSTM_WIDTH, 4 * MLSTM_HEADS)
IN_WIDTH = sum(IN_SIZES)

kernel_name = "hybrid_gqa_gla_mlstm_hiermoe_encoder"

F32 = jnp.float32


def rms_norm(x, g):
    xf = x.astype(F32)
    y = xf * lax.rsqrt(jnp.mean(xf * xf, axis=-1, keepdims=True) + EPS)
    return (y * g.astype(F32)).astype(x.dtype)


def split_cols(z, sizes):
    out, off = [], 0
    for s in sizes:
        out.append(z[..., off:off + s])
        off += s
    return out


def axial_rope_tables(T):
    rows = T // GRID_W
    row = jnp.repeat(jnp.arange(rows, dtype=F32), GRID_W)
    col = jnp.tile(jnp.arange(GRID_W, dtype=F32), rows)
    inv = ROPE_THETA ** (-jnp.arange(0, ROPE_AXIS_DIM, 2, dtype=F32) / ROPE_AXIS_DIM)
    ang_r = row[:, None] * inv[None, :]
    ang_c = col[:, None] * inv[None, :]
    return (jnp.cos(ang_r), jnp.sin(ang_r), jnp.cos(ang_c), jnp.sin(ang_c))


def rotate(xa, cos, sin):
    half = ROPE_AXIS_DIM // 2
    x1, x2 = xa[..., :half], xa[..., half:]
    c = cos[:, None, :]
    s = sin[:, None, :]
    return jnp.concatenate([x1 * c - x2 * s, x2 * c + x1 * s], axis=-1)


def apply_axial_rope(x, tables):
    cr, sr, cc, sc = tables
    xf = x.astype(F32)
    out = jnp.concatenate([rotate(xf[..., :ROPE_AXIS_DIM], cr, sr),
                           rotate(xf[..., ROPE_AXIS_DIM:], cc, sc)], axis=-1)
    return out.astype(x.dtype)


def grouped_query_attention(q, k, v):
    B, T = q.shape[0], q.shape[1]
    nb = T // Q_BLOCK
    G = ATTN_HEADS // ATTN_KV_HEADS
    qb = q.reshape(B, nb, Q_BLOCK, ATTN_KV_HEADS, G, HEAD_DIM).transpose(1, 0, 3, 4, 2, 5)
    kt = k.transpose(0, 2, 1, 3)
    vt = v.transpose(0, 2, 1, 3)
    scale = HEAD_DIM ** -0.5

    def block(qblk):
        s = jnp.einsum('bkgqd,bksd->bkgqs', qblk, kt, preferred_element_type=F32) * scale
        pr = jax.nn.softmax(s, axis=-1).astype(vt.dtype)
        return jnp.einsum('bkgqs,bksd->bkgqd', pr, vt)

    o = lax.map(block, qb)
    return o.transpose(1, 0, 4, 2, 3, 5).reshape(B, T, ATTN_WIDTH)


def gla_causal(q, k, v, la):
    B, H, T, dk = q.shape
    n = T // CHUNK
    q, k, v, la = [a.reshape(B, H, n, CHUNK, a.shape[-1]) for a in (q, k, v, la)]
    b = jnp.cumsum(la, axis=-2)
    b_mid = b[..., CHUNK // 2 - 1:CHUNK // 2, :]
    b_last = b[..., -1:, :]
    mask = jnp.tril(jnp.ones((CHUNK, CHUNK), dtype=bool))
    a = jnp.einsum('bhncd,bhnsd->bhncs', q * jnp.exp(b - b_mid), k * jnp.exp(b_mid - b))
    a = jnp.where(mask, a, 0.0)
    o_intra = jnp.einsum('bhncs,bhnse->bhnce', a, v)
    kv = jnp.einsum('bhncd,bhnce->bhnde', k * jnp.exp(b_last - b), v)
    decay = jnp.exp(b_last[..., 0, :])

    def step(S, inp):
        dec, kvc = inp
        return dec[..., None] * S + kvc, S

    S0 = jnp.zeros((B, H, dk, v.shape[-1]), F32)
    _, S_prev = lax.scan(step, S0, (jnp.moveaxis(decay, 2, 0), jnp.moveaxis(kv, 2, 0)))
    S_prev = jnp.moveaxis(S_prev, 0, 2)
    o_inter = jnp.einsum('bhncd,bhnde->bhnce', q * jnp.exp(b), S_prev)
    return (o_intra + o_inter).reshape(B, H, T, -1)


def mlstm_causal(q, k, v, logi, logf):
    B, H, T, d = q.shape
    n = T // CHUNK
    q, k, v = [a.reshape(B, H, n, CHUNK, d) for a in (q, k, v)]
    logi = logi.reshape(B, H, n, CHUNK)
    b = jnp.cumsum(logf.reshape(B, H, n, CHUNK), axis=-1)
    b_last = b[..., -1]
    g = b_last[..., None] - b + logi

    def step(carry, inp):
        C_s, n_s, m_s = carry
        a_c, g_c, k_c, v_c = inp
        m_new = jnp.maximum(a_c + m_s, jnp.max(g_c, axis=-1))
        w_prev = jnp.exp(a_c + m_s - m_new)
        w_k = jnp.exp(g_c - m_new[..., None])
        C_new = w_prev[..., None, None] * C_s + jnp.einsum('bhc,bhcd,bhce->bhde', w_k, k_c, v_c)
        n_new = w_prev[..., None] * n_s + jnp.einsum('bhc,bhcd->bhd', w_k, k_c)
        return (C_new, n_new, m_new), (C_s, n_s, m_s)

    init = (jnp.zeros((B, H, d, d), F32), jnp.zeros((B, H, d), F32), jnp.full((B, H), NEG, F32))
    xs = (jnp.moveaxis(b_last, 2, 0), jnp.moveaxis(g, 2, 0),
          jnp.moveaxis(k, 2, 0), jnp.moveaxis(v, 2, 0))
    _, (Cp, Np, Mp) = lax.scan(step, init, xs)
    Cp = jnp.moveaxis(Cp, 0, 2)
    Np = jnp.moveaxis(Np, 0, 2)
    Mp = jnp.moveaxis(Mp, 0, 2)
    mask = jnp.tril(jnp.ones((CHUNK, CHUNK), dtype=bool))
    inter = b + Mp[..., None]
    logD = b[..., :, None] - b[..., None, :] + logi[..., None, :]
    logD = jnp.where(mask, logD, NEG)
    m = jnp.maximum(inter, jnp.max(logD, axis=-1))
    w_inter = jnp.exp(inter - m)
    s = jnp.einsum('bhncd,bhnsd->bhncs', q, k) * jnp.exp(logD - m[..., None])
    num = w_inter[..., None] * jnp.einsum('bhncd,bhnde->bhnce', q, Cp) \
        + jnp.einsum('bhncs,bhnse->bhnce', s, v)
    den = w_inter * jnp.einsum('bhncd,bhnd->bhnc', q, Np) + jnp.sum(s, axis=-1)
    h = num / jnp.maximum(jnp.abs(den), jnp.exp(-m))[..., None]
    return h.reshape(B, H, T, d)


def to_heads(a, n_heads):
    B, T = a.shape[0], a.shape[1]
    return a.astype(F32).reshape(B, T, n_heads, HEAD_DIM).transpose(0, 2, 1, 3)


def flip_t(a):
    return jnp.flip(a, axis=2)


def gla_branch(gq, gk, gv, gg, glr, w_dec, b_dec, norm_g):
    B, T = gq.shape[0], gq.shape[1]
    q = to_heads(gq, GLA_HEADS) * HEAD_DIM ** -0.5
    k = to_heads(gk, GLA_HEADS)
    v = to_heads(gv, GLA_HEADS)
    lr = glr.astype(F32)
    wd = w_dec.astype(F32)
    bd = b_dec.astype(F32)
    la_f = to_heads(jax.nn.log_sigmoid(lr[..., :GLA_RANK] @ wd[0] + bd[0]) / GLA_TAU, GLA_HEADS)
    la_b = to_heads(jax.nn.log_sigmoid(lr[..., GLA_RANK:] @ wd[1] + bd[1]) / GLA_TAU, GLA_HEADS)
    o = gla_causal(q, k, v, la_f) \
        + flip_t(gla_causal(flip_t(q), flip_t(k), flip_t(v), flip_t(la_b)))
    o = rms_norm(o.transpose(0, 2, 1, 3), norm_g).reshape(B, T, GLA_WIDTH)
    return (o * jax.nn.silu(gg.astype(F32))).astype(gq.dtype)


def centred_conv(x, w, b):
    T = x.shape[1]
    pad = CONV_W // 2
    xp = jnp.pad(x, ((0, 0), (pad, pad), (0, 0)))
    return sum(xp[:, j:j + T] * w[j] for j in range(CONV_W)) + b


def mlstm_branch(mq, mk, mv, mo, mg, conv_w, conv_b, b_in, b_fg, norm_g):
    B, T = mq.shape[0], mq.shape[1]
    qk = jax.nn.silu(centred_conv(jnp.concatenate([mq, mk], axis=-1), conv_w, conv_b))
    q = to_heads(qk[..., :MLSTM_WIDTH], MLSTM_HEADS)
    k = to_heads(qk[..., MLSTM_WIDTH:], MLSTM_HEADS) * HEAD_DIM ** -0.5
    v = to_heads(mv, MLSTM_HEADS)
    gates = mg.astype(F32).transpose(0, 2, 1)
    Hm = MLSTM_HEADS
    bi = b_in.astype(F32)
    bf = b_fg.astype(F32)
    logi_f = gates[:, 0:Hm] + bi[0][None, :, None]
    logf_f = jax.nn.log_sigmoid(gates[:, Hm:2 * Hm] + bf[0][None, :, None])
    logi_b = gates[:, 2 * Hm:3 * Hm] + bi[1][None, :, None]
    logf_b = jax.nn.log_sigmoid(gates[:, 3 * Hm:4 * Hm] + bf[1][None, :, None])
    h = mlstm_causal(q, k, v, logi_f, logf_f) \
        + flip_t(mlstm_causal(flip_t(q), flip_t(k), flip_t(v), flip_t(logi_b), flip_t(logf_b)))
    h = rms_norm(h.transpose(0, 2, 1, 3), norm_g).reshape(B, T, MLSTM_WIDTH)
    return (h * jax.nn.sigmoid(mo.astype(F32))).astype(mq.dtype)


def hier_moe(h, w_group, b_group, w_router, b_router, w_gate, w_up, w_down):
    B, T, D = h.shape
    t = h.reshape(-1, D)
    gl = (t @ w_group + b_group).astype(F32)
    gi = jnp.argmax(gl, axis=-1)
    g_prob = jnp.take_along_axis(jax.nn.softmax(gl, axis=-1), gi[:, None], axis=-1)
    el = (t @ w_router + b_router).astype(F32).reshape(-1, N_GROUPS, EXPERTS_PER_GROUP)
    el_sel = jnp.take_along_axis(el, gi[:, None, None], axis=1)[:, 0]
    top_v, top_i = lax.top_k(el_sel, TOP_K)
    top_w = jax.nn.softmax(top_v, axis=-1) * g_prob
    eid = gi[:, None] * EXPERTS_PER_GROUP + top_i
    comb = jnp.sum(jax.nn.one_hot(eid, N_EXPERTS, dtype=F32) * top_w[..., None], axis=1)
    y = jnp.zeros((t.shape[0], D), F32)
    for e in range(N_EXPERTS):
        a = jax.nn.silu(t @ w_gate[e]) * (t @ w_up[e])
        y = y + comb[:, e:e + 1] * (a @ w_down[e]).astype(F32)
    return y.astype(h.dtype).reshape(B, T, D)


def setup_inputs(seed: int = 0) -> dict:
    key = jax.random.key(seed)
    ks = jax.random.split(key, 32)
    L = DEPTH

    def nrm(k, shape, scale):
        return jax.random.normal(k, shape, F32) * scale

    def gain(k, shape):
        return 1.0 + 0.02 * jax.random.normal(k, shape, F32)

    return {
        "x": nrm(ks[0], (BATCH, SEQ, D_MODEL), 1.0),
        "p": nrm(ks[1], (DEPTH, BATCH, SEQ, PLE_DIM), 1.0),
        "norm_mix_g": gain(ks[2], (L, D_MODEL)),
        "w_in": nrm(ks[3], (L, D_MODEL, IN_WIDTH), D_MODEL ** -0.5),
        "attn_q_norm_g": gain(ks[4], (L, HEAD_DIM)),
        "attn_k_norm_g": gain(ks[5], (L, HEAD_DIM)),
        "gla_w_decay": nrm(ks[6], (L, 2, GLA_RANK, GLA_WIDTH), GLA_RANK ** -0.5),
        "gla_b_decay": nrm(ks[7], (L, 2, GLA_WIDTH), 0.1),
        "gla_out_norm_g": gain(ks[8], (L, HEAD_DIM)),
        "mlstm_conv_w": nrm(ks[9], (L, CONV_W, 2 * MLSTM_WIDTH), CONV_W ** -0.5),
        "mlstm_conv_b": nrm(ks[10], (L, 2 * MLSTM_WIDTH), 0.02),
        "mlstm_b_input": nrm(ks[11], (L, 2, MLSTM_HEADS), 0.1),
        "mlstm_b_forget": jnp.linspace(3.0, 6.0, MLSTM_HEADS, dtype=F32)
                          + nrm(ks[12], (L, 2, MLSTM_HEADS), 0.1),
        "mlstm_out_norm_g": gain(ks[13], (L, HEAD_DIM)),
        "w_out": nrm(ks[14], (L, MIX_WIDTH, D_MODEL), MIX_WIDTH ** -0.5),
        "norm_ffn_g": gain(ks[15], (L, D_MODEL)),
        "w_group": nrm(ks[16], (L, D_MODEL, N_GROUPS), D_MODEL ** -0.5),
        "b_group": nrm(ks[17], (L, N_GROUPS), 0.01),
        "w_router": nrm(ks[18], (L, D_MODEL, N_EXPERTS), D_MODEL ** -0.5),
        "b_router": nrm(ks[19], (L, N_EXPERTS), 0.01),
        "w_expert_gate": nrm(ks[20], (L, N_EXPERTS, D_MODEL, D_EXPERT), D_MODEL ** -0.5),
        "w_expert_up": nrm(ks[21], (L, N_EXPERTS, D_MODEL, D_EXPERT), D_MODEL ** -0.5),
        "w_expert_down": nrm(ks[22], (L, N_EXPERTS, D_EXPERT, D_MODEL), D_EXPERT ** -0.5),
        "norm_ple_g": gain(ks[23], (L, D_MODEL)),
        "w_ple_gate": nrm(ks[24], (L, D_MODEL, D_MODEL), D_MODEL ** -0.5),
        "w_ple_proj": nrm(ks[25], (L, PLE_DIM, D_MODEL), PLE_DIM ** -0.5),
        "final_norm_g": gain(ks[26], (D_MODEL,)),
    }


def reference(x, p, norm_mix_g, w_in, attn_q_norm_g, attn_k_norm_g, gla_w_decay, gla_b_decay,
              gla_out_norm_g, mlstm_conv_w, mlstm_conv_b, mlstm_b_input, mlstm_b_forget,
              mlstm_out_norm_g, w_out, norm_ffn_g, w_group, b_group, w_router, b_router,
              w_expert_gate, w_expert_up, w_expert_down, norm_ple_g, w_ple_gate, w_ple_proj,
              final_norm_g):
    B, T, _ = x.shape
    rope = axial_rope_tables(T)
    for i in range(DEPTH):
        h = rms_norm(x, norm_mix_g[i])
        z = h @ w_in[i]
        (aq, ak, av, gq, gk, gv, gg, glr, mq, mk, mv, mo, mg) = split_cols(z, IN_SIZES)
        aq = rms_norm(aq.reshape(B, T, ATTN_HEADS, HEAD_DIM), attn_q_norm_g[i])
        ak = rms_norm(ak.reshape(B, T, ATTN_KV_HEADS, HEAD_DIM), attn_k_norm_g[i])
        aq = apply_axial_rope(aq, rope)
        ak = apply_axial_rope(ak, rope)
        av = av.reshape(B, T, ATTN_KV_HEADS, HEAD_DIM)
        attn_out = grouped_query_attention(aq, ak, av)
        gla_out = gla_branch(gq, gk, gv, gg, glr, gla_w_decay[i], gla_b_decay[i],
                             gla_out_norm_g[i])
        ml_out = mlstm_branch(mq, mk, mv, mo, mg, mlstm_conv_w[i], mlstm_conv_b[i],
                              mlstm_b_input[i], mlstm_b_forget[i],
                              mlstm_out_norm_g[i])
        mix = jnp.concatenate([attn_out.astype(x.dtype), gla_out, ml_out], axis=-1)
        x = x + mix @ w_out[i]
        x = x + hier_moe(rms_norm(x, norm_ffn_g[i]), w_group[i], b_group[i], w_router[i],
                         b_router[i], w_expert_gate[i], w_expert_up[i], w_expert_down[i])
        gate = jax.nn.sigmoid(rms_norm(x, norm_ple_g[i]) @ w_ple_gate[i])
        x = x + gate * (p[i] @ w_ple_proj[i])
    return rms_norm(x, final_norm_g)
```

```python
import numpy as np
import ml_dtypes
from contextlib import ExitStack
import concourse.bass as bass
import concourse.mybir as mybir
from concourse.bass_utils import run_bass_kernel_spmd

F32 = mybir.dt.float32
BF16 = mybir.dt.bfloat16
AF = mybir.ActivationFunctionType
ALU = mybir.AluOpType
AX = mybir.AxisListType

EPS = 1e-6
EPOCH = 30000
_DBG = {}


class Buf:
    __slots__ = ("w", "rs", "name")

    def __init__(self, name=""):
        self.w = None
        self.rs = {}
        self.name = name


class _Q:
    def __init__(self, name):
        self.name = name
        self.ops = []
        self.n = 0
        self.known = {}
        self.shared = False
        self.maxep = {}


class Prog:
    ENG = ["pe", "act", "dve", "pool", "sp"]

    def __init__(self, nc, stack, n_dma_sems=24):
        self.nc = nc
        self.stack = stack
        self.q = {e: _Q(e) for e in self.ENG}
        self.esems = {e: [] for e in self.ENG}
        self.dsems = [stack.enter_context(nc.semaphore(f"dma{i}")) for i in range(n_dma_sems)]
        self.dcnt = [0] * n_dma_sems
        self.drr = 0
        self.snap = {}
        self.out_toks = []
        self.count = 0
        self.stop_at = None
        self.csems = []

    def _esem(self, e, epoch):
        while len(self.esems[e]) <= epoch:
            k = len(self.esems[e])
            self.esems[e].append(self.stack.enter_context(self.nc.semaphore(f"s_{e}{k}")))
        return self.esems[e][epoch]

    def _sem_of(self, key):
        if key[0] == "d":
            return self.dsems[key[1]] if key[1] < 1000 else self.csems[key[1] - 1000]
        return self._esem(key[0], key[1])

    def _is_known(self, q, key, val):
        if q.known.get(key, 0) >= val:
            return True
        if key[0] != "d" and q.maxep.get(key[0], -1) > key[1]:
            return True
        return False

    def _learn1(self, q, key, val):
        if q.known.get(key, 0) < val:
            if q.shared:
                q.known = dict(q.known)
                q.shared = False
            q.known[key] = val
        if key[0] != "d" and q.maxep.get(key[0], -1) < key[1]:
            q.maxep[key[0]] = key[1]

    def _wait(self, q, tok):
        key, val = tok
        if self._is_known(q, key, val):
            return
        sem = self._sem_of(key)
        q.ops.append(lambda eng, sem=sem, val=val: eng.wait_ge(sem, val))
        self._learn1(q, key, val)
        sn = self.snap.get(tok)
        if sn:
            for k, v in sn.items():
                self._learn1(q, k, v)

    def _deps(self, q, e, reads, writes):
        deps = {}
        for b in reads:
            if b.w is not None:
                deps[b.w] = 1
        for b in writes:
            if b.w is not None:
                deps[b.w] = 1
            for k, v in b.rs.items():
                deps[(k, v)] = 1
        for tok in deps:
            if e == "pe" and tok[0][0] == "pe":
                continue
            self._wait(q, tok)

    def _mark(self, tok, q, reads, writes):
        self.snap[tok] = q.known
        q.shared = True
        key, val = tok
        for b in reads:
            if b.rs.get(key, 0) < val:
                b.rs[key] = val
        for b in writes:
            b.w = tok
            b.rs = {}

    def op(self, e, fn, reads=(), writes=()):
        self.count += 1
        if self.stop_at is not None and self.count > self.stop_at:
            return None
        q = self.q[e]
        self._deps(q, e, reads, writes)
        epoch, cnt = divmod(q.n, EPOCH)
        cnt += 1
        q.n += 1
        sem = self._esem(e, epoch)
        q.ops.append(lambda eng, sem=sem, fn=fn: fn(eng).then_inc(sem, 1))
        tok = ((e, epoch), cnt)
        self._mark(tok, q, reads, writes)
        return tok

    def dma(self, e, out, in_, reads=(), writes=(), is_output=False, slow=False):
        self.count += 1
        if self.stop_at is not None and self.count > self.stop_at:
            return None
        q = self.q[e]
        i = self.drr
        self.drr = (self.drr + 1) % len(self.dsems)
        if self.dcnt[i] > 0:
            self._wait(q, (("d", i), self.dcnt[i]))
        self._deps(q, e, reads, writes)
        self.dcnt[i] += 16
        sem = self.dsems[i]
        kw = dict(allow_slow_non_contiguous=True) if slow else {}
        q.ops.append(lambda eng, sem=sem, out=out, in_=in_, kw=kw: eng.dma_start(out=out, in_=in_, **kw).then_inc(sem, 16))
        tok = (("d", i), self.dcnt[i])
        self._mark(tok, q, reads, writes)
        if is_output:
            self.out_toks.append(tok)
        return tok

    def collective(self, kind, in_ap, out_ap, groups, reads=(), writes=()):
        if _DBG.get("no_cc"):
            return None
        q = self.q["pool"]
        self._deps(q, "pool", reads, writes)
        k = len(self.csems)
        sem = self.stack.enter_context(self.nc.semaphore(f"cc{k}"))
        self.csems.append(sem)
        q.ops.append(lambda eng, sem=sem: eng.collective_compute(kind, ALU.bypass, replica_groups=groups,
                                                                 ins=[in_ap.opt()], outs=[out_ap.opt()]).then_inc(sem, 1))
        tok = (("d", 1000 + k), 1)
        self._mark(tok, q, reads, writes)
        return tok

    def barrier(self, bufs=()):
        toks = []
        for e in self.ENG:
            q = self.q[e]
            if q.n > 0:
                epoch, cnt = divmod(q.n - 1, EPOCH)
                toks.append(((e, epoch), cnt + 1))
        for i, c in enumerate(self.dcnt):
            if c > 0:
                toks.append((("d", i), c))
        for k in range(len(self.csems)):
            toks.append((("d", 1000 + k), 1))
        for e in self.ENG:
            for tok in toks:
                if tok[0][0] == e:
                    continue
                self._wait(self.q[e], tok)

    def final_wait(self):
        q = self.q["sp"]
        for tok in self.out_toks:
            self._wait(q, tok)

    def flush(self):
        qs = {e: list(self.q[e].ops) for e in self.ENG}
        for e in self.ENG:
            self.q[e].ops = []
        if not any(qs.values()):
            return
        with self.nc.Block() as block:
            @block.tensor
            def _(eng):
                for f in qs["pe"]:
                    f(eng)

            @block.scalar
            def _(eng):
                for f in qs["act"]:
                    f(eng)

            @block.vector
            def _(eng):
                for f in qs["dve"]:
                    f(eng)

            @block.gpsimd
            def _(eng):
                for f in qs["pool"]:
                    f(eng)

            @block.sync
            def _(eng):
                for f in qs["sp"]:
                    f(eng)


class Ctx:
    def __init__(self):
        self.nc = bass.Bass("TRN2", target_bir_lowering=False)
        self.stack = ExitStack()
        self.P = Prog(self.nc, self.stack)
        self._n = 0
        self.cur = self.stack

    def phase(self):
        C = self

        class _Ph:
            def __enter__(s2):
                s2.prev = C.cur
                s2.st = ExitStack()
                C.cur = s2.st
                return s2.st

            def __exit__(s2, *a):
                if a[0] is None:
                    C.P.barrier()
                    C.P.flush()
                C.cur = s2.prev
                s2.st.close()
                return False
        return _Ph()

    def sb(self, shape, dt, name=None, stack=None):
        self._n += 1
        t = (stack or self.cur).enter_context(self.nc.sbuf_tensor(f"{name or 't'}_{self._n}", list(shape), dt))
        return t

    def ps(self, shape, dt=F32, name=None):
        self._n += 1
        return self.cur.enter_context(self.nc.psum_tensor(f"{name or 'p'}_{self._n}", list(shape), dt))

    def din(self, name, shape, dt):
        if not hasattr(self, "_dins"):
            self._dins = {}
        if name not in self._dins:
            self._dins[name] = self.nc.dram_tensor(name, list(shape), dt, kind="ExternalInput").ap()
        return self._dins[name]

    def dout(self, name, shape, dt):
        return self.nc.dram_tensor(name, list(shape), dt, kind="ExternalOutput").ap()


def bc(ap, axis, shape):
    return ap.unsqueeze(axis).to_broadcast(list(shape))


def load_ident(C, dt=F32):
    P = C.P
    ident = C.sb([128, 128], dt, "ident")
    b = Buf("ident")
    P.op("pool", lambda g: g.memset(ident[:], 1.0), writes=[b])
    P.op("pool", lambda g: g.affine_select(out=ident[:], in_=ident[:], pattern=[[-1, 128]],
                                           compare_op=ALU.is_equal, fill=0.0, base=0, channel_multiplier=1),
         reads=[b], writes=[b])
    return ident, b


def rms_to_hT(C, x_sb, xb, NT, g32col, gb, ident, ib, hT, hTb, ps2, ps2b, fp32_cb=None, tag=""):
    P, nc = C.P, C.nc
    ss = C.sb([128, NT], F32, "ss" + tag)
    ssb = Buf("ss")
    r = C.sb([128, NT], F32, "r" + tag)
    rb = Buf("r")
    junk = C.sb([128, 1024], BF16, "junk" + tag)
    junkb = Buf("junk")
    P.op("pool", lambda g: g.memset(ss[:], 0.0), writes=[ssb])
    for i in range(NT):
        P.op("act", lambda a, i=i: a.activation(out=junk[:], in_=x_sb[:, i, :], func=AF.Square,
                                                 accum_out=ss[:, i:i + 1]),
             reads=[xb[i]], writes=[junkb, ssb])
    P.op("act", lambda a: a.activation(out=r[:], in_=ss[:], func=AF.Sqrt, scale=1.0 / 1024.0, bias=EPS),
         reads=[ssb], writes=[rb])
    P.op("dve", lambda v: v.reciprocal(out=r[:], in_=r[:]), reads=[rb], writes=[rb])
    xs = [C.sb([128, 1024], F32, f"xs{tag}{j}") for j in range(2)]
    xsb = [Buf("xs") for _ in range(2)]
    hTf = [C.sb([128, 8, 128], F32, f"hTf{tag}{j}") for j in range(2)]
    hTfb = [Buf("hTf") for _ in range(2)]
    for i in range(NT):
        j = i % 2
        P.op("act", lambda a, i=i, j=j: a.activation(out=xs[j][:], in_=x_sb[:, i, :], func=AF.Copy,
                                                      scale=r[:, i:i + 1]),
             reads=[xb[i], rb], writes=[xsb[j]])
        for kc in range(8):
            P.op("pe", lambda t, j=j, kc=kc: t.transpose(out=ps2[j][:, kc * 128:(kc + 1) * 128],
                                                         in_=xs[j][:, kc * 128:(kc + 1) * 128],
                                                         identity=ident[:]),
                 reads=[xsb[j], ib], writes=[ps2b[j]])
        if fp32_cb is not None:
            P.op("dve", lambda v, j=j: v.tensor_tensor(out=hTf[j][:], in0=ps2[j][:].rearrange("p (k t) -> p k t", k=8),
                                                        in1=bc(g32col[:], 2, [128, 8, 128]), op=ALU.mult),
                 reads=[ps2b[j], gb], writes=[hTfb[j]])
            P.op("pool", lambda g, i=i, j=j: g.tensor_copy(out=hT[:, :, i * 128:(i + 1) * 128], in_=hTf[j][:]),
                 reads=[hTfb[j]], writes=[hTb[i]])
            fp32_cb(i, hTf[j], hTfb[j])
        else:
            P.op("dve", lambda v, i=i, j=j: v.tensor_tensor(out=hT[:, :, i * 128:(i + 1) * 128],
                                                             in0=ps2[j][:].rearrange("p (k t) -> p k t", k=8),
                                                             in1=bc(g32col[:], 2, [128, 8, 128]), op=ALU.mult),
                 reads=[ps2b[j], gb], writes=[hTb[i]])


def load_gcol32(C, g_d, tag):
    P = C.P
    graw = C.sb([128, 8], F32, "graw" + tag)
    g32 = C.sb([128, 8], F32, "g32" + tag)
    b0, b1 = Buf(), Buf()
    P.dma("sp", graw[:], g_d.rearrange("(k p) -> p k", p=128), writes=[b0], slow=True)
    return graw, b0


def build_k2(T, last, C=None, sfx="", fz=None):
    standalone = C is None
    fz = fz or {}
    if C is None:
        C = Ctx()
    nc, P = C.nc, C.P
    NT = T // 128
    TB = min(512, T)
    NTB = T // TB
    SUB = TB // 128

    if "x_src" in fz:
        x_d, x_srcb = fz["x_src"]
        x_reads = [x_srcb]
    else:
        x_d = C.din("xh" + sfx, [T, 1024], F32)
        x_reads = []
    mixT_d = None if "mixg" in fz else C.din("mixT" + sfx, [1024, T], BF16)
    p_d = C.din("p" + sfx, [T, 256], F32)
    wout_d = C.din("w_out" + sfx, [1024, 1024], F32)
    gffn_d = C.din("norm_ffn_g" + sfx, [1024], F32)
    wr_d = C.din("w_gr" + sfx, [1024, 20], F32)
    br_d = C.din("b_gr" + sfx, [20], F32)
    wg_d = C.din("w_gate" + sfx, [16, 1024, 512], F32)
    wu_d = C.din("w_up" + sfx, [16, 1024, 512], F32)
    wd_d = C.din("w_down" + sfx, [16, 512, 1024], F32)
    gple_d = C.din("norm_ple_g" + sfx, [1024], F32)
    wpg_d = C.din("w_ple_gate" + sfx, [1024, 1024], F32)
    wpp_d = C.din("w_ple_proj" + sfx, [256, 1024], F32)
    gfin_d = C.din("final_norm_g" + sfx, [1024], F32) if last else None
    if "x_dst" in fz:
        out_d, out_dstb = fz["x_dst"]
        out_writes, out_is_output = [out_dstb], False
    else:
        out_d = C.dout("out", [T, 1024], F32)
        out_writes, out_is_output = [], True
    gnext_d = C.din("norm_mix_g_next" + sfx, [1024], F32) if "h_dst" in fz else None

    x_sb = C.sb([128, NT, 1024], F32, "x")
    xb = [Buf(f"x{i}") for i in range(NT)]
    ident, ib = load_ident(C)
    psA = [C.ps([128, 1024], F32, f"psA{j}") for j in range(2)]
    psAb = [Buf() for _ in range(2)]
    psB = [C.ps([128, 512], F32, f"psB{j}") for j in range(4)]
    psBb = [Buf() for _ in range(4)]

    x_v = x_d.rearrange("(n p) d -> p n d", p=128)
    for i0 in range(0, NT, 4):
        n = min(4, NT - i0)
        P.dma("sp", x_sb[:, i0:i0 + n, :], x_v[:, i0:i0 + n, :], reads=x_reads, writes=xb[i0:i0 + n])

    with C.phase() as st:
        mixT = C.sb([128, 8, T], BF16, "mixT", st)
        mb = Buf()
        wout = C.sb([128, 8, 1024], BF16, "wout", st)
        wb = Buf()
        if "mixg" in fz:
            mixg_rows = fz["mixg"]
            sel, selB = fz["sel"]
            cand = [[C.sb([128, T], BF16, f"cand{h}{s_}", st) for s_ in range(2)] for h in range(2)]
            candb = [[Buf() for _ in range(2)] for _ in range(2)]
            for kc in range(8):
                s_ = kc % 2
                for h in range(2):
                    P.dma("sp", cand[h][s_][:], mixg_rows[kc][0][:, h * T:(h + 1) * T], reads=[mixg_rows[kc][1]],
                          writes=[candb[h][s_]])
                P.op("dve", lambda v, kc=kc, s_=s_: v.tensor_scalar(out=mixT[:, kc, :], in0=cand[0][s_][:],
                                                                     scalar1=sel[:, 0:1], scalar2=None, op0=ALU.mult),
                     reads=[candb[0][s_], selB], writes=[mb])
                P.op("dve", lambda v, kc=kc, s_=s_: v.scalar_tensor_tensor(out=mixT[:, kc, :], in0=cand[1][s_][:],
                                                                            scalar=sel[:, 1:2], in1=mixT[:, kc, :],
                                                                            op0=ALU.mult, op1=ALU.add),
                     reads=[candb[1][s_], selB, mb], writes=[mb])
        else:
            P.dma("sp", mixT[:], mixT_d.rearrange("(k p) t -> p k t", p=128), writes=[mb])
        P.dma("pool", wout[:], wout_d.rearrange("(k p) n -> p k n", p=128), writes=[wb])
        for i in range(NT):
            for nh in range(2):
                j = (i * 2 + nh) % 4
                for kc in range(8):
                    P.op("pe", lambda t, i=i, nh=nh, kc=kc, j=j: t.matmul(
                        out=psB[j][:], lhsT=mixT[:, kc, i * 128:(i + 1) * 128],
                        rhs=wout[:, kc, nh * 512:(nh + 1) * 512], start=(kc == 0), stop=(kc == 7)),
                        reads=[mb, wb], writes=[psBb[j]])
                P.op("dve", lambda v, i=i, nh=nh, j=j: v.tensor_tensor(
                    out=x_sb[:, i, nh * 512:(nh + 1) * 512], in0=x_sb[:, i, nh * 512:(nh + 1) * 512],
                    in1=psB[j][:], op=ALU.add), reads=[psBb[j], xb[i]], writes=[xb[i]])

    with C.phase() as st:
        hT = C.sb([128, 8, T], BF16, "hT", st)
        hTb = [Buf() for _ in range(NT)]
        g32, g32b = load_gcol32(C, gffn_d, "ffn")
        wr = C.sb([128, 8, 20], F32, "wr", st)
        wrb = Buf()
        P.dma("sp", wr[:], wr_d.rearrange("(k p) n -> p k n", p=128), writes=[wrb])
        brb_t = C.sb([128, 20], F32, "brb", st)
        brb = Buf()
        P.dma("sp", brb_t[:], br_d.partition_broadcast(128), writes=[brb])
        L = C.sb([128, NT, 20], F32, "logits", st)
        Lb = Buf()

        def router_cb(i, hTf, hTfb):
            j = 2 + (i % 2)
            for kc in range(8):
                P.op("pe", lambda t, kc=kc, j=j: t.matmul(out=psB[j][:, 0:20], lhsT=hTf[:, kc, :],
                                                          rhs=wr[:, kc, :], start=(kc == 0), stop=(kc == 7)),
                     reads=[hTfb, wrb], writes=[psBb[j]])
            P.op("dve", lambda v, i=i, j=j: v.tensor_tensor(out=L[:, i, :], in0=psB[j][:, 0:20], in1=brb_t[:],
                                                             op=ALU.add), reads=[psBb[j], brb], writes=[Lb])

        rms_to_hT(C, x_sb, xb, NT, g32, g32b, ident, ib, hT, hTb, psA, psAb, fp32_cb=router_cb, tag="f")

        def S(shape, nm):
            return C.sb(shape, F32, nm, st), Buf(nm)
        gmax, gmaxb = S([128, NT], "gmax")
        gm, gmb = S([128, NT, 4], "gm")
        ge, geb = S([128, NT, 4], "ge")
        gsum, gsumb = S([128, NT], "gsum")
        gprob, gprobb = S([128, NT], "gprob")
        tmp4, tmp4b = S([128, NT, 4, 4], "tmp4")
        els, elsb = S([128, NT, 4], "els")
        m1, m1b = S([128, NT], "m1")
        mk1, mk1b = S([128, NT, 4], "mk1")
        el2, el2b = S([128, NT, 4], "el2")
        m2, m2b = S([128, NT], "m2")
        mk2, mk2b = S([128, NT, 4], "mk2")
        dd, ddb = S([128, NT], "dd")
        e2, e2b = S([128, NT], "e2")
        w1, w1b = S([128, NT], "w1")
        w2, w2b = S([128, NT], "w2")
        ws, wsb = S([128, NT, 4], "ws")
        ws2, ws2b = S([128, NT, 4], "ws2")
        comb, combb = S([128, NT, 4, 4], "comb")
        gl = L[:, :, 0:4]
        el = L[:, :, 4:20].rearrange("p n (g e) -> p n g e", g=4)
        V = lambda fn, r, w: P.op("dve", fn, reads=r, writes=w)
        V(lambda v: v.tensor_reduce(out=gmax[:], in_=gl, axis=AX.X, op=ALU.max), [Lb], [gmaxb])
        V(lambda v: v.tensor_tensor(out=gm[:], in0=gl, in1=bc(gmax[:], 2, [128, NT, 4]), op=ALU.is_equal),
          [Lb, gmaxb], [gmb])
        V(lambda v: v.tensor_tensor(out=ge[:], in0=gl, in1=bc(gmax[:], 2, [128, NT, 4]), op=ALU.subtract),
          [Lb, gmaxb], [geb])
        P.op("act", lambda a: a.activation(out=ge[:], in_=ge[:], func=AF.Exp), reads=[geb], writes=[geb])
        V(lambda v: v.tensor_reduce(out=gsum[:], in_=ge[:], axis=AX.X, op=ALU.add), [geb], [gsumb])
        V(lambda v: v.reciprocal(out=gprob[:], in_=gsum[:]), [gsumb], [gprobb])
        V(lambda v: v.tensor_tensor(out=tmp4[:], in0=el, in1=bc(gm[:], 3, [128, NT, 4, 4]), op=ALU.mult),
          [Lb, gmb], [tmp4b])
        V(lambda v: v.tensor_reduce(out=els[:], in_=tmp4[:].rearrange("p n g e -> p n e g"), axis=AX.X, op=ALU.add),
          [tmp4b], [elsb])
        V(lambda v: v.tensor_reduce(out=m1[:], in_=els[:], axis=AX.X, op=ALU.max), [elsb], [m1b])
        V(lambda v: v.tensor_tensor(out=mk1[:], in0=els[:], in1=bc(m1[:], 2, [128, NT, 4]), op=ALU.is_equal),
          [elsb, m1b], [mk1b])
        V(lambda v: v.scalar_tensor_tensor(out=el2[:].rearrange("p n e -> p (n e)"),
                                           in0=mk1[:].rearrange("p n e -> p (n e)"), scalar=-1e30,
                                           in1=els[:].rearrange("p n e -> p (n e)"), op0=ALU.mult, op1=ALU.add),
          [mk1b, elsb], [el2b])
        V(lambda v: v.tensor_reduce(out=m2[:], in_=el2[:], axis=AX.X, op=ALU.max), [el2b], [m2b])
        V(lambda v: v.tensor_tensor(out=mk2[:], in0=el2[:], in1=bc(m2[:], 2, [128, NT, 4]), op=ALU.is_equal),
          [el2b, m2b], [mk2b])
        V(lambda v: v.tensor_tensor(out=dd[:], in0=m2[:], in1=m1[:], op=ALU.subtract), [m1b, m2b], [ddb])
        P.op("act", lambda a: a.activation(out=e2[:], in_=dd[:], func=AF.Exp), reads=[ddb], writes=[e2b])
        V(lambda v: v.tensor_scalar(out=dd[:], in0=e2[:], scalar1=1.0, scalar2=None, op0=ALU.add), [e2b], [ddb])
        V(lambda v: v.reciprocal(out=w1[:], in_=dd[:]), [ddb], [w1b])
        V(lambda v: v.tensor_tensor(out=w1[:], in0=w1[:], in1=gprob[:], op=ALU.mult), [w1b, gprobb], [w1b])
        V(lambda v: v.tensor_tensor(out=w2[:], in0=w1[:], in1=e2[:], op=ALU.mult), [w1b, e2b], [w2b])
        V(lambda v: v.tensor_tensor(out=ws[:], in0=mk1[:], in1=bc(w1[:], 2, [128, NT, 4]), op=ALU.mult),
          [mk1b, w1b], [wsb])
        V(lambda v: v.tensor_tensor(out=ws2[:], in0=mk2[:], in1=bc(w2[:], 2, [128, NT, 4]), op=ALU.mult),
          [mk2b, w2b], [ws2b])
        V(lambda v: v.tensor_tensor(out=ws[:], in0=ws[:], in1=ws2[:], op=ALU.add), [wsb, ws2b], [wsb])
        V(lambda v: v.tensor_tensor(out=comb[:], in0=bc(gm[:], 3, [128, NT, 4, 4]),
                                    in1=bc(ws[:], 2, [128, NT, 4, 4]), op=ALU.mult), [gmb, wsb], [combb])
        combf = comb[:].rearrange("p n g e -> p n (g e)")

        NS = 2
        wgs = [C.sb([128, 8, 512], BF16, f"wg{s}", st) for s in range(NS)]
        wus = [C.sb([128, 8, 512], BF16, f"wu{s}", st) for s in range(NS)]
        wds = [C.sb([128, 4, 1024], BF16, f"wd{s}", st) for s in range(NS)]
        wgb = [Buf() for _ in range(NS)]
        wub = [Buf() for _ in range(NS)]
        wdb = [Buf() for _ in range(NS)]
        aT = [C.sb([128, 4, TB], BF16, f"aT{s}", st) for s in range(2)]
        aTb = [Buf() for _ in range(2)]
        sg = [C.sb([128, TB], BF16, f"sg{s}", st) for s in range(2)]
        sgb = [Buf() for _ in range(2)]
        gu = [(psA[0], 0), (psA[0], 512), (psA[1], 0), (psA[1], 512)]
        gub = [Buf() for _ in range(4)]

        def load_expert(e):
            s = e % NS
            P.dma("pool", wgs[s][:], wg_d[e].rearrange("(k p) n -> p k n", p=128), writes=[wgb[s]])
            P.dma("pool", wus[s][:], wu_d[e].rearrange("(k p) n -> p k n", p=128), writes=[wub[s]])
            P.dma("pool", wds[s][:], wd_d[e].rearrange("(k p) n -> p k n", p=128), writes=[wdb[s]])

        load_expert(0)
        dcnt_ = [0]

        def moe_step(e, tb, a, s):
            if True:
                if True:
                    pass
                tsl = slice(tb * TB, (tb + 1) * TB)
                for c in range(4):
                    gq = (c % 2) * 2
                    (gt, go), (ut, uo) = gu[gq], gu[gq + 1]
                    for kc in range(8):
                        P.op("pe", lambda t, kc=kc, c=c, gt=gt, go=go, s=s, tsl=tsl: t.matmul(
                            out=gt[:, go:go + TB], lhsT=wgs[s][:, kc, c * 128:(c + 1) * 128], rhs=hT[:, kc, tsl],
                            start=(kc == 0), stop=(kc == 7)), reads=[wgb[s]] + hTb[tb * SUB:(tb + 1) * SUB],
                            writes=[gub[gq]])
                    for kc in range(8):
                        P.op("pe", lambda t, kc=kc, c=c, ut=ut, uo=uo, s=s, tsl=tsl: t.matmul(
                            out=ut[:, uo:uo + TB], lhsT=wus[s][:, kc, c * 128:(c + 1) * 128], rhs=hT[:, kc, tsl],
                            start=(kc == 0), stop=(kc == 7)), reads=[wub[s]] + hTb[tb * SUB:(tb + 1) * SUB],
                            writes=[gub[gq + 1]])
                    sj = c % 2
                    P.op("act", lambda ac, gt=gt, go=go, sj=sj: ac.activation(out=sg[sj][:], in_=gt[:, go:go + TB],
                                                                              func=AF.Silu),
                         reads=[gub[gq]], writes=[sgb[sj]])
                    P.op("dve", lambda v, ut=ut, uo=uo, sj=sj, a=a, c=c: v.tensor_tensor(
                        out=aT[a][:, c, :], in0=sg[sj][:], in1=ut[:, uo:uo + TB], op=ALU.mult),
                        reads=[sgb[sj], gub[gq + 1]], writes=[aTb[a]])
                yield
                for ts in range(SUB):
                    i = tb * SUB + ts
                    for nh in range(2):
                        j = dcnt_[0] % 4
                        dcnt_[0] += 1
                        for c in range(4):
                            P.op("pe", lambda t, c=c, j=j, a=a, ts=ts, nh=nh, s=s: t.matmul(
                                out=psB[j][:], lhsT=aT[a][:, c, ts * 128:(ts + 1) * 128],
                                rhs=wds[s][:, c, nh * 512:(nh + 1) * 512], start=(c == 0), stop=(c == 3)),
                                reads=[aTb[a], wdb[s]], writes=[psBb[j]])
                        P.op("dve", lambda v, i=i, nh=nh, j=j, e=e: v.scalar_tensor_tensor(
                            out=x_sb[:, i, nh * 512:(nh + 1) * 512], in0=psB[j][:], scalar=combf[:, i, e:e + 1],
                            in1=x_sb[:, i, nh * 512:(nh + 1) * 512], op0=ALU.mult, op1=ALU.add),
                            reads=[psBb[j], combb, xb[i]], writes=[xb[i]])

        msteps = [(e, tb) for e in range(16) for tb in range(NTB)]
        pend = None
        for k_, (e, tb) in enumerate(msteps):
            g_ = moe_step(e, tb, k_ % 2, e % NS)
            next(g_)
            if pend is not None:
                for _ in pend:
                    pass
            pend = g_
            if tb == 0 and e + 1 < 16:
                load_expert(e + 1)
        for _ in pend:
            pass

    with C.phase() as st:
        hT = C.sb([128, 8, T], BF16, "h2T", st)
        hTb = [Buf() for _ in range(NT)]
        g32, g32b = load_gcol32(C, gple_d, "ple")
        wpg = C.sb([128, 8, 1024], BF16, "wpg", st)
        wpgb = Buf()
        wpp = C.sb([128, 2, 1024], BF16, "wpp", st)
        wppb = Buf()
        P.dma("pool", wpg[:], wpg_d.rearrange("(k p) n -> p k n", p=128), writes=[wpgb])
        P.dma("pool", wpp[:], wpp_d.rearrange("(k p) n -> p k n", p=128), writes=[wppb])
        p_sb = C.sb([128, NT, 256], F32, "p", st)
        pb = Buf()
        P.dma("sp", p_sb[:], p_d.rearrange("(n p) d -> p n d", p=128), writes=[pb])
        pT = C.sb([128, 2, T], BF16, "pT", st)
        pTb = [Buf() for _ in range(NT)]
        rms_to_hT(C, x_sb, xb, NT, g32, g32b, ident, ib, hT, hTb, psA, psAb, tag="p")
        for i in range(NT):
            j = i % 2
            for kc in range(2):
                P.op("pe", lambda t, i=i, kc=kc, j=j: t.transpose(out=psB[j][:, kc * 128:(kc + 1) * 128],
                                                                  in_=p_sb[:, i, kc * 128:(kc + 1) * 128],
                                                                  identity=ident[:]),
                     reads=[pb, ib], writes=[psBb[j]])
            P.op("act", lambda a, i=i, j=j: a.activation(out=pT[:, :, i * 128:(i + 1) * 128],
                                                          in_=psB[j][:, 0:256].rearrange("p (k t) -> p k t", k=2),
                                                          func=AF.Copy), reads=[psBb[j]], writes=[pTb[i]])
        sgp = [C.sb([128, 512], F32, f"sgp{s}", st) for s in range(2)]
        sgpb = [Buf() for _ in range(2)]
        gfin = C.sb([128, 1024], F32, "gfin", st)
        gfinb = Buf()
        if last:
            P.dma("sp", gfin[:], gfin_d.partition_broadcast(128), writes=[gfinb])
        ssf = C.sb([128, NT], F32, "ssf", st)
        rf = C.sb([128, NT], F32, "rf", st)
        junkf = C.sb([128, 1024], BF16, "junkf", st)
        junkfb = Buf()
        ssfb = [Buf() for _ in range(NT)]
        rfb = [Buf() for _ in range(NT)]
        ob = [C.sb([128, 1024], F32, f"ob{s}", st) for s in range(2)]
        obb = [Buf() for _ in range(2)]
        out_v = out_d.rearrange("(n p) d -> p n d", p=128)
        cnt = 0
        for i in range(NT):
            for nh in range(2):
                jg = (cnt % 2)
                jp = 2 + (cnt % 2)
                sj = cnt % 2
                cnt += 1
                for kc in range(8):
                    P.op("pe", lambda t, i=i, kc=kc, nh=nh, jg=jg: t.matmul(
                        out=psB[jg][:], lhsT=hT[:, kc, i * 128:(i + 1) * 128],
                        rhs=wpg[:, kc, nh * 512:(nh + 1) * 512], start=(kc == 0), stop=(kc == 7)),
                        reads=[hTb[i], wpgb], writes=[psBb[jg]])
                for kc in range(2):
                    P.op("pe", lambda t, i=i, kc=kc, nh=nh, jp=jp: t.matmul(
                        out=psB[jp][:], lhsT=pT[:, kc, i * 128:(i + 1) * 128],
                        rhs=wpp[:, kc, nh * 512:(nh + 1) * 512], start=(kc == 0), stop=(kc == 1)),
                        reads=[pTb[i], wppb], writes=[psBb[jp]])
                P.op("act", lambda a, jg=jg, sj=sj: a.activation(out=sgp[sj][:], in_=psB[jg][:], func=AF.Sigmoid),
                     reads=[psBb[jg]], writes=[sgpb[sj]])
                P.op("dve", lambda v, jp=jp, sj=sj: v.tensor_tensor(out=sgp[sj][:], in0=sgp[sj][:], in1=psB[jp][:],
                                                                     op=ALU.mult),
                     reads=[sgpb[sj], psBb[jp]], writes=[sgpb[sj]])
                P.op("dve", lambda v, i=i, nh=nh, sj=sj: v.tensor_tensor(
                    out=x_sb[:, i, nh * 512:(nh + 1) * 512], in0=x_sb[:, i, nh * 512:(nh + 1) * 512],
                    in1=sgp[sj][:], op=ALU.add), reads=[sgpb[sj], xb[i]], writes=[xb[i]])
            if last:
                o = i % 2
                P.op("pool", lambda g, i=i: g.memset(ssf[:, i:i + 1], 0.0), writes=[ssfb[i]])
                P.op("act", lambda a, i=i: a.activation(out=junkf[:], in_=x_sb[:, i, :], func=AF.Square,
                                                         accum_out=ssf[:, i:i + 1]),
                     reads=[xb[i]], writes=[junkfb, ssfb[i]])
                P.op("act", lambda a, i=i: a.activation(out=rf[:, i:i + 1], in_=ssf[:, i:i + 1], func=AF.Sqrt,
                                                         scale=1.0 / 1024.0, bias=EPS),
                     reads=[ssfb[i]], writes=[rfb[i]])
                P.op("dve", lambda v, i=i: v.reciprocal(out=rf[:, i:i + 1], in_=rf[:, i:i + 1]), reads=[rfb[i]],
                     writes=[rfb[i]])
                P.op("dve", lambda v, i=i, o=o: v.scalar_tensor_tensor(
                    out=ob[o][:], in0=x_sb[:, i, :], scalar=rf[:, i:i + 1], in1=gfin[:], op0=ALU.mult,
                    op1=ALU.mult), reads=[xb[i], rfb[i], gfinb], writes=[obb[o]])
                P.dma("sp", out_v[:, i, :], ob[o][:], reads=[obb[o]], is_output=True)
            else:
                P.dma("sp", out_v[:, i, :], x_sb[:, i, :], reads=[xb[i]], writes=out_writes, is_output=out_is_output)
        if standalone:
            P.final_wait()
    if "h_dst" in fz:
        h_rows = fz["h_dst"]
        with C.phase() as st:
            hTn = C.sb([128, 8, T], BF16, "hTn", st)
            hTnb = [Buf() for _ in range(NT)]
            gn, gnb = load_gcol32(C, gnext_d, "nxt")
            rms_to_hT(C, x_sb, xb, NT, gn, gnb, ident, ib, hTn, hTnb, psA, psAb, tag="n")
            for k0 in range(0, 8, 2):
                hap, hB_ = h_rows[k0 // 2]
                P.dma("sp", hap.rearrange("(k p) t -> p k t", p=128), hTn[:, k0:k0 + 2, :], reads=hTnb, writes=[hB_])
    if standalone:
        P.flush()
    return C


def finalize_heads(C, hacc, haccb, gt, gtb, NT, identb, ibb, pbank, outT, outTb, tag):
    P = C.P
    ssq = C.sb([128, NT * 2], F32, "fssq" + tag)
    ssqb = Buf()
    on = C.sb([128, NT, 128], BF16, "fon" + tag)
    onb = Buf()
    P.op("dve", lambda v: v.tensor_tensor(out=on[:], in0=hacc[:], in1=hacc[:], op=ALU.mult), reads=haccb, writes=[onb])
    P.op("dve", lambda v: v.tensor_reduce(out=ssq[:], in_=on[:].rearrange("p n (h e) -> p (n h) e", h=2), axis=AX.X,
                                          op=ALU.add), reads=[onb], writes=[ssqb])
    P.op("act", lambda a: a.activation(out=ssq[:], in_=ssq[:], func=AF.Sqrt, scale=1.0 / 64.0, bias=EPS),
         reads=[ssqb], writes=[ssqb])
    P.op("dve", lambda v: v.reciprocal(out=ssq[:], in_=ssq[:]), reads=[ssqb], writes=[ssqb])
    P.op("dve", lambda v: v.tensor_tensor(out=hacc[:].rearrange("p n (h e) -> p (n h) e", h=2),
                                          in0=hacc[:].rearrange("p n (h e) -> p (n h) e", h=2),
                                          in1=bc(ssq[:], 2, [128, NT * 2, 64]), op=ALU.mult),
         reads=haccb + [ssqb], writes=haccb)
    P.op("dve", lambda v: v.tensor_tensor(out=on[:], in0=hacc[:], in1=gt[:], op=ALU.mult), reads=haccb + gtb,
         writes=[onb])
    GRP = min(4, NT)
    pb1 = Buf()
    for g0 in range(0, NT, GRP):
        for k in range(GRP):
            P.op("pe", lambda t, g0=g0, k=k: t.transpose(out=pbank[:, k * 128:(k + 1) * 128], in_=on[:, g0 + k, :],
                                                         identity=identb[:]),
                 reads=[onb, ibb], writes=[pb1])
        ob_ = Buf()
        outTb.append(ob_)
        P.op("dve", lambda a, g0=g0: a.tensor_copy(out=outT[:, g0 * 128:(g0 + GRP) * 128], in_=pbank[:, 0:GRP * 128]),
             reads=[pb1], writes=[ob_])


def build_k1(T, phases="AGM", stop_at=None, C=None, sfx="", hsrc=None, mix_dst=None):
    standalone = C is None
    if C is None:
        C = Ctx()
        C.P.stop_at = stop_at
    nc, P = C.nc, C.P
    NT = T // 128
    NCH = T // 64
    TB = min(512, T)
    NTB = T // TB
    SUB = TB // 128

    if hsrc is None:
        x_d = C.din("x" + sfx, [T, 1024], F32)
        gmix_d = C.din("norm_mix_g" + sfx, [1024], F32)
    watt_d = C.din("w_att" + sfx, [1024, 448], F32)
    wglaf_d = C.din("w_gla_f" + sfx, [1024, 256], F32)
    wglat_d = C.din("w_gla_t" + sfx, [1024, 384], F32)
    wglr_d = C.din("w_glr" + sfx, [1024, 32], F32)
    wdb_d = C.din("wdb" + sfx, [2, 17, 128], F32)
    wmlf_d = C.din("w_ml_f" + sfx, [1024, 256], F32)
    wmlt_d = C.din("w_ml_t" + sfx, [1024, 264], F32)
    cw_d = C.din("cw" + sfx, [256, 4], F32)
    gatesb_d = C.din("gates_b" + sfx, [8], F32)
    g6_d = C.din("g6" + sfx, [384], F32)
    glag_d = C.din("gla_g" + sfx, [128], F32)
    mlg_d = C.din("ml_g" + sfx, [128], F32)
    rope_d = C.din("rope", [T, 128], F32)
    cf_d = C.din("cf", [2, 128, 128], F32)
    cs_d = C.din("cs", [6, 128, 128], F32)
    hm_d = C.din("hm", [128, 2], F32)
    if mix_dst is None:
        mixT_d = C.dout("mixT", [512, T], BF16)
        mix_rows = [(mixT_d[k * 128:(k + 1) * 128, :], []) for k in range(4)]
        mix_is_output = True
    else:
        mix_rows = [(ap_, [b_]) for ap_, b_ in mix_dst]
        mix_is_output = False

    hT = C.sb([128, 8, T], BF16, "hT")
    hTb = [Buf() for _ in range(NT)]
    ident, ib = load_ident(C)
    identb, ibb = load_ident(C, BF16)
    cf = C.sb([128, 2, 128], F32, "cf")
    cfb = Buf()
    P.dma("sp", cf[:], cf_d.rearrange("c p n -> p c n"), writes=[cfb])
    ones = C.sb([128, 128], F32, "ones")
    onesb = Buf()
    P.op("pool", lambda g: g.memset(ones[:], 1.0), writes=[onesb])

    def hT_blk(tb):
        return hTb[tb * SUB:(tb + 1) * SUB]

    if hsrc is not None:
        T2_ = T // 2
        for r_ in range(2):
            for k0 in range(0, 8, 2):
                hap, hgB = hsrc[(r_, k0)]
                P.dma("sp", hT[:, k0:k0 + 2, r_ * T2_:(r_ + 1) * T2_], hap.rearrange("(k p) t -> p k t", p=128),
                      reads=[hgB], writes=hTb[r_ * (NT // 2):(r_ + 1) * (NT // 2)])
    for _once in ([] if hsrc is not None else [0]):
      with C.phase():
        PS = [C.sb, None]
        ps2 = [C.ps([128, 1024], F32, f"p0_{j}") for j in range(2)]
        ps2b = [Buf() for _ in range(2)]
        g, gb = load_gcol32(C, gmix_d, "mix")
        xg = [C.sb([128, 4, 1024], F32, f"xg{s}") for s in range(2)]
        xgb = [[Buf() for _ in range(4)] for _ in range(2)]
        ss = C.sb([128, NT], F32, "ss")
        r = C.sb([128, NT], F32, "r")
        ssb = [Buf() for _ in range(NT)]
        rb = [Buf() for _ in range(NT)]
        junk = C.sb([128, 1024], BF16, "junk")
        junkb = Buf()
        xs = [C.sb([128, 1024], F32, f"xs{j}") for j in range(2)]
        xsb = [Buf() for _ in range(2)]
        x_v = x_d.rearrange("(n p) d -> p n d", p=128)
        P.op("pool", lambda g_: g_.memset(ss[:], 0.0), writes=ssb)
        for i in range(NT):
            gi, k = divmod(i, 4)
            s = gi % 2
            j = i % 2
            if k == 0:
                n = min(4, NT - i)
                P.dma("sp", xg[s][:, 0:n, :], x_v[:, i:i + n, :], writes=xgb[s][0:n])
            P.op("act", lambda a, i=i, s=s, k=k: a.activation(out=junk[:], in_=xg[s][:, k, :], func=AF.Square,
                                                              accum_out=ss[:, i:i + 1]),
                 reads=[xgb[s][k]], writes=[junkb, ssb[i]])
            P.op("act", lambda a, i=i: a.activation(out=r[:, i:i + 1], in_=ss[:, i:i + 1], func=AF.Sqrt,
                                                     scale=1.0 / 1024.0, bias=EPS), reads=[ssb[i]], writes=[rb[i]])
            P.op("dve", lambda v, i=i: v.reciprocal(out=r[:, i:i + 1], in_=r[:, i:i + 1]), reads=[rb[i]],
                 writes=[rb[i]])
            P.op("act", lambda a, i=i, s=s, k=k, j=j: a.activation(out=xs[j][:], in_=xg[s][:, k, :], func=AF.Copy,
                                                                   scale=r[:, i:i + 1]),
                 reads=[xgb[s][k], rb[i]], writes=[xsb[j]])
            for kc in range(8):
                P.op("pe", lambda t, j=j, kc=kc: t.transpose(out=ps2[j][:, kc * 128:(kc + 1) * 128],
                                                             in_=xs[j][:, kc * 128:(kc + 1) * 128],
                                                             identity=ident[:]),
                     reads=[xsb[j], ib], writes=[ps2b[j]])
            P.op("dve", lambda v, i=i, j=j: v.tensor_tensor(out=hT[:, :, i * 128:(i + 1) * 128],
                                                             in0=ps2[j][:].rearrange("p (k t) -> p k t", k=8),
                                                             in1=bc(g[:], 2, [128, 8, 128]), op=ALU.mult),
                 reads=[ps2b[j], gb], writes=[hTb[i]])

    def psum_banks(n, dt=F32, cols=512):
        ts = [C.cur.enter_context(nc.psum_tensor(f"pb{C._n}_{k}", [128, cols], dt)) for k in range(n)]
        C._n += 1
        return ts, [Buf() for _ in range(n)]

    if "A" in phases:
        with C.phase():
            zps, zpsb = psum_banks(2)
            tps, tpsb = psum_banks(1, BF16, 1024)
            sps, spsb = psum_banks(3)
            ops_, opsb = psum_banks(2)
            watt = C.sb([128, 8, 448], BF16, "watt")
            wattb = Buf()
            P.dma("pool", watt[:], watt_d.rearrange("(k p) n -> p k n", p=128), writes=[wattb])
            rope = C.sb([128, NT, 128], F32, "rope")
            ropeb = Buf()
            P.dma("sp", rope[:], rope_d.rearrange("(n p) c -> p n c", p=128), writes=[ropeb])
            g6 = C.sb([128, 384], F32, "g6")
            g6b = Buf()
            P.dma("sp", g6[:], g6_d.partition_broadcast(128), writes=[g6b])
            qkT = C.sb([128, 3, T], BF16, "qkT")
            qkTb = [Buf() for _ in range(NT)]
            va0 = C.sb([128, NT, 65], BF16, "va0")
            va1 = C.sb([128, NT, 128], BF16, "va1")
            vab = [Buf() for _ in range(NT)]
            mixA = C.sb([128, 2, T], BF16, "mixA")
            mixAb = Buf()
            P.op("pool", lambda g_: g_.memset(va0[:], 1.0), writes=vab)
            P.op("pool", lambda g_: g_.memset(va1[:], 0.0), writes=vab)
            P.op("pool", lambda g_: g_.memset(va1[:, :, 0:1], 1.0), writes=vab)
            amx = C.sb([128, 2], F32, "amx")
            amxb = Buf()
            nb = C.sb([128, 1], F32, "nb")
            nbb = Buf()
            P.op("dve", lambda v: v.tensor_reduce(out=amx[:, 0:1], in_=g6[:, 0:64], axis=AX.X, op=ALU.max,
                                                  apply_absolute_value=True), reads=[g6b], writes=[amxb])
            P.op("dve", lambda v: v.tensor_reduce(out=amx[:, 1:2], in_=g6[:, 256:320], axis=AX.X, op=ALU.max,
                                                  apply_absolute_value=True), reads=[g6b, amxb], writes=[amxb])
            P.op("dve", lambda v: v.tensor_tensor(out=nb[:], in0=amx[:, 0:1], in1=amx[:, 1:2], op=ALU.mult),
                 reads=[amxb], writes=[nbb])
            P.op("dve", lambda v: v.tensor_scalar(out=nb[:], in0=nb[:], scalar1=-8.0, scalar2=None, op0=ALU.mult),
                 reads=[nbb], writes=[nbb])

            def T2(nm, shape, dt=F32):
                return [C.sb(shape, dt, f"{nm}{j}") for j in range(2)], [Buf() for _ in range(2)]
            zsb_, zsbb = T2("zsb", [128, 384])
            sq_, sqb_ = T2("asq", [128, 384])
            ssq_, ssqb_ = T2("assq", [128, 6])
            qn_, qnb_ = T2("qn", [128, 384])
            t1_, t1b_ = T2("t1", [128, 384])
            t2_, t2b_ = T2("t2", [128, 384])
            qr_, qrb_ = T2("qr", [128, 384], BF16)
            def a_tile(i, j):
                for kc in range(8):
                    P.op("pe", lambda t, i=i, kc=kc, j=j: t.matmul(out=zps[j][:, 0:448], lhsT=hT[:, kc, i * 128:(i + 1) * 128],
                                                                  rhs=watt[:, kc, :], start=(kc == 0), stop=(kc == 7)),
                         reads=[hTb[i], wattb], writes=[zpsb[j]])
                yield
                P.op("act", lambda a, j=j: a.activation(out=zsb_[j][:], in_=zps[j][:, 0:384], func=AF.Copy),
                     reads=[zpsb[j]], writes=[zsbb[j]])
                yield
                P.op("act", lambda a, i=i, j=j: a.activation(out=va0[:, i, 0:64], in_=zps[j][:, 384:448], func=AF.Copy),
                     reads=[zpsb[j]], writes=[vab[i]])
                yield
                P.op("act", lambda a, i=i, j=j: a.activation(out=va1[:, i, 64:128], in_=zps[j][:, 384:448], func=AF.Copy),
                     reads=[zpsb[j]], writes=[vab[i]])
                yield
                P.op("dve", lambda v, j=j: v.tensor_tensor(out=sq_[j][:], in0=zsb_[j][:], in1=zsb_[j][:], op=ALU.mult),
                     reads=[zsbb[j]], writes=[sqb_[j]])
                yield
                P.op("dve", lambda v, j=j: v.tensor_reduce(out=ssq_[j][:], in_=sq_[j][:].rearrange("p (h e) -> p h e", h=6),
                                                            axis=AX.X, op=ALU.add), reads=[sqb_[j]], writes=[ssqb_[j]])
                yield
                P.op("act", lambda a, j=j: a.activation(out=ssq_[j][:], in_=ssq_[j][:], func=AF.Sqrt, scale=1.0 / 64.0,
                                                         bias=EPS), reads=[ssqb_[j]], writes=[ssqb_[j]])
                yield
                P.op("dve", lambda v, j=j: v.reciprocal(out=ssq_[j][:], in_=ssq_[j][:]), reads=[ssqb_[j]],
                     writes=[ssqb_[j]])
                yield
                P.op("dve", lambda v, j=j: v.tensor_tensor(out=qn_[j][:].rearrange("p (h e) -> p h e", h=6),
                                                            in0=zsb_[j][:].rearrange("p (h e) -> p h e", h=6),
                                                            in1=bc(ssq_[j][:], 2, [128, 6, 64]), op=ALU.mult),
                     reads=[zsbb[j], ssqb_[j]], writes=[qnb_[j]])
                yield
                P.op("dve", lambda v, j=j: v.tensor_tensor(out=qn_[j][:], in0=qn_[j][:], in1=g6[:], op=ALU.mult),
                     reads=[qnb_[j], g6b], writes=[qnb_[j]])
                yield
                P.op("dve", lambda v, i=i, j=j: v.tensor_tensor(out=t1_[j][:].rearrange("p (h e) -> p h e", h=6),
                                                                 in0=qn_[j][:].rearrange("p (h e) -> p h e", h=6),
                                                                 in1=bc(rope[:, i, 0:64], 1, [128, 6, 64]), op=ALU.mult),
                     reads=[qnb_[j], ropeb], writes=[t1b_[j]])
                yield
                for w in range(2):
                    P.op("dve", lambda v, i=i, j=j, w=w: v.tensor_tensor(
                        out=t2_[j][:].rearrange("p (h a w e) -> p h a w e", h=6, a=2, w=2)[:, :, :, w, :],
                        in0=qn_[j][:].rearrange("p (h a w e) -> p h a w e", h=6, a=2, w=2)[:, :, :, 1 - w, :],
                        in1=bc(rope[:, i, 64:128].rearrange("p (a w e) -> p a w e", a=2, w=2)[:, :, w, :], 1,
                               [128, 6, 2, 16]), op=ALU.mult),
                        reads=[qnb_[j], ropeb], writes=[t2b_[j]])
                yield
                P.op("dve", lambda v, j=j: v.tensor_tensor(out=qr_[j][:], in0=t1_[j][:], in1=t2_[j][:], op=ALU.add),
                     reads=[t1b_[j], t2b_[j]], writes=[qrb_[j]])
                yield
                for k in range(3):
                    P.op("pe", lambda t, j=j, k=k: t.transpose(out=tps[0][:, k * 128:(k + 1) * 128],
                                                               in_=qr_[j][:, k * 128:(k + 1) * 128], identity=identb[:]),
                         reads=[qrb_[j], ibb], writes=[tpsb[0]])
                P.op("act", lambda a, i=i: a.activation(out=qkT[:, :, i * 128:(i + 1) * 128],
                                                         in_=tps[0][:, 0:384].rearrange("p (k t) -> p k t", k=3),
                                                         func=AF.Copy), reads=[tpsb[0]], writes=[qkTb[i]])

            for i0 in range(0, NT, 2):
                gens = [a_tile(i0 + k_, k_) for k_ in range(min(2, NT - i0))]
                live = [True] * len(gens)
                while any(live):
                    for k_ in range(len(gens)):
                        if live[k_]:
                            try:
                                next(gens[k_])
                            except StopIteration:
                                live[k_] = False

            pe_ = [C.sb([128, TB], BF16, f"pexp{s}") for s in range(3)]
            peb = [Buf() for _ in range(3)]
            denr = C.sb([128, TB], F32, "denr")
            denrb = Buf()
            bcs = C.sb([128, TB], F32, "bcs")
            bcsb = Buf()
            pe_.append(C.sb([128, TB], BF16, "pexp3"))
            peb.append(Buf())
            slots = [(sps[0], spsb[0]), (sps[1], spsb[1]), (sps[2], spsb[2]), (zps[0], zpsb[0])]
            steps = [(pr, qb, kt) for pr in range(2) for qb in range(NTB) for kt in range(NT)]

            def emit_S(j):
                pr, qb, kt = steps[j]
                qsl = slice(qb * TB, (qb + 1) * TB)
                for hh in range(2):
                    s = (2 * j + hh) % 4
                    rows = slice(hh * 64, (hh + 1) * 64)
                    P.op("pe", lambda t, s=s, rows=rows: t.matmul(out=slots[s][0][:, 0:TB],
                                                                  lhsT=qkT[rows, 2, kt * 128:(kt + 1) * 128],
                                                                  rhs=qkT[rows, pr, qsl], start=True, stop=True),
                         reads=[qkTb[kt]] + qkTb[qb * SUB:(qb + 1) * SUB], writes=[slots[s][1]])
                for hh in range(2):
                    s = (2 * j + hh) % 4
                    P.op("act", lambda a, s=s: a.activation(out=pe_[s][:], in_=slots[s][0][:, 0:TB], func=AF.Exp,
                                                            bias=nb[:, 0:1], scale=0.125),
                         reads=[slots[s][1], nbb], writes=[peb[s]])

            def emit_O(j):
                pr, qb, kt = steps[j]
                qsl = slice(qb * TB, (qb + 1) * TB)
                s0, s1 = (2 * j) % 4, (2 * j + 1) % 4
                P.op("pe", lambda t: t.matmul(out=ops_[0][0:65, 0:TB], lhsT=va0[:, kt, :], rhs=pe_[s0][:],
                                              start=(kt == 0), stop=(kt == NT - 1)),
                     reads=[vab[kt], peb[s0]], writes=[opsb[0]])
                P.op("pe", lambda t: t.matmul(out=ops_[1][:, 0:TB], lhsT=va1[:, kt, :], rhs=pe_[s1][:],
                                              start=(kt == 0), stop=(kt == NT - 1)),
                     reads=[vab[kt], peb[s1]], writes=[opsb[1]])
                if kt == NT - 1:
                    for h2 in range(2):
                        fin(pr, qsl, h2)

            def fin(pr, qsl, hh):
                dr = slice(64, 65) if hh == 0 else slice(0, 1)
                rows = slice(hh * 64, (hh + 1) * 64)
                P.op("dve", lambda v: v.reciprocal(out=denr[dr, :], in_=ops_[hh][dr, 0:TB]),
                     reads=[opsb[hh]], writes=[denrb])
                if hh == 0:
                    P.op("pe", lambda t: t.matmul(out=zps[1][0:64, 0:TB], lhsT=ones[dr, 0:64], rhs=denr[dr, :],
                                                  start=True, stop=True), reads=[denrb, onesb], writes=[zpsb[1]])
                else:
                    P.op("pe", lambda t: t.matmul(out=zps[1][:, 0:TB], lhsT=ones[dr, :], rhs=denr[dr, :],
                                                  start=True, stop=True), reads=[denrb, onesb], writes=[zpsb[1]])
                P.op("dve", lambda v: v.tensor_copy(out=bcs[rows, :], in_=zps[1][rows, 0:TB]),
                     reads=[zpsb[1]], writes=[bcsb])
                P.op("dve", lambda v: v.tensor_tensor(out=mixA[rows, pr, qsl], in0=ops_[hh][rows, 0:TB],
                                                      in1=bcs[rows, :], op=ALU.mult),
                     reads=[opsb[hh], bcsb], writes=[mixAb])

            for j in range(len(steps) + 1):
                if j < len(steps):
                    emit_S(j)
                if j >= 1:
                    emit_O(j - 1)
            for pr in range(2):
                P.dma("sp", mix_rows[pr][0], mixA[:, pr, :], reads=[mixAb], writes=mix_rows[pr][1], is_output=mix_is_output)

    if "G" in phases:
        with C.phase():
            pz, pzb = psum_banks(2)
            pc, pcb = psum_banks(2)
            pa, pab = psum_banks(2)
            C._n += 1
            po_ctx = [nc.psum_tensor(f"po{C._n}_{k}", [128, 512], F32) for k in range(2)]
            po = [c_.__enter__() for c_ in po_ctx]
            cs = C.sb([128, 6, 128], BF16, "cs")
            csb = Buf()
            P.dma("pool", cs[:], cs_d.rearrange("c p n -> p c n"), writes=[csb])
            wf = C.sb([128, 8, 256], BF16, "wglaf")
            wfb = Buf()
            P.dma("pool", wf[:], wglaf_d.rearrange("(k p) n -> p k n", p=128), writes=[wfb])
            wt = C.sb([128, 8, 384], BF16, "wglat")
            wtb = Buf()
            P.dma("pool", wt[:], wglat_d.rearrange("(k p) n -> p k n", p=128), writes=[wtb])
            wl = C.sb([128, 8, 32], BF16, "wglr")
            wlb = Buf()
            P.dma("pool", wl[:], wglr_d.rearrange("(k p) n -> p k n", p=128), writes=[wlb])
            wdb = C.sb([17, 2, 128], F32, "wdb")
            wdbb = Buf()
            P.dma("sp", wdb[:], wdb_d.rearrange("d r n -> r d n"), writes=[wdbb])
            gg_ = C.sb([128, 128], F32, "glag")
            ggb = Buf()
            P.dma("sp", gg_[:], glag_d.partition_broadcast(128), writes=[ggb])
            qgT = C.sb([128, T], BF16, "qgT")
            kgT = C.sb([128, T], BF16, "kgT")
            qgTb = [Buf() for _ in range(NTB)]
            kgTb = [Buf() for _ in range(NTB)]
            ktm = C.sb([128, NT, 128], BF16, "ktm")
            vtm = C.sb([128, NT, 128], BF16, "vtm")
            gate = C.sb([128, NT, 128], F32, "ggate")
            tmb = [Buf() for _ in range(NT)]
            la = [C.sb([128, NT, 128], BF16, f"la{d}") for d in range(2)]
            lab = [[Buf() for _ in range(NT)] for _ in range(2)]
            lrT = [C.sb([17, TB], F32, f"lrT{d}") for d in range(2)]
            lrTb = [Buf() for _ in range(2)]
            oacc = C.sb([128, NT, 128], F32, "oacc")
            oaccb = [Buf() for _ in range(NT)]
            mixG = C.sb([128, T], BF16, "mixG")
            mixGb = []
            for d in range(2):
                P.op("pool", lambda g_, d=d: g_.memset(lrT[d][:], 1.0), writes=[lrTb[d]])
            etmp = C.sb([128, 128], F32, "etmp")
            etmpb = Buf()
            for tb in range(NTB):
                tsl = slice(tb * TB, (tb + 1) * TB)
                for qk in range(2):
                    j = qk
                    for kc in range(8):
                        P.op("pe", lambda t, kc=kc, qk=qk, j=j, tsl=tsl: t.matmul(
                            out=pz[j][:, 0:TB], lhsT=wf[:, kc, qk * 128:(qk + 1) * 128], rhs=hT[:, kc, tsl],
                            start=(kc == 0), stop=(kc == 7)), reads=[wfb] + hT_blk(tb), writes=[pzb[j]])
                    if qk == 0:
                        P.op("act", lambda a, j=j, tsl=tsl: a.activation(out=qgT[:, tsl], in_=pz[j][:, 0:TB], func=AF.Copy,
                                                                         scale=0.125), reads=[pzb[j]], writes=[qgTb[tb]])
                    else:
                        P.op("act", lambda a, j=j, tsl=tsl: a.activation(out=kgT[:, tsl], in_=pz[j][:, 0:TB], func=AF.Copy),
                             reads=[pzb[j]], writes=[kgTb[tb]])
                for d in range(2):
                    j = d
                    for kc in range(8):
                        P.op("pe", lambda t, kc=kc, d=d, j=j, tsl=tsl: t.matmul(
                            out=pc[j][0:16, 0:TB], lhsT=wl[:, kc, d * 16:(d + 1) * 16], rhs=hT[:, kc, tsl],
                            start=(kc == 0), stop=(kc == 7)), reads=[wlb] + hT_blk(tb), writes=[pcb[j]])
                    P.op("dve", lambda v, d=d, j=j: v.tensor_copy(out=lrT[d][0:16, :], in_=pc[j][0:16, 0:TB]),
                         reads=[pcb[j]], writes=[lrTb[d]])
                for ts in range(SUB):
                    i = tb * SUB + ts
                    j = i % 2
                    for kc in range(8):
                        P.op("pe", lambda t, i=i, kc=kc, j=j: t.matmul(out=pz[j][:, 0:384], lhsT=hT[:, kc, i * 128:(i + 1) * 128],
                                                                      rhs=wt[:, kc, :], start=(kc == 0), stop=(kc == 7)),
                             reads=[hTb[i], wtb], writes=[pzb[j]])
                    P.op("act", lambda a, i=i, j=j: a.activation(out=ktm[:, i, :], in_=pz[j][:, 0:128], func=AF.Copy),
                         reads=[pzb[j]], writes=[tmb[i]])
                    P.op("act", lambda a, i=i, j=j: a.activation(out=vtm[:, i, :], in_=pz[j][:, 128:256], func=AF.Copy),
                         reads=[pzb[j]], writes=[tmb[i]])
                    P.op("act", lambda a, i=i, j=j: a.activation(out=gate[:, i, :], in_=pz[j][:, 256:384], func=AF.Silu),
                         reads=[pzb[j]], writes=[tmb[i]])
                    P.op("dve", lambda v, i=i: v.tensor_tensor(out=gate[:, i, :], in0=gate[:, i, :], in1=gg_[:], op=ALU.mult),
                         reads=[tmb[i], ggb], writes=[tmb[i]])
                    for d in range(2):
                        jj = d
                        P.op("pe", lambda t, d=d, jj=jj, ts=ts: t.matmul(out=pa[jj][:, 0:128],
                                                                        lhsT=lrT[d][0:17, ts * 128:(ts + 1) * 128],
                                                                        rhs=wdb[0:17, d, :], start=True, stop=True),
                             reads=[lrTb[d], wdbb], writes=[pab[jj]])
                        P.op("act", lambda a, jj=jj: a.activation(out=etmp[:], in_=pa[jj][:, 0:128], func=AF.Exp, scale=-1.0),
                             reads=[pab[jj]], writes=[etmpb])
                        P.op("act", lambda a, d=d, i=i: a.activation(out=la[d][:, i, :], in_=etmp[:], func=AF.Ln, bias=1.0),
                             reads=[etmpb], writes=[lab[d][i]])

            def G2(nm, shape, dt):
                return [C.sb(shape, dt, f"{nm}{j}") for j in range(2)], [Buf() for _ in range(2)]
            ebm_, ebmb = G2("ebm", [128, 128], F32)
            enbm_, enbmb = G2("enbm", [128, 128], F32)
            eb_, ebb = G2("eb", [128, 128], F32)
            ebl_, eblb = G2("ebl", [128, 128], F32)
            qd_, qdb = G2("qd", [128, 128], BF16)
            kd_, kdb = G2("kd", [128, 128], BF16)
            qbz_, qbzb = G2("qbz", [128, 2, 128], BF16)
            kl_, klb = G2("kl", [128, 128], BF16)
            at_, atb = G2("at", [128, 2, 128], BF16)
            for j in range(2):
                P.op("pool", lambda g_, j=j: g_.memset(qbz_[j][:], 0.0), writes=[qbzb[j]])
            S32d = [C.sb([128, 128], F32, f"S32_{d}") for d in range(2)]
            S32db = [Buf() for _ in range(2)]
            Sbfd = [[C.sb([128, 128], BF16, f"Sbf{d}_{j}") for j in range(2)] for d in range(2)]
            Sbfdb = [[Buf() for _ in range(2)] for _ in range(2)]
            for d in range(2):
                P.op("pool", lambda g_, d=d: g_.memset(S32d[d][:], 0.0), writes=[S32db[d]])
                for j in range(2):
                    P.op("pool", lambda g_, d=d, j=j: g_.memset(Sbfd[d][j][:], 0.0), writes=[Sbfdb[d][j]])
            it = 0
            scurd = [0, 0]
            seen = set()
            order = []
            for st_ in range(NT):
                order += [(0, st_), (1, NT - 1 - st_)]
            def g_body(d, i, j):
                chunks = (0, 1) if d == 0 else (1, 0)
                for _one in (0,):
                    tb = i // SUB
                    tsl = slice(i * 128, (i + 1) * 128)
                    P.op("pe", lambda t, d=d, i=i, j=j: t.matmul(out=pc[j][:, 0:128], lhsT=la[d][:, i, :], rhs=cs[:, 2 + d, :],
                                                                start=True, stop=True), reads=[lab[d][i], csb], writes=[pcb[j]])
                    P.op("pe", lambda t, d=d, i=i, j=j: t.matmul(out=pc[j][:, 128:256], lhsT=la[d][:, i, :], rhs=cs[:, 0 + d, :],
                                                                start=True, stop=True), reads=[lab[d][i], csb], writes=[pcb[j]])
                    P.op("pe", lambda t, d=d, i=i, j=j: t.matmul(out=pc[j][:, 256:384], lhsT=cs[:, 4 + d, :], rhs=la[d][:, i, :],
                                                                start=True, stop=True), reads=[lab[d][i], csb], writes=[pcb[j]])
                    yield
                    P.op("act", lambda a, j=j: a.activation(out=ebm_[j][:], in_=pc[j][:, 0:128], func=AF.Exp), reads=[pcb[j]],
                         writes=[ebmb[j]])
                    P.op("act", lambda a, j=j: a.activation(out=enbm_[j][:], in_=pc[j][:, 0:128], func=AF.Exp, scale=-1.0),
                         reads=[pcb[j]], writes=[enbmb[j]])
                    P.op("act", lambda a, j=j: a.activation(out=eb_[j][:], in_=pc[j][:, 128:256], func=AF.Exp), reads=[pcb[j]],
                         writes=[ebb[j]])
                    P.op("act", lambda a, j=j: a.activation(out=ebl_[j][:], in_=pc[j][:, 256:384], func=AF.Exp), reads=[pcb[j]],
                         writes=[eblb[j]])
                    yield
                    P.op("dve", lambda v, j=j, tsl=tsl: v.tensor_tensor(out=qd_[j][:], in0=qgT[:, tsl], in1=ebm_[j][:], op=ALU.mult),
                         reads=[qgTb[tb], ebmb[j]], writes=[qdb[j]])
                    P.op("dve", lambda v, j=j, tsl=tsl: v.tensor_tensor(out=kd_[j][:], in0=kgT[:, tsl], in1=enbm_[j][:], op=ALU.mult),
                         reads=[kgTb[tb], enbmb[j]], writes=[kdb[j]])
                    for c in range(2):
                        P.op("dve", lambda v, j=j, c=c, i=i: v.tensor_tensor(
                            out=qbz_[j][:, c, c * 64:(c + 1) * 64], in0=qgT[:, i * 128 + c * 64:i * 128 + (c + 1) * 64],
                            in1=eb_[j][:, c * 64:(c + 1) * 64], op=ALU.mult), reads=[qgTb[tb], ebb[j]], writes=[qbzb[j]])
                    P.op("dve", lambda v, j=j, i=i: v.tensor_tensor(out=kl_[j][:], in0=ktm[:, i, :], in1=ebl_[j][:], op=ALU.mult),
                         reads=[tmb[i], eblb[j]], writes=[klb[j]])
                    yield
                    S32, S32b, Sbf, Sbfb, scur = S32d[d], S32db[d], Sbfd[d], Sbfdb[d], scurd[d]
                    rb_ = [(pa[j], pab[j]), (pz[j], pzb[j])]
                    for h in range(2):
                        rows = slice(h * 64, (h + 1) * 64)
                        P.op("pe", lambda t, j=j, h=h, rows=rows, rb_=rb_: t.matmul(out=rb_[h][0][:, 0:128], lhsT=kd_[j][rows, :],
                                                                           rhs=qd_[j][rows, :], start=True, stop=True),
                             reads=[kdb[j], qdb[j]], writes=[rb_[h][1]])
                        P.op("dve", lambda v, j=j, d=d, h=h, rb_=rb_: v.tensor_tensor(out=at_[j][:, h, :], in0=rb_[h][0][:, 0:128],
                                                                         in1=cf[:, d, :], op=ALU.mult),
                             reads=[rb_[h][1], cfb], writes=[atb[j]])
                    for c in range(2):
                        crow = slice(c * 64, (c + 1) * 64)
                        P.op("pe", lambda t, j=j, c=c, crow=crow, i=i, rb_=rb_: t.matmul(out=rb_[c][0][:, 128:256],
                                                                                lhsT=kl_[j][crow, :], rhs=vtm[crow, i, :],
                                                                                start=True, stop=True),
                             reads=[klb[j], tmb[i]], writes=[rb_[c][1]])
                    yield
                    first = True
                    for c in chunks:
                        P.op("pe", lambda t, j=j, c=c, scur=scur, first=first, Sbf=Sbf: t.matmul(
                            out=po[j][:, 0:128], lhsT=qbz_[j][:, c, :], rhs=Sbf[scur][:], start=first, stop=False),
                            reads=[qbzb[j], Sbfb[scur]], writes=[poB[j]])
                        yield
                        first = False
                        dcol = (c * 64 + 63) if d == 0 else (c * 64)
                        for h in range(2):
                            rows = slice(h * 64, (h + 1) * 64)
                            P.op("dve", lambda v, j=j, c=c, h=h, rows=rows, dcol=dcol, rb_=rb_, S32=S32: v.scalar_tensor_tensor(
                                out=S32[rows, h * 64:(h + 1) * 64], in0=S32[rows, h * 64:(h + 1) * 64],
                                scalar=eb_[j][rows, dcol:dcol + 1],
                                in1=rb_[c][0][rows, 128 + h * 64:128 + (h + 1) * 64], op0=ALU.mult, op1=ALU.add),
                                reads=[S32b, ebb[j], rb_[c][1]], writes=[S32b])
                        yield
                        scur = 1 - scur
                        P.op("pool", lambda g_, scur=scur, Sbf=Sbf, S32=S32: g_.tensor_copy(out=Sbf[scur][:], in_=S32[:]),
                             reads=[S32b], writes=[Sbfb[scur]])
                        yield
                    for h in range(2):
                        P.op("pe", lambda t, j=j, h=h, i=i: t.matmul(out=po[j][:, h * 64:(h + 1) * 64], lhsT=at_[j][:, h, :],
                                                                     rhs=vtm[:, i, h * 64:(h + 1) * 64], start=False, stop=(h == 1)),
                             reads=[atb[j], tmb[i]], writes=[poB[j]])
                    yield
                    if i not in seen:
                        seen.add(i)
                        P.op("act", lambda a, i=i, j=j: a.activation(out=oacc[:, i, :], in_=po[j][:, 0:128], func=AF.Copy),
                             reads=[poB[j]], writes=[oaccb[i]])
                    else:
                        P.op("dve", lambda v, i=i, j=j: v.tensor_tensor(out=oacc[:, i, :], in0=oacc[:, i, :], in1=po[j][:, 0:128],
                                                                    op=ALU.add), reads=[poB[j], oaccb[i]], writes=[oaccb[i]])
                    scurd[d] = scur

            poB = [Buf(), Buf()]
            for p_ in range(NT):
                gens = [g_body(order[2 * p_][0], order[2 * p_][1], 0), g_body(order[2 * p_ + 1][0], order[2 * p_ + 1][1], 1)]
                live = [True, True]
                while any(live):
                    for k_ in range(2):
                        if live[k_]:
                            try:
                                next(gens[k_])
                            except StopIteration:
                                live[k_] = False
            P.barrier()
            P.flush()
            for c_ in reversed(po_ctx):
                c_.__exit__(None, None, None)
            pt, ptb = psum_banks(1, BF16, 1024)
            finalize_heads(C, oacc, oaccb, gate, tmb, NT, identb, ibb, pt[0], mixG, mixGb, "g")
            P.dma("sp", mix_rows[2][0], mixG[:], reads=mixGb, writes=mix_rows[2][1], is_output=mix_is_output)

    if "M" in phases:
        with C.phase():
            pz, pzb = psum_banks(2)
            prp, prpb = psum_banks(1)
            pst, pstb = psum_banks(2)
            pk, pkb = psum_banks(1)
            C._n += 1
            pt_ctx0 = nc.psum_tensor(f"ptm{C._n}", [128, 1024], BF16)
            pt = [pt_ctx0.__enter__()]
            wmf = C.sb([128, 8, 256], BF16, "wmlf")
            wmfb = Buf()
            P.dma("pool", wmf[:], wmlf_d.rearrange("(k p) n -> p k n", p=128), writes=[wmfb])
            wmt = C.sb([128, 8, 264], BF16, "wmlt")
            wmtb = Buf()
            P.dma("pool", wmt[:], wmlt_d.rearrange("(k p) n -> p k n", p=128), writes=[wmtb])
            cw = C.sb([128, 2, 4], F32, "cw")
            cwb = Buf()
            P.dma("sp", cw[:], cw_d.rearrange("(a p) c -> p a c", p=128), writes=[cwb])
            gb8 = C.sb([128, 8], F32, "gb8")
            gb8b = Buf()
            P.dma("sp", gb8[:], gatesb_d.partition_broadcast(128), writes=[gb8b])
            mlg = C.sb([128, 128], F32, "mlg")
            mlgb = Buf()
            P.dma("sp", mlg[:], mlg_d.partition_broadcast(128), writes=[mlgb])
            hm = C.sb([128, 2], F32, "hm")
            hmb = Buf()
            P.dma("sp", hm[:], hm_d, writes=[hmb])
            mqT = C.sb([128, T], BF16, "mqT")
            mkT = C.sb([128, T], BF16, "mkT")
            mqTb, mkTb = Buf(), Buf()
            vaug = C.sb([128, NT, 2, 65], BF16, "mvaug")
            smo = C.sb([128, NT, 128], F32, "smo")
            gts = C.sb([128, NT, 8], F32, "gts")
            tmb = [Buf() for _ in range(NT)]
            P.op("pool", lambda g_: g_.memset(vaug[:], 1.0), writes=tmb)
            with C.phase():
                raw = C.sb([128, T + 2], F32, "raw")
                rawb = Buf()
                y = C.sb([128, T], F32, "convy")
                yb = Buf()
                for qk in range(2):
                    P.op("pool", lambda g_: g_.memset(raw[:, 0:1], 0.0), writes=[rawb])
                    P.op("pool", lambda g_: g_.memset(raw[:, T + 1:T + 2], 0.0), writes=[rawb])
                    for tb in range(NTB):
                        j = tb % 2
                        tsl = slice(tb * TB, (tb + 1) * TB)
                        for kc in range(8):
                            P.op("pe", lambda t, kc=kc, qk=qk, j=j, tsl=tsl: t.matmul(
                                out=pz[j][:, 0:TB], lhsT=wmf[:, kc, qk * 128:(qk + 1) * 128], rhs=hT[:, kc, tsl],
                                start=(kc == 0), stop=(kc == 7)), reads=[wmfb] + hT_blk(tb), writes=[pzb[j]])
                        P.op("act", lambda a, j=j, tb=tb: a.activation(out=raw[:, 1 + tb * TB:1 + (tb + 1) * TB],
                                                                       in_=pz[j][:, 0:TB], func=AF.Copy),
                             reads=[pzb[j]], writes=[rawb])
                    P.op("dve", lambda v, qk=qk: v.tensor_scalar(out=y[:], in0=raw[:, 0:T], scalar1=cw[:, qk, 0:1],
                                                                  scalar2=cw[:, qk, 3:4], op0=ALU.mult, op1=ALU.add),
                         reads=[rawb, cwb], writes=[yb])
                    P.op("dve", lambda v, qk=qk: v.scalar_tensor_tensor(out=y[:], in0=raw[:, 1:T + 1], scalar=cw[:, qk, 1:2],
                                                                         in1=y[:], op0=ALU.mult, op1=ALU.add),
                         reads=[rawb, cwb, yb], writes=[yb])
                    P.op("dve", lambda v, qk=qk: v.scalar_tensor_tensor(out=y[:], in0=raw[:, 2:T + 2], scalar=cw[:, qk, 2:3],
                                                                         in1=y[:], op0=ALU.mult, op1=ALU.add),
                         reads=[rawb, cwb, yb], writes=[yb])
                    if qk == 0:
                        P.op("act", lambda a: a.activation(out=mqT[:], in_=y[:], func=AF.Silu), reads=[yb], writes=[mqTb])
                    else:
                        P.op("act", lambda a: a.activation(out=y[:], in_=y[:], func=AF.Silu), reads=[yb], writes=[yb])
                        P.op("pool", lambda g_: g_.tensor_scalar(out=mkT[:], in0=y[:], scalar1=0.125, scalar2=None,
                                                                 op0=ALU.mult), reads=[yb], writes=[mkTb])
            for i in range(NT):
                j = i % 2
                for kc in range(8):
                    P.op("pe", lambda t, i=i, kc=kc, j=j: t.matmul(out=pz[j][:, 0:264], lhsT=hT[:, kc, i * 128:(i + 1) * 128],
                                                                  rhs=wmt[:, kc, :], start=(kc == 0), stop=(kc == 7)),
                         reads=[hTb[i], wmtb], writes=[pzb[j]])
                P.op("act", lambda a, i=i, j=j: a.activation(out=vaug[:, i, :, 0:64],
                                                              in_=pz[j][:, 0:128].rearrange("p (h e) -> p h e", h=2),
                                                              func=AF.Copy), reads=[pzb[j]], writes=[tmb[i]])
                P.op("act", lambda a, i=i, j=j: a.activation(out=smo[:, i, :], in_=pz[j][:, 128:256], func=AF.Sigmoid),
                     reads=[pzb[j]], writes=[tmb[i]])
                P.op("dve", lambda v, i=i: v.tensor_tensor(out=smo[:, i, :], in0=smo[:, i, :], in1=mlg[:], op=ALU.mult),
                     reads=[tmb[i], mlgb], writes=[tmb[i]])
                P.op("dve", lambda v, i=i, j=j: v.tensor_tensor(out=gts[:, i, :], in0=pz[j][:, 256:264], in1=gb8[:], op=ALU.add),
                     reads=[pzb[j], gb8b], writes=[tmb[i]])

            def S(shape, nm, dt=F32):
                return C.sb(shape, dt, nm), Buf(nm)
            lf, lfb = S([128, NT, 4], "lf")
            cums, cumsb = S([128, NT, 4], "cums")
            aa, aab = S([128, NT, 4], "aa")
            xcat, xcatb = S([128, NT, 8], "xcat")
            xm, xmb = S([128, NT, 2, 8], "xm")
            LSE, LSEb = S([128, NCH, 4], "LSE")
            bl, blb = S([128, NCH, 4], "bl")
            ein, einb = S([128, NCH + 1, 4], "ein")
            einB = [Buf(), Buf()]
            tmx = [C.sb([128, 2], F32, f"tmx{d}") for d in range(2)]
            tmxb = [Buf(), Buf()]
            Mp, Mpb = S([128, NCH, 4], "Mp")
            wpv, wpvb = S([128, NCH, 4], "wpv")
            Mtm, Mtmb = S([128, NT, 4], "Mtm")
            wptm, wptmb = S([128, NT, 4], "wptm")
            es, esb = S([128, NT, 4], "es")
            fden, fdenb = S([128, NT, 4], "fden")
            P.op("act", lambda a: a.activation(out=lf[:], in_=gts[:, :, 4:8], func=AF.Exp, scale=-1.0), reads=tmb, writes=[lfb])
            P.op("act", lambda a: a.activation(out=lf[:], in_=lf[:], func=AF.Ln, bias=1.0), reads=[lfb], writes=[lfb])
            for d in range(2):
                P.op("pe", lambda t, d=d: t.matmul(out=prp[0][:, d * NT * 2:(d + 1) * NT * 2], lhsT=cf[:, d, :],
                                                   rhs=lf[:, :, d * 2:(d + 1) * 2], start=True, stop=True),
                     reads=[cfb, lfb], writes=[prpb[0]])
            for d in range(2):
                P.op("dve", lambda v, d=d: v.tensor_copy(out=cums[:, :, d * 2:(d + 1) * 2],
                                                         in_=prp[0][:, d * NT * 2:(d + 1) * NT * 2].rearrange("p (n s) -> p n s", s=2)),
                     reads=[prpb[0]], writes=[cumsb])
            P.op("dve", lambda v: v.tensor_tensor(out=aa[:], in0=gts[:, :, 0:4], in1=cums[:], op=ALU.add), reads=tmb + [cumsb],
                 writes=[aab])
            P.op("act", lambda a: a.activation(out=xcat[:, :, 0:4], in_=aa[:], func=AF.Exp), reads=[aab], writes=[xcatb])
            P.op("dve", lambda g_: g_.tensor_copy(out=xcat[:, :, 4:8], in_=lf[:]), reads=[lfb, xcatb], writes=[xcatb])
            for jh in range(2):
                P.op("dve", lambda v, jh=jh: v.tensor_scalar(out=xm[:, :, jh, :], in0=xcat[:], scalar1=hm[:, jh:jh + 1],
                                                              scalar2=None, op0=ALU.mult), reads=[xcatb, hmb], writes=[xmb])
            P.op("pe", lambda t: t.matmul(out=prp[0][:, 0:NT * 16], lhsT=ones[:],
                                          rhs=xm[:].rearrange("p n j s -> p (n j s)"), start=True, stop=True),
                 reads=[onesb, xmb, cumsb], writes=[prpb[0]])
            P.op("act", lambda a: a.activation(out=LSE[:],
                                               in_=prp[0][:, 0:NCH * 8].rearrange("p (n s) -> p n s", s=8)[:, :, 0:4],
                                               func=AF.Ln), reads=[prpb[0]], writes=[LSEb])
            P.op("act", lambda a: a.activation(out=bl[:],
                                               in_=prp[0][:, 0:NCH * 8].rearrange("p (n s) -> p n s", s=8)[:, :, 4:8],
                                               func=AF.Copy), reads=[prpb[0]], writes=[blb])
            P.op("pool", lambda g_: g_.memset(ein[:], -1e30), writes=[einb] + einB)
            for n in range(NCH):
                P.op("dve", lambda v, n=n: v.tensor_tensor(out=tmx[0][:], in0=LSE[:, n, 0:2], in1=ein[:, n, 0:2], op=ALU.max),
                     reads=[LSEb, einB[0]], writes=[tmxb[0]])
                P.op("dve", lambda v, n=n: v.tensor_tensor(out=ein[:, n + 1, 0:2], in0=tmx[0][:], in1=bl[:, n, 0:2],
                                                            op=ALU.subtract), reads=[tmxb[0], blb], writes=[einB[0]])
                m = NCH - 1 - n
                P.op("dve", lambda g_, m=m: g_.tensor_tensor(out=tmx[1][:], in0=LSE[:, m, 2:4], in1=ein[:, m + 1, 2:4],
                                                              op=ALU.max), reads=[LSEb, einB[1]], writes=[tmxb[1]])
                P.op("dve", lambda g_, m=m: g_.tensor_tensor(out=ein[:, m, 2:4], in0=tmx[1][:], in1=bl[:, m, 2:4],
                                                              op=ALU.subtract), reads=[tmxb[1], blb], writes=[einB[1]])
            P.op("dve", lambda v: v.tensor_tensor(out=Mp[:, :, 0:2], in0=LSE[:, :, 0:2], in1=ein[:, 0:NCH, 0:2], op=ALU.max),
                 reads=[LSEb] + einB, writes=[Mpb])
            P.op("dve", lambda v: v.tensor_tensor(out=Mp[:, :, 2:4], in0=LSE[:, :, 2:4], in1=ein[:, 1:NCH + 1, 2:4], op=ALU.max),
                 reads=[LSEb] + einB, writes=[Mpb])
            P.op("dve", lambda v: v.tensor_tensor(out=wpv[:, :, 0:2], in0=ein[:, 0:NCH, 0:2], in1=Mp[:, :, 0:2], op=ALU.subtract),
                 reads=[Mpb] + einB, writes=[wpvb])
            P.op("dve", lambda v: v.tensor_tensor(out=wpv[:, :, 2:4], in0=ein[:, 1:NCH + 1, 2:4], in1=Mp[:, :, 2:4], op=ALU.subtract),
                 reads=[Mpb] + einB, writes=[wpvb])
            P.op("act", lambda a: a.activation(out=wpv[:], in_=wpv[:], func=AF.Exp), reads=[wpvb], writes=[wpvb])
            for hf in range(2):
                rows = slice(hf * 64, (hf + 1) * 64)
                P.op("dve", lambda v, hf=hf, rows=rows: v.tensor_copy(
                    out=Mtm[rows, :, :], in_=Mp[rows, :, :].rearrange("p (i c) s -> p i c s", c=2)[:, :, hf, :]),
                    reads=[Mpb], writes=[Mtmb])
                P.op("dve", lambda v, hf=hf, rows=rows: v.tensor_copy(
                    out=wptm[rows, :, :], in_=wpv[rows, :, :].rearrange("p (i c) s -> p i c s", c=2)[:, :, hf, :]),
                    reads=[wpvb], writes=[wptmb])
            P.op("dve", lambda v: v.tensor_tensor(out=es[:], in0=aa[:], in1=Mtm[:], op=ALU.subtract), reads=[aab, Mtmb], writes=[esb])
            P.op("act", lambda a: a.activation(out=es[:], in_=es[:], func=AF.Exp), reads=[esb], writes=[esb])
            P.op("dve", lambda v: v.tensor_tensor(out=fden[:], in0=cums[:], in1=Mtm[:], op=ALU.subtract), reads=[cumsb, Mtmb],
                 writes=[fdenb])
            P.op("act", lambda a: a.activation(out=fden[:], in_=fden[:], func=AF.Exp), reads=[fdenb], writes=[fdenb])

            ktm = C.sb([128, NT, 128], BF16, "mktm")
            ktmb = [Buf() for _ in range(NT)]
            ptb1 = Buf()
            GRP = min(4, NT)
            for g0 in range(0, NT, GRP):
                for k in range(GRP):
                    P.op("pe", lambda t, g0=g0, k=k: t.transpose(out=pt[0][:, k * 128:(k + 1) * 128],
                                                                 in_=mkT[:, (g0 + k) * 128:(g0 + k + 1) * 128],
                                                                 identity=identb[:]), reads=[mkTb, ibb], writes=[ptb1])
                P.op("dve", lambda a, g0=g0: a.tensor_copy(out=ktm[:, g0:g0 + GRP, :],
                                                            in_=pt[0][:, 0:GRP * 128].rearrange("p (k t) -> p k t", k=GRP)),
                     reads=[ptb1], writes=ktmb[g0:g0 + GRP])
            qz = C.sb([128, NT, 2, 128], BF16, "qz")
            qzb = Buf()
            P.op("pool", lambda g_: g_.memset(qz[:], 0.0), writes=[qzb])
            for c in range(2):
                P.op("pool", lambda g_, c=c: g_.tensor_copy(out=qz[:, :, c, c * 64:(c + 1) * 64],
                                                            in_=mqT[:].rearrange("p (n c e) -> p n c e", c=2, e=64)[:, :, c, :]),
                     reads=[mqTb, qzb], writes=[qzb])
            hacc = C.sb([128, NT, 128], F32, "hacc")
            haccb = [Buf() for _ in range(NT)]
            mixM = C.sb([128, T], BF16, "mixM")
            mixMb = []

            def M2(nm, shape, dt):
                return [C.sb(shape, dt, f"{nm}{j}") for j in range(2)], [Buf() for _ in range(2)]
            st_, stb = M2("mst", [128, 2, 128], BF16)
            kw_, kwb = M2("mkw", [128, 128], BF16)
            isb_, isbb = M2("misb", [128, 130], F32)
            res_, resb = M2("mres", [128, 130], F32)
            den_, denb = M2("mden", [128, 2], F32)
            htmp_, htmpb = M2("mhtmp", [128, 128], F32)
            C32d = [C.sb([128, 130], F32, f"C32_{d}") for d in range(2)]
            C32db = [Buf(), Buf()]
            Cbfd = [[C.sb([128, 130], BF16, f"Cbf{d}_{j}") for j in range(2)] for d in range(2)]
            Cbfdb = [[Buf(), Buf()], [Buf(), Buf()]]
            kwz = [C.sb([128, 2, 128], BF16, f"kwz{j}") for j in range(2)]
            kwzb = [Buf(), Buf()]
            for d in range(2):
                P.op("pool", lambda g_, d=d: g_.memset(C32d[d][:], 0.0), writes=[C32db[d]])
                P.op("pool", lambda g_, d=d: g_.memset(kwz[d][:], 0.0), writes=[kwzb[d]])
                for j in range(2):
                    P.op("pool", lambda g_, d=d, j=j: g_.memset(Cbfd[d][j][:], 0.0), writes=[Cbfdb[d][j]])
            P.barrier()
            P.flush()
            pt_ctx0.__exit__(None, None, None)
            C._n += 1
            pi_ctx = [nc.psum_tensor(f"pi{C._n}_{k}", [128, 512], F32) for k in range(2)]
            pi2 = [c_.__enter__() for c_ in pi_ctx]
            pi2b = [Buf(), Buf()]
            kvb = [(pk[0], pkb[0]), (prp[0], prpb[0])]
            scurd = [0, 0]
            seen = set()

            def m_body(d, i, j):
                chunks = (0, 1) if d == 0 else (1, 0)
                tsl = slice(i * 128, (i + 1) * 128)
                sb_ = [(pst[j], pstb[j]), (pz[j], pzb[j])]
                kvt, kvtb = kvb[j]
                pit, pitb = pi2[j], pi2b[j]
                for h in range(2):
                    rows = slice(h * 64, (h + 1) * 64)
                    P.op("pe", lambda t, h=h, rows=rows: t.matmul(out=sb_[h][0][:, 0:128], lhsT=mkT[rows, tsl],
                                                                  rhs=mqT[rows, tsl], start=True, stop=True),
                         reads=[mkTb, mqTb], writes=[sb_[h][1]])
                for h in range(2):
                    sc = d * 2 + h
                    for c in range(2):
                        crow = slice(c * 64, (c + 1) * 64)
                        P.op("act", lambda a, h=h, sc=sc, c=c, crow=crow: a.activation(
                            out=kwz[j][crow, c, h * 64:(h + 1) * 64], in_=ktm[crow, i, h * 64:(h + 1) * 64], func=AF.Copy,
                            scale=es[crow, i, sc:sc + 1]), reads=[ktmb[i], esb], writes=[kwzb[j]])
                yield
                for h in range(2):
                    sc = d * 2 + h
                    P.op("dve", lambda v, h=h, sc=sc: v.scalar_tensor_tensor(
                        out=st_[j][:, h, :], in0=sb_[h][0][:, 0:128], scalar=es[:, i, sc:sc + 1],
                        in1=cf[:, d, :], op0=ALU.mult, op1=ALU.mult), reads=[sb_[h][1], esb, cfb], writes=[stb[j]])
                for c in range(2):
                    P.op("pe", lambda t, c=c: t.matmul(out=kvt[:, c * 130:(c + 1) * 130], lhsT=kwz[j][:, c, :],
                                                       rhs=vaug[:, i, :, :].rearrange("p h e -> p (h e)"),
                                                       start=True, stop=True),
                         reads=[kwzb[j], tmb[i]], writes=[kvtb])
                yield
                for h in range(2):
                    P.op("pe", lambda t, h=h: t.matmul(out=pit[:, h * 65:(h + 1) * 65], lhsT=st_[j][:, h, :],
                                                       rhs=vaug[:, i, h, :], start=True, stop=True),
                         reads=[stb[j], tmb[i]], writes=[pitb])
                yield
                C32, C32b, Cbf, Cbfb, scur = C32d[d], C32db[d], Cbfd[d], Cbfdb[d], scurd[d]
                first = True
                for ci, c in enumerate(chunks):
                    n = 2 * i + c
                    P.op("pe", lambda t, c=c, scur=scur, first=first, ci=ci: t.matmul(
                        out=pit[:, 130:260], lhsT=qz[:, i, c, :], rhs=Cbf[scur][:], start=first, stop=(ci == 1)),
                        reads=[qzb, Cbfb[scur]], writes=[pitb])
                    first = False
                    yield
                    for h in range(2):
                        rows = slice(h * 64, (h + 1) * 64)
                        sc = d * 2 + h
                        P.op("dve", lambda v, c=c, h=h, rows=rows, sc=sc, n=n: v.scalar_tensor_tensor(
                            out=C32[rows, h * 65:(h + 1) * 65], in0=C32[rows, h * 65:(h + 1) * 65],
                            scalar=wpv[rows, n, sc:sc + 1], in1=kvt[rows, c * 130 + h * 65:c * 130 + (h + 1) * 65],
                            op0=ALU.mult, op1=ALU.add), reads=[C32b, wpvb, kvtb], writes=[C32b])
                    yield
                    scur = 1 - scur
                    P.op("act", lambda a, scur=scur: a.activation(out=Cbf[scur][:], in_=C32[:], func=AF.Copy),
                         reads=[C32b], writes=[Cbfb[scur]])
                    yield
                scurd[d] = scur
                P.op("act", lambda a: a.activation(out=isb_[j][:], in_=pit[:, 0:130], func=AF.Copy),
                     reads=[pitb], writes=[isbb[j]])
                yield
                for h in range(2):
                    sc = d * 2 + h
                    P.op("dve", lambda v, h=h, sc=sc: v.scalar_tensor_tensor(
                        out=res_[j][:, h * 65:(h + 1) * 65], in0=pit[:, 130 + h * 65:130 + (h + 1) * 65],
                        scalar=wptm[:, i, sc:sc + 1], in1=isb_[j][:, h * 65:(h + 1) * 65], op0=ALU.mult, op1=ALU.add),
                        reads=[pitb, wptmb, isbb[j]], writes=[resb[j]])
                P.op("dve", lambda v: v.scalar_tensor_tensor(
                    out=den_[j][:], in0=res_[j][:].rearrange("p (h e) -> p h e", h=2)[:, :, 64], scalar=-1.0,
                    in1=res_[j][:].rearrange("p (h e) -> p h e", h=2)[:, :, 64], op0=ALU.mult, op1=ALU.max),
                    reads=[resb[j]], writes=[denb[j]])
                yield
                P.op("dve", lambda v: v.tensor_tensor(out=den_[j][:], in0=den_[j][:], in1=fden[:, i, d * 2:(d + 1) * 2],
                                                      op=ALU.max), reads=[denb[j], fdenb], writes=[denb[j]])
                P.op("dve", lambda v: v.reciprocal(out=den_[j][:], in_=den_[j][:]), reads=[denb[j]], writes=[denb[j]])
                yield
                if i not in seen:
                    seen.add(i)
                    P.op("dve", lambda v: v.tensor_tensor(
                        out=hacc[:, i, :].rearrange("p (h e) -> p h e", h=2),
                        in0=res_[j][:].rearrange("p (h e) -> p h e", h=2)[:, :, 0:64],
                        in1=bc(den_[j][:], 2, [128, 2, 64]), op=ALU.mult), reads=[resb[j], denb[j]], writes=[haccb[i]])
                else:
                    P.op("dve", lambda v: v.tensor_tensor(
                        out=htmp_[j][:].rearrange("p (h e) -> p h e", h=2),
                        in0=res_[j][:].rearrange("p (h e) -> p h e", h=2)[:, :, 0:64],
                        in1=bc(den_[j][:], 2, [128, 2, 64]), op=ALU.mult), reads=[resb[j], denb[j]], writes=[htmpb[j]])
                    P.op("pool", lambda g_: g_.tensor_tensor(out=hacc[:, i, :], in0=hacc[:, i, :], in1=htmp_[j][:],
                                                             op=ALU.add), reads=[htmpb[j], haccb[i]], writes=[haccb[i]])

            for p_ in range(NT):
                gens = [m_body(0, p_, 0), m_body(1, NT - 1 - p_, 1)]
                live = [True, True]
                while any(live):
                    for k_ in range(2):
                        if live[k_]:
                            try:
                                next(gens[k_])
                            except StopIteration:
                                live[k_] = False
            P.barrier()
            P.flush()
            for c_ in reversed(pi_ctx):
                c_.__exit__(None, None, None)
            pt, ptb = psum_banks(1, BF16, 1024)
            finalize_heads(C, hacc, haccb, smo, tmb, NT, identb, ibb, pt[0], mixM, mixMb, "m")
            P.dma("sp", mix_rows[3][0], mixM[:], reads=mixMb, writes=mix_rows[3][1], is_output=mix_is_output)
    if standalone:
        P.final_wait()
        P.flush()
    return C


PAIRS = [[0, 1], [2, 3], [4, 5], [6, 7]]


def build_fused(T):
    C = Ctx()
    nc, P = C.nc, C.P
    T2 = T // 2
    sel_d = C.din("sel", [128, 2], F32)
    sel = C.sb([128, 2], F32, "sel")
    selB = Buf()
    P.dma("sp", sel[:], sel_d, writes=[selB])
    mixb = [[nc.dram_tensor(f"mixb{l}_{c}", [256, T], BF16).ap() for c in range(2)] for l in range(2)]
    mixg = [[nc.dram_tensor(f"mixg{l}_{c}", [512, T], BF16).ap() for c in range(2)] for l in range(2)]
    hb = [nc.dram_tensor(f"hb_{c}", [512, T2], BF16).ap() for c in range(2)]
    hg = [nc.dram_tensor(f"hg_{c}", [1024, T2], BF16).ap() for c in range(2)]
    xs = nc.dram_tensor("xs", [T2, 1024], F32).ap()
    mixbB = [[Buf(), Buf()] for _ in range(2)]
    mixgB = [[Buf(), Buf()] for _ in range(2)]
    hbB, hgB, xsB = [Buf(), Buf()], [Buf(), Buf()], Buf()

    def stage(fn):
        ph = C.phase()
        ph.__enter__()
        fn()
        ph.__exit__(None, None, None)

    def mix_dst(l):
        return [(mixb[l][q // 2][(q % 2) * 128:(q % 2) * 128 + 128, :], mixbB[l][q // 2]) for q in range(4)]

    def mix_src(l):
        out = []
        for kc in range(8):
            r, q = kc // 4, kc % 4
            off = r * 256 + (q % 2) * 128
            out.append((mixg[l][q // 2][off:off + 128, :], mixgB[l][q // 2]))
        return out

    def gather_mix(l):
        for c in range(2):
            P.collective("AllGather", mixb[l][c], mixg[l][c], PAIRS, reads=[mixbB[l][c]], writes=[mixgB[l][c]])

    h_dst = [(hb[k0 // 4][(k0 % 4) * 128:(k0 % 4) * 128 + 256, :], hbB[k0 // 4]) for k0 in range(0, 8, 2)]
    h_src = {(r, k0): (hg[k0 // 4][r * 512 + (k0 % 4) * 128:r * 512 + (k0 % 4) * 128 + 256, :], hgB[k0 // 4])
             for r in range(2) for k0 in range(0, 8, 2)}

    stage(lambda: build_k1(T, C=C, sfx="_l0", mix_dst=mix_dst(0)))
    gather_mix(0)
    stage(lambda: build_k2(T2, False, C=C, sfx="_l0", fz=dict(mixg=mix_src(0), sel=(sel, selB),
                                                              x_dst=(xs, xsB), h_dst=h_dst)))
    for c in range(2):
        P.collective("AllGather", hb[c], hg[c], PAIRS, reads=[hbB[c]], writes=[hgB[c]])
    stage(lambda: build_k1(T, C=C, sfx="_l1", hsrc=h_src, mix_dst=mix_dst(1)))
    gather_mix(1)
    stage(lambda: build_k2(T2, True, C=C, sfx="_l1", fz=dict(x_src=(xs, xsB), mixg=mix_src(1), sel=(sel, selB))))
    P.final_wait()
    P.flush()
    return C


_OFF = {}
_o = 0
for _n, _s in zip(("aq", "ak", "av", "gq", "gk", "gv", "gg", "glr", "mq", "mk", "mv", "mo", "mg"),
                  (512, 128, 128, 256, 256, 256, 256, 32, 256, 256, 256, 256, 16)):
    _OFF[_n] = (_o, _o + _s)
    _o += _s


def _consts(T):
    t = np.arange(T)
    row = (t // 64).astype(np.float32)
    col = (t % 64).astype(np.float32)
    inv = (np.float32(10000.0) ** (-np.arange(0, 32, 2, dtype=np.float32) / np.float32(32))).astype(np.float32)
    ar = row[:, None] * inv[None, :]
    ac = col[:, None] * inv[None, :]
    cr, sr, cc, sc = np.cos(ar), np.sin(ar), np.cos(ac), np.sin(ac)
    rope = np.concatenate([cr, cr, cc, cc, -sr, sr, -sc, sc], axis=1).astype(np.float32)
    s = np.arange(128)[:, None]
    u = np.arange(128)[None, :]
    same = (s // 64) == (u // 64)
    tri_f = (same & (s <= u)).astype(np.float32)
    tri_b = (same & (s >= u)).astype(np.float32)
    cstart = (np.arange(128) // 64) * 64
    mid_f = tri_f - tri_f[:, cstart + 31]
    mid_b = tri_b - tri_b[:, cstart + 32]
    last_f = (same & (s > u)).astype(np.float32)
    last_b = (same & (s < u)).astype(np.float32)
    cf = np.stack([tri_f, tri_b]).astype(np.float32)
    cs = (np.stack([tri_f, tri_b, mid_f, mid_b, last_f, last_b]) * np.float32(-1.0 / 16.0)).astype(np.float32)
    hm = np.stack([(np.arange(128) // 64) == 0, (np.arange(128) // 64) == 1], axis=1).astype(np.float32)
    return dict(rope=rope, cf=cf, cs=cs, hm=hm)


def k1_inputs(xb, W, li, hf, consts):
    w_in = W["w_in"][li]

    def cols(name, lo, hi):
        a, _ = _OFF[name]
        return w_in[:, a + lo:a + hi]
    ak = cols("ak", hf * 64, hf * 64 + 64)
    h0, h1 = 2 * hf, 2 * hf + 1
    mg = w_in[:, _OFF["mg"][0]:_OFF["mg"][1]]
    gidx = [0 + h0, 0 + h1, 8 + h0, 8 + h1, 4 + h0, 4 + h1, 12 + h0, 12 + h1]
    bi, bf = W["mlstm_b_input"][li], W["mlstm_b_forget"][li]
    gates_b = np.array([bi[0][h0], bi[0][h1], bi[1][h0], bi[1][h1], bf[0][h0], bf[0][h1], bf[1][h0], bf[1][h1]],
                       np.float32)
    sl = slice(hf * 128, hf * 128 + 128)
    wdb = np.zeros((2, 17, 128), np.float32)
    wdb[:, 0:16, :] = W["gla_w_decay"][li][:, :, sl]
    wdb[:, 16, :] = W["gla_b_decay"][li][:, sl]
    cwv, cbv = W["mlstm_conv_w"][li], W["mlstm_conv_b"][li]
    ch = np.concatenate([np.arange(hf * 128, hf * 128 + 128), 256 + np.arange(hf * 128, hf * 128 + 128)])
    cw = np.concatenate([cwv[:, ch].T, cbv[ch][:, None]], axis=1).astype(np.float32)
    qg, kg = W["attn_q_norm_g"][li], W["attn_k_norm_g"][li]
    d = dict(
        x=np.ascontiguousarray(xb), norm_mix_g=W["norm_mix_g"][li],
        w_att=np.ascontiguousarray(np.concatenate([cols("aq", hf * 256, hf * 256 + 256), ak, ak,
                                                   cols("av", hf * 64, hf * 64 + 64)], axis=1)),
        w_gla_f=np.ascontiguousarray(np.concatenate([cols("gq", sl.start, sl.stop), cols("gk", sl.start, sl.stop)], 1)),
        w_gla_t=np.ascontiguousarray(np.concatenate([cols("gk", sl.start, sl.stop), cols("gv", sl.start, sl.stop),
                                                     cols("gg", sl.start, sl.stop)], 1)),
        w_glr=np.ascontiguousarray(cols("glr", 0, 32)), wdb=wdb,
        w_ml_f=np.ascontiguousarray(np.concatenate([cols("mq", sl.start, sl.stop), cols("mk", sl.start, sl.stop)], 1)),
        w_ml_t=np.ascontiguousarray(np.concatenate([cols("mv", sl.start, sl.stop), cols("mo", sl.start, sl.stop),
                                                    mg[:, gidx]], 1)),
        cw=cw, gates_b=gates_b, g6=np.concatenate([qg, qg, qg, qg, kg, kg]).astype(np.float32),
        gla_g=np.tile(W["gla_out_norm_g"][li], 2).astype(np.float32),
        ml_g=np.tile(W["mlstm_out_norm_g"][li], 2).astype(np.float32),
    )
    d.update(consts)
    return d


_PROGS = {}


def _prog(key, fn):
    if key not in _PROGS:
        _PROGS[key] = fn()
    return _PROGS[key]


def _k2_inputs(W, li, last, fused):
    d = dict(
        norm_ffn_g=W["norm_ffn_g"][li],
        w_gr=np.ascontiguousarray(np.concatenate([W["w_group"][li], W["w_router"][li]], axis=1)),
        b_gr=np.concatenate([W["b_group"][li], W["b_router"][li]]),
        w_gate=W["w_expert_gate"][li], w_up=W["w_expert_up"][li], w_down=W["w_expert_down"][li],
        norm_ple_g=W["norm_ple_g"][li], w_ple_gate=W["w_ple_gate"][li], w_ple_proj=W["w_ple_proj"][li])
    if fused:
        perm = np.concatenate([np.concatenate([np.arange(r * 256, (r + 1) * 256), 512 + np.arange(r * 128, (r + 1) * 128),
                                               768 + np.arange(r * 128, (r + 1) * 128)]) for r in range(2)])
        d["w_out"] = np.ascontiguousarray(W["w_out"][li][perm])
    else:
        d["w_out"] = W["w_out"][li]
    if last:
        d["final_norm_g"] = W["final_norm_g"]
    return d


def kernel_unfused(**inputs):
    W = {k: np.asarray(v) for k, v in inputs.items()}
    x = np.ascontiguousarray(W["x"], dtype=np.float32)
    B, T, D = x.shape
    TH = T // 2
    consts = _consts(T)
    cores = list(range(8))
    for li in range(2):
        k1 = _prog(("k1", T), lambda: build_k1(T))
        in_maps = [k1_inputs(x[c // 2], W, li, c % 2, consts) for c in cores]
        res = run_bass_kernel_spmd(k1.nc, in_maps, core_ids=cores).results
        mixT = np.zeros((B, 1024, T), dtype=ml_dtypes.bfloat16)
        for c in cores:
            b, hf = c // 2, c % 2
            r = np.asarray(res[c]["mixT"])
            mixT[b, hf * 256:(hf + 1) * 256] = r[0:256]
            mixT[b, 512 + hf * 128:512 + (hf + 1) * 128] = r[256:384]
            mixT[b, 768 + hf * 128:768 + (hf + 1) * 128] = r[384:512]
        last = (li == 1)
        k2 = _prog(("k2", TH, last), lambda: build_k2(TH, last))
        in_maps = []
        for c in cores:
            b, half = c // 2, c % 2
            th = slice(half * TH, (half + 1) * TH)
            d = _k2_inputs(W, li, last, False)
            d.update(xh=np.ascontiguousarray(x[b, th]), mixT=np.ascontiguousarray(mixT[b][:, th]),
                     p=np.ascontiguousarray(W["p"][li, b, th]))
            in_maps.append(d)
        res = run_bass_kernel_spmd(k2.nc, in_maps, core_ids=cores).results
        xn = np.empty_like(x)
        for c in cores:
            b, half = c // 2, c % 2
            xn[b, half * TH:(half + 1) * TH] = np.asarray(res[c]["out"])
        x = xn
    return x.astype(np.float32)


def kernel(**inputs):
    W = {k: np.asarray(v) for k, v in inputs.items()}
    x = np.ascontiguousarray(W["x"], dtype=np.float32)
    B, T, D = x.shape
    TH = T // 2
    consts = _consts(T)
    cores = list(range(8))
    prog = _prog(("fused", T), lambda: build_fused(T))
    k2w = [_k2_inputs(W, li, li == 1, True) for li in range(2)]
    in_maps = []
    for c in cores:
        b, half = c // 2, c % 2
        th = slice(half * TH, (half + 1) * TH)
        d = {}
        for li in range(2):
            k1 = k1_inputs(x[b], W, li, half, consts)
            if li == 1:
                k1.pop("x")
                k1.pop("norm_mix_g")
            for k, v in k1.items():
                if k in consts:
                    d[k] = v
                else:
                    d[f"{k}_l{li}"] = v
            for k, v in k2w[li].items():
                d[f"{k}_l{li}"] = v
            d[f"p_l{li}"] = np.ascontiguousarray(W["p"][li, b, th])
        d["xh_l0"] = np.ascontiguousarray(x[b, th])
        d["norm_mix_g_next_l0"] = W["norm_mix_g"][1]
        d["sel"] = np.tile(np.array([[1.0 - half, float(half)]], np.float32), (128, 1))
        in_maps.append(d)
    res = run_bass_kernel_spmd(prog.nc, in_maps, core_ids=cores).results
    out = np.empty_like(x)
    for c in cores:
        b, half = c // 2, c % 2
        out[b, half * TH:(half + 1) * TH] = np.asarray(res[c]["out"])
    return out.astype(np.float32)
```

```python
import numpy as np
import ml_dtypes
from contextlib import ExitStack
import concourse.bass as bass
import concourse.mybir as mybir
from concourse.bass_utils import run_bass_kernel_spmd

F32 = mybir.dt.float32
BF16 = mybir.dt.bfloat16
AF = mybir.ActivationFunctionType
ALU = mybir.AluOpType
AX = mybir.AxisListType

EPS = 1e-6
EPOCH = 30000
_DBG = {}


class Buf:
    __slots__ = ("w", "rs", "name")

    def __init__(self, name=""):
        self.w = None
        self.rs = {}
        self.name = name


class _Q:
    def __init__(self, name):
        self.name = name
        self.ops = []
        self.n = 0
        self.known = {}
        self.shared = False
        self.maxep = {}


class Prog:
    ENG = ["pe", "act", "dve", "pool", "sp"]

    def __init__(self, nc, stack, n_dma_sems=24):
        self.nc = nc
        self.stack = stack
        self.q = {e: _Q(e) for e in self.ENG}
        self.esems = {e: [] for e in self.ENG}
        self.dsems = [stack.enter_context(nc.semaphore(f"dma{i}")) for i in range(n_dma_sems)]
        self.dcnt = [0] * n_dma_sems
        self.drr = 0
        self.snap = {}
        self.out_toks = []
        self.count = 0
        self.stop_at = None
        self.csems = []

    def _esem(self, e, epoch):
        while len(self.esems[e]) <= epoch:
            k = len(self.esems[e])
            self.esems[e].append(self.stack.enter_context(self.nc.semaphore(f"s_{e}{k}")))
        return self.esems[e][epoch]

    def _sem_of(self, key):
        if key[0] == "d":
            return self.dsems[key[1]] if key[1] < 1000 else self.csems[key[1] - 1000]
        return self._esem(key[0], key[1])

    def _is_known(self, q, key, val):
        if q.known.get(key, 0) >= val:
            return True
        if key[0] != "d" and q.maxep.get(key[0], -1) > key[1]:
            return True
        return False

    def _learn1(self, q, key, val):
        if q.known.get(key, 0) < val:
            if q.shared:
                q.known = dict(q.known)
                q.shared = False
            q.known[key] = val
        if key[0] != "d" and q.maxep.get(key[0], -1) < key[1]:
            q.maxep[key[0]] = key[1]

    def _wait(self, q, tok):
        key, val = tok
        if self._is_known(q, key, val):
            return
        sem = self._sem_of(key)
        q.ops.append(lambda eng, sem=sem, val=val: eng.wait_ge(sem, val))
        self._learn1(q, key, val)
        sn = self.snap.get(tok)
        if sn:
            for k, v in sn.items():
                self._learn1(q, k, v)

    def _deps(self, q, e, reads, writes):
        deps = {}
        for b in reads:
            if b.w is not None:
                deps[b.w] = 1
        for b in writes:
            if b.w is not None:
                deps[b.w] = 1
            for k, v in b.rs.items():
                deps[(k, v)] = 1
        for tok in deps:
            if e == "pe" and tok[0][0] == "pe":
                continue
            self._wait(q, tok)

    def _mark(self, tok, q, reads, writes):
        self.snap[tok] = q.known
        q.shared = True
        key, val = tok
        for b in reads:
            if b.rs.get(key, 0) < val:
                b.rs[key] = val
        for b in writes:
            b.w = tok
            b.rs = {}

    def op(self, e, fn, reads=(), writes=()):
        self.count += 1
        if self.stop_at is not None and self.count > self.stop_at:
            return None
        q = self.q[e]
        self._deps(q, e, reads, writes)
        epoch, cnt = divmod(q.n, EPOCH)
        cnt += 1
        q.n += 1
        sem = self._esem(e, epoch)
        q.ops.append(lambda eng, sem=sem, fn=fn: fn(eng).then_inc(sem, 1))
        tok = ((e, epoch), cnt)
        self._mark(tok, q, reads, writes)
        return tok

    def dma(self, e, out, in_, reads=(), writes=(), is_output=False, slow=False):
        self.count += 1
        if self.stop_at is not None and self.count > self.stop_at:
            return None
        q = self.q[e]
        i = self.drr
        self.drr = (self.drr + 1) % len(self.dsems)
        if self.dcnt[i] > 0:
            self._wait(q, (("d", i), self.dcnt[i]))
        self._deps(q, e, reads, writes)
        self.dcnt[i] += 16
        sem = self.dsems[i]
        kw = dict(allow_slow_non_contiguous=True) if slow else {}
        q.ops.append(lambda eng, sem=sem, out=out, in_=in_, kw=kw: eng.dma_start(out=out, in_=in_, **kw).then_inc(sem, 16))
        tok = (("d", i), self.dcnt[i])
        self._mark(tok, q, reads, writes)
        if is_output:
            self.out_toks.append(tok)
        return tok

    def collective(self, kind, in_ap, out_ap, groups, reads=(), writes=()):
        if _DBG.get("no_cc"):
            return None
        q = self.q["pool"]
        self._deps(q, "pool", reads, writes)
        k = len(self.csems)
        sem = self.stack.enter_context(self.nc.semaphore(f"cc{k}"))
        self.csems.append(sem)
        q.ops.append(lambda eng, sem=sem: eng.collective_compute(kind, ALU.bypass, replica_groups=groups,
                                                                 ins=[in_ap.opt()], outs=[out_ap.opt()]).then_inc(sem, 1))
        tok = (("d", 1000 + k), 1)
        self._mark(tok, q, reads, writes)
        return tok

    def barrier(self, bufs=()):
        toks = []
        for e in self.ENG:
            q = self.q[e]
            if q.n > 0:
                epoch, cnt = divmod(q.n - 1, EPOCH)
                toks.append(((e, epoch), cnt + 1))
        for i, c in enumerate(self.dcnt):
            if c > 0:
                toks.append((("d", i), c))
        for k in range(len(self.csems)):
            toks.append((("d", 1000 + k), 1))
        for e in self.ENG:
            for tok in toks:
                if tok[0][0] == e:
                    continue
                self._wait(self.q[e], tok)

    def final_wait(self):
        q = self.q["sp"]
        for tok in self.out_toks:
            self._wait(q, tok)

    def flush(self):
        qs = {e: list(self.q[e].ops) for e in self.ENG}
        for e in self.ENG:
            self.q[e].ops = []
        if not any(qs.values()):
            return
        with self.nc.Block() as block:
            @block.tensor
            def _(eng):
                for f in qs["pe"]:
                    f(eng)

            @block.scalar
            def _(eng):
                for f in qs["act"]:
                    f(eng)

            @block.vector
            def _(eng):
                for f in qs["dve"]:
                    f(eng)

            @block.gpsimd
            def _(eng):
                for f in qs["pool"]:
                    f(eng)

            @block.sync
            def _(eng):
                for f in qs["sp"]:
                    f(eng)


class Ctx:
    def __init__(self):
        self.nc = bass.Bass("TRN2", target_bir_lowering=False)
        self.stack = ExitStack()
        self.P = Prog(self.nc, self.stack)
        self._n = 0
        self.cur = self.stack

    def phase(self):
        C = self

        class _Ph:
            def __enter__(s2):
                s2.prev = C.cur
                s2.st = ExitStack()
                C.cur = s2.st
                return s2.st

            def __exit__(s2, *a):
                if a[0] is None:
                    C.P.barrier()
                    C.P.flush()
                C.cur = s2.prev
                s2.st.close()
                return False
        return _Ph()

    def sb(self, shape, dt, name=None, stack=None):
        self._n += 1
        t = (stack or self.cur).enter_context(self.nc.sbuf_tensor(f"{name or 't'}_{self._n}", list(shape), dt))
        return t

    def ps(self, shape, dt=F32, name=None):
        self._n += 1
        return self.cur.enter_context(self.nc.psum_tensor(f"{name or 'p'}_{self._n}", list(shape), dt))

    def din(self, name, shape, dt):
        if not hasattr(self, "_dins"):
            self._dins = {}
        if name not in self._dins:
            self._dins[name] = self.nc.dram_tensor(name, list(shape), dt, kind="ExternalInput").ap()
        return self._dins[name]

    def dout(self, name, shape, dt):
        return self.nc.dram_tensor(name, list(shape), dt, kind="ExternalOutput").ap()


def bc(ap, axis, shape):
    return ap.unsqueeze(axis).to_broadcast(list(shape))


def load_ident(C, dt=F32):
    P = C.P
    ident = C.sb([128, 128], dt, "ident")
    b = Buf("ident")
    P.op("pool", lambda g: g.memset(ident[:], 1.0), writes=[b])
    P.op("pool", lambda g: g.affine_select(out=ident[:], in_=ident[:], pattern=[[-1, 128]],
                                           compare_op=ALU.is_equal, fill=0.0, base=0, channel_multiplier=1),
         reads=[b], writes=[b])
    return ident, b


def rms_to_hT(C, x_sb, xb, NT, g32col, gb, ident, ib, hT, hTb, ps2, ps2b, fp32_cb=None, tag=""):
    P, nc = C.P, C.nc
    ss = C.sb([128, NT], F32, "ss" + tag)
    ssb = Buf("ss")
    r = C.sb([128, NT], F32, "r" + tag)
    rb = Buf("r")
    junk = C.sb([128, 1024], BF16, "junk" + tag)
    junkb = Buf("junk")
    P.op("pool", lambda g: g.memset(ss[:], 0.0), writes=[ssb])
    for i in range(NT):
        P.op("act", lambda a, i=i: a.activation(out=junk[:], in_=x_sb[:, i, :], func=AF.Square,
                                                 accum_out=ss[:, i:i + 1]),
             reads=[xb[i]], writes=[junkb, ssb])
    P.op("act", lambda a: a.activation(out=r[:], in_=ss[:], func=AF.Sqrt, scale=1.0 / 1024.0, bias=EPS),
         reads=[ssb], writes=[rb])
    P.op("dve", lambda v: v.reciprocal(out=r[:], in_=r[:]), reads=[rb], writes=[rb])
    xs = [C.sb([128, 1024], F32, f"xs{tag}{j}") for j in range(2)]
    xsb = [Buf("xs") for _ in range(2)]
    hTf = [C.sb([128, 8, 128], F32, f"hTf{tag}{j}") for j in range(2)]
    hTfb = [Buf("hTf") for _ in range(2)]
    for i in range(NT):
        j = i % 2
        P.op("act", lambda a, i=i, j=j: a.activation(out=xs[j][:], in_=x_sb[:, i, :], func=AF.Copy,
                                                      scale=r[:, i:i + 1]),
             reads=[xb[i], rb], writes=[xsb[j]])
        for kc in range(8):
            P.op("pe", lambda t, j=j, kc=kc: t.transpose(out=ps2[j][:, kc * 128:(kc + 1) * 128],
                                                         in_=xs[j][:, kc * 128:(kc + 1) * 128],
                                                         identity=ident[:]),
                 reads=[xsb[j], ib], writes=[ps2b[j]])
        if fp32_cb is not None:
            P.op("dve", lambda v, j=j: v.tensor_tensor(out=hTf[j][:], in0=ps2[j][:].rearrange("p (k t) -> p k t", k=8),
                                                        in1=bc(g32col[:], 2, [128, 8, 128]), op=ALU.mult),
                 reads=[ps2b[j], gb], writes=[hTfb[j]])
            P.op("pool", lambda g, i=i, j=j: g.tensor_copy(out=hT[:, :, i * 128:(i + 1) * 128], in_=hTf[j][:]),
                 reads=[hTfb[j]], writes=[hTb[i]])
            fp32_cb(i, hTf[j], hTfb[j])
        else:
            P.op("dve", lambda v, i=i, j=j: v.tensor_tensor(out=hT[:, :, i * 128:(i + 1) * 128],
                                                             in0=ps2[j][:].rearrange("p (k t) -> p k t", k=8),
                                                             in1=bc(g32col[:], 2, [128, 8, 128]), op=ALU.mult),
                 reads=[ps2b[j], gb], writes=[hTb[i]])


def load_gcol32(C, g_d, tag):
    P = C.P
    graw = C.sb([128, 8], F32, "graw" + tag)
    g32 = C.sb([128, 8], F32, "g32" + tag)
    b0, b1 = Buf(), Buf()
    P.dma("sp", graw[:], g_d.rearrange("(k p) -> p k", p=128), writes=[b0], slow=True)
    return graw, b0


def build_k2(T, last, C=None, sfx="", fz=None):
    standalone = C is None
    fz = fz or {}
    if C is None:
        C = Ctx()
    nc, P = C.nc, C.P
    NT = T // 128
    TB = min(512, T)
    NTB = T // TB
    SUB = TB // 128

    if "x_src" in fz:
        x_d, x_srcb = fz["x_src"]
        x_reads = [x_srcb]
    else:
        x_d = C.din("xh" + sfx, [T, 1024], F32)
        x_reads = []
    mixT_d = None if "mixg" in fz else C.din("mixT" + sfx, [1024, T], BF16)
    p_d = C.din("p" + sfx, [T, 256], F32)
    wout_d = C.din("w_out" + sfx, [1024, 1024], F32)
    gffn_d = C.din("norm_ffn_g" + sfx, [1024], F32)
    wr_d = C.din("w_gr" + sfx, [1024, 20], F32)
    br_d = C.din("b_gr" + sfx, [20], F32)
    wg_d = C.din("w_gate" + sfx, [16, 1024, 512], F32)
    wu_d = C.din("w_up" + sfx, [16, 1024, 512], F32)
    wd_d = C.din("w_down" + sfx, [16, 512, 1024], F32)
    gple_d = C.din("norm_ple_g" + sfx, [1024], F32)
    wpg_d = C.din("w_ple_gate" + sfx, [1024, 1024], F32)
    wpp_d = C.din("w_ple_proj" + sfx, [256, 1024], F32)
    gfin_d = C.din("final_norm_g" + sfx, [1024], F32) if last else None
    if "x_dst" in fz:
        out_d, out_dstb = fz["x_dst"]
        out_writes, out_is_output = [out_dstb], False
    else:
        out_d = C.dout("out", [T, 1024], F32)
        out_writes, out_is_output = [], True
    gnext_d = C.din("norm_mix_g_next" + sfx, [1024], F32) if "h_dst" in fz else None

    x_sb = C.sb([128, NT, 1024], F32, "x")
    xb = [Buf(f"x{i}") for i in range(NT)]
    ident, ib = load_ident(C)
    psA = [C.ps([128, 1024], F32, f"psA{j}") for j in range(2)]
    psAb = [Buf() for _ in range(2)]
    psB = [C.ps([128, 512], F32, f"psB{j}") for j in range(4)]
    psBb = [Buf() for _ in range(4)]

    x_v = x_d.rearrange("(n p) d -> p n d", p=128)
    for i0 in range(0, NT, 4):
        n = min(4, NT - i0)
        P.dma("sp", x_sb[:, i0:i0 + n, :], x_v[:, i0:i0 + n, :], reads=x_reads, writes=xb[i0:i0 + n])

    with C.phase() as st:
        mixT = C.sb([128, 8, T], BF16, "mixT", st)
        mb = Buf()
        wout = C.sb([128, 8, 1024], BF16, "wout", st)
        wb = Buf()
        if "mixg" in fz:
            mixg_rows = fz["mixg"]
            sel, selB = fz["sel"]
            cand = [[C.sb([128, T], BF16, f"cand{h}{s_}", st) for s_ in range(2)] for h in range(2)]
            candb = [[Buf() for _ in range(2)] for _ in range(2)]
            for kc in range(8):
                s_ = kc % 2
                for h in range(2):
                    P.dma("sp", cand[h][s_][:], mixg_rows[kc][0][:, h * T:(h + 1) * T], reads=[mixg_rows[kc][1]],
                          writes=[candb[h][s_]])
                P.op("dve", lambda v, kc=kc, s_=s_: v.tensor_scalar(out=mixT[:, kc, :], in0=cand[0][s_][:],
                                                                     scalar1=sel[:, 0:1], scalar2=None, op0=ALU.mult),
                     reads=[candb[0][s_], selB], writes=[mb])
                P.op("dve", lambda v, kc=kc, s_=s_: v.scalar_tensor_tensor(out=mixT[:, kc, :], in0=cand[1][s_][:],
                                                                            scalar=sel[:, 1:2], in1=mixT[:, kc, :],
                                                                            op0=ALU.mult, op1=ALU.add),
                     reads=[candb[1][s_], selB, mb], writes=[mb])
        else:
            P.dma("sp", mixT[:], mixT_d.rearrange("(k p) t -> p k t", p=128), writes=[mb])
        P.dma("pool", wout[:], wout_d.rearrange("(k p) n -> p k n", p=128), writes=[wb])
        for i in range(NT):
            for nh in range(2):
                j = (i * 2 + nh) % 4
                for kc in range(8):
                    P.op("pe", lambda t, i=i, nh=nh, kc=kc, j=j: t.matmul(
                        out=psB[j][:], lhsT=mixT[:, kc, i * 128:(i + 1) * 128],
                        rhs=wout[:, kc, nh * 512:(nh + 1) * 512], start=(kc == 0), stop=(kc == 7)),
                        reads=[mb, wb], writes=[psBb[j]])
                P.op("dve", lambda v, i=i, nh=nh, j=j: v.tensor_tensor(
                    out=x_sb[:, i, nh * 512:(nh + 1) * 512], in0=x_sb[:, i, nh * 512:(nh + 1) * 512],
                    in1=psB[j][:], op=ALU.add), reads=[psBb[j], xb[i]], writes=[xb[i]])

    with C.phase() as st:
        hT = C.sb([128, 8, T], BF16, "hT", st)
        hTb = [Buf() for _ in range(NT)]
        g32, g32b = load_gcol32(C, gffn_d, "ffn")
        wr = C.sb([128, 8, 20], F32, "wr", st)
        wrb = Buf()
        P.dma("sp", wr[:], wr_d.rearrange("(k p) n -> p k n", p=128), writes=[wrb])
        brb_t = C.sb([128, 20], F32, "brb", st)
        brb = Buf()
        P.dma("sp", brb_t[:], br_d.partition_broadcast(128), writes=[brb])
        L = C.sb([128, NT, 20], F32, "logits", st)
        Lb = Buf()
        NS = 2
        wgs = [C.sb([128, 8, 512], BF16, f"wg{s}", st) for s in range(NS)]
        wus = [C.sb([128, 8, 512], BF16, f"wu{s}", st) for s in range(NS)]
        wds = [C.sb([128, 4, 1024], BF16, f"wd{s}", st) for s in range(NS)]
        wgb = [Buf() for _ in range(NS)]
        wub = [Buf() for _ in range(NS)]
        wdb = [Buf() for _ in range(NS)]
        def load_expert(e):
            s = e % NS
            P.dma("pool", wgs[s][:], wg_d[e].rearrange("(k p) n -> p k n", p=128), writes=[wgb[s]])
            P.dma("pool", wus[s][:], wu_d[e].rearrange("(k p) n -> p k n", p=128), writes=[wub[s]])
            P.dma("pool", wds[s][:], wd_d[e].rearrange("(k p) n -> p k n", p=128), writes=[wdb[s]])

        load_expert(0)

        def router_cb(i, hTf, hTfb):
            j = 2 + (i % 2)
            for kc in range(8):
                P.op("pe", lambda t, kc=kc, j=j: t.matmul(out=psB[j][:, 0:20], lhsT=hTf[:, kc, :],
                                                          rhs=wr[:, kc, :], start=(kc == 0), stop=(kc == 7)),
                     reads=[hTfb, wrb], writes=[psBb[j]])
            P.op("dve", lambda v, i=i, j=j: v.tensor_tensor(out=L[:, i, :], in0=psB[j][:, 0:20], in1=brb_t[:],
                                                             op=ALU.add), reads=[psBb[j], brb], writes=[Lb])

        rms_to_hT(C, x_sb, xb, NT, g32, g32b, ident, ib, hT, hTb, psA, psAb, fp32_cb=router_cb, tag="f")

        def S(shape, nm):
            return C.sb(shape, F32, nm, st), Buf(nm)
        gmax, gmaxb = S([128, NT], "gmax")
        gm, gmb = S([128, NT, 4], "gm")
        ge, geb = S([128, NT, 4], "ge")
        gsum, gsumb = S([128, NT], "gsum")
        gprob, gprobb = S([128, NT], "gprob")
        tmp4, tmp4b = S([128, NT, 4, 4], "tmp4")
        els, elsb = S([128, NT, 4], "els")
        m1, m1b = S([128, NT], "m1")
        mk1, mk1b = S([128, NT, 4], "mk1")
        el2, el2b = S([128, NT, 4], "el2")
        m2, m2b = S([128, NT], "m2")
        mk2, mk2b = S([128, NT, 4], "mk2")
        dd, ddb = S([128, NT], "dd")
        e2, e2b = S([128, NT], "e2")
        w1, w1b = S([128, NT], "w1")
        w2, w2b = S([128, NT], "w2")
        ws, wsb = S([128, NT, 4], "ws")
        ws2, ws2b = S([128, NT, 4], "ws2")
        comb, combb = S([128, NT, 4, 4], "comb")
        gl = L[:, :, 0:4]
        el = L[:, :, 4:20].rearrange("p n (g e) -> p n g e", g=4)
        V = lambda fn, r, w: P.op("dve", fn, reads=r, writes=w)
        V(lambda v: v.tensor_reduce(out=gmax[:], in_=gl, axis=AX.X, op=ALU.max), [Lb], [gmaxb])
        V(lambda v: v.tensor_tensor(out=gm[:], in0=gl, in1=bc(gmax[:], 2, [128, NT, 4]), op=ALU.is_equal),
          [Lb, gmaxb], [gmb])
        V(lambda v: v.tensor_tensor(out=ge[:], in0=gl, in1=bc(gmax[:], 2, [128, NT, 4]), op=ALU.subtract),
          [Lb, gmaxb], [geb])
        P.op("act", lambda a: a.activation(out=ge[:], in_=ge[:], func=AF.Exp), reads=[geb], writes=[geb])
        V(lambda v: v.tensor_reduce(out=gsum[:], in_=ge[:], axis=AX.X, op=ALU.add), [geb], [gsumb])
        V(lambda v: v.reciprocal(out=gprob[:], in_=gsum[:]), [gsumb], [gprobb])
        V(lambda v: v.tensor_tensor(out=tmp4[:], in0=el, in1=bc(gm[:], 3, [128, NT, 4, 4]), op=ALU.mult),
          [Lb, gmb], [tmp4b])
        V(lambda v: v.tensor_reduce(out=els[:], in_=tmp4[:].rearrange("p n g e -> p n e g"), axis=AX.X, op=ALU.add),
          [tmp4b], [elsb])
        V(lambda v: v.tensor_reduce(out=m1[:], in_=els[:], axis=AX.X, op=ALU.max), [elsb], [m1b])
        V(lambda v: v.tensor_tensor(out=mk1[:], in0=els[:], in1=bc(m1[:], 2, [128, NT, 4]), op=ALU.is_equal),
          [elsb, m1b], [mk1b])
        V(lambda v: v.scalar_tensor_tensor(out=el2[:].rearrange("p n e -> p (n e)"),
                                           in0=mk1[:].rearrange("p n e -> p (n e)"), scalar=-1e30,
                                           in1=els[:].rearrange("p n e -> p (n e)"), op0=ALU.mult, op1=ALU.add),
          [mk1b, elsb], [el2b])
        V(lambda v: v.tensor_reduce(out=m2[:], in_=el2[:], axis=AX.X, op=ALU.max), [el2b], [m2b])
        V(lambda v: v.tensor_tensor(out=mk2[:], in0=el2[:], in1=bc(m2[:], 2, [128, NT, 4]), op=ALU.is_equal),
          [el2b, m2b], [mk2b])
        V(lambda v: v.tensor_tensor(out=dd[:], in0=m2[:], in1=m1[:], op=ALU.subtract), [m1b, m2b], [ddb])
        P.op("act", lambda a: a.activation(out=e2[:], in_=dd[:], func=AF.Exp), reads=[ddb], writes=[e2b])
        V(lambda v: v.tensor_scalar(out=dd[:], in0=e2[:], scalar1=1.0, scalar2=None, op0=ALU.add), [e2b], [ddb])
        V(lambda v: v.reciprocal(out=w1[:], in_=dd[:]), [ddb], [w1b])
        V(lambda v: v.tensor_tensor(out=w1[:], in0=w1[:], in1=gprob[:], op=ALU.mult), [w1b, gprobb], [w1b])
        V(lambda v: v.tensor_tensor(out=w2[:], in0=w1[:], in1=e2[:], op=ALU.mult), [w1b, e2b], [w2b])
        V(lambda v: v.tensor_tensor(out=ws[:], in0=mk1[:], in1=bc(w1[:], 2, [128, NT, 4]), op=ALU.mult),
          [mk1b, w1b], [wsb])
        V(lambda v: v.tensor_tensor(out=ws2[:], in0=mk2[:], in1=bc(w2[:], 2, [128, NT, 4]), op=ALU.mult),
          [mk2b, w2b], [ws2b])
        V(lambda v: v.tensor_tensor(out=ws[:], in0=ws[:], in1=ws2[:], op=ALU.add), [wsb, ws2b], [wsb])
        V(lambda v: v.tensor_tensor(out=comb[:], in0=bc(gm[:], 3, [128, NT, 4, 4]),
                                    in1=bc(ws[:], 2, [128, NT, 4, 4]), op=ALU.mult), [gmb, wsb], [combb])
        combf = comb[:].rearrange("p n g e -> p n (g e)")

        aT = [C.sb([128, 4, TB], BF16, f"aT{s}", st) for s in range(2)]
        aTb = [Buf() for _ in range(2)]
        sg = [C.sb([128, TB], BF16, f"sg{s}", st) for s in range(2)]
        sgb = [Buf() for _ in range(2)]
        gu = [(psA[0], 0), (psA[0], 512), (psA[1], 0), (psA[1], 512)]
        gub = [Buf() for _ in range(4)]

        dcnt_ = [0]

        def moe_step(e, tb, a, s):
            if True:
                if True:
                    pass
                tsl = slice(tb * TB, (tb + 1) * TB)
                for c in range(4):
                    gq = (c % 2) * 2
                    (gt, go), (ut, uo) = gu[gq], gu[gq + 1]
                    for kc in range(8):
                        P.op("pe", lambda t, kc=kc, c=c, gt=gt, go=go, s=s, tsl=tsl: t.matmul(
                            out=gt[:, go:go + TB], lhsT=wgs[s][:, kc, c * 128:(c + 1) * 128], rhs=hT[:, kc, tsl],
                            start=(kc == 0), stop=(kc == 7)), reads=[wgb[s]] + hTb[tb * SUB:(tb + 1) * SUB],
                            writes=[gub[gq]])
                    for kc in range(8):
                        P.op("pe", lambda t, kc=kc, c=c, ut=ut, uo=uo, s=s, tsl=tsl: t.matmul(
                            out=ut[:, uo:uo + TB], lhsT=wus[s][:, kc, c * 128:(c + 1) * 128], rhs=hT[:, kc, tsl],
                            start=(kc == 0), stop=(kc == 7)), reads=[wub[s]] + hTb[tb * SUB:(tb + 1) * SUB],
                            writes=[gub[gq + 1]])
                    sj = c % 2
                    P.op("act", lambda ac, gt=gt, go=go, sj=sj: ac.activation(out=sg[sj][:], in_=gt[:, go:go + TB],
                                                                              func=AF.Silu),
                         reads=[gub[gq]], writes=[sgb[sj]])
                    P.op("dve", lambda v, ut=ut, uo=uo, sj=sj, a=a, c=c: v.tensor_tensor(
                        out=aT[a][:, c, :], in0=sg[sj][:], in1=ut[:, uo:uo + TB], op=ALU.mult),
                        reads=[sgb[sj], gub[gq + 1]], writes=[aTb[a]])
                yield
                for ts in range(SUB):
                    i = tb * SUB + ts
                    for nh in range(2):
                        j = dcnt_[0] % 4
                        dcnt_[0] += 1
                        for c in range(4):
                            P.op("pe", lambda t, c=c, j=j, a=a, ts=ts, nh=nh, s=s: t.matmul(
                                out=psB[j][:], lhsT=aT[a][:, c, ts * 128:(ts + 1) * 128],
                                rhs=wds[s][:, c, nh * 512:(nh + 1) * 512], start=(c == 0), stop=(c == 3)),
                                reads=[aTb[a], wdb[s]], writes=[psBb[j]])
                        P.op("dve", lambda v, i=i, nh=nh, j=j, e=e: v.scalar_tensor_tensor(
                            out=x_sb[:, i, nh * 512:(nh + 1) * 512], in0=psB[j][:], scalar=combf[:, i, e:e + 1],
                            in1=x_sb[:, i, nh * 512:(nh + 1) * 512], op0=ALU.mult, op1=ALU.add),
                            reads=[psBb[j], combb, xb[i]], writes=[xb[i]])

        msteps = [(e, tb) for e in range(16) for tb in range(NTB)]
        pend = None
        for k_, (e, tb) in enumerate(msteps):
            g_ = moe_step(e, tb, k_ % 2, e % NS)
            next(g_)
            if pend is not None:
                for _ in pend:
                    pass
            pend = g_
            if tb == 0 and e + 1 < 16:
                load_expert(e + 1)
        for _ in pend:
            pass

    with C.phase() as st:
        hT = C.sb([128, 8, T], BF16, "h2T", st)
        hTb = [Buf() for _ in range(NT)]
        g32, g32b = load_gcol32(C, gple_d, "ple")
        wpg = C.sb([128, 8, 1024], BF16, "wpg", st)
        wpgb = Buf()
        wpp = C.sb([128, 2, 1024], BF16, "wpp", st)
        wppb = Buf()
        P.dma("pool", wpg[:], wpg_d.rearrange("(k p) n -> p k n", p=128), writes=[wpgb])
        P.dma("pool", wpp[:], wpp_d.rearrange("(k p) n -> p k n", p=128), writes=[wppb])
        p_sb = C.sb([128, NT, 256], F32, "p", st)
        pb = Buf()
        P.dma("sp", p_sb[:], p_d.rearrange("(n p) d -> p n d", p=128), writes=[pb])
        pT = C.sb([128, 2, T], BF16, "pT", st)
        pTb = [Buf() for _ in range(NT)]
        rms_to_hT(C, x_sb, xb, NT, g32, g32b, ident, ib, hT, hTb, psA, psAb, tag="p")
        for i in range(NT):
            j = i % 2
            for kc in range(2):
                P.op("pe", lambda t, i=i, kc=kc, j=j: t.transpose(out=psB[j][:, kc * 128:(kc + 1) * 128],
                                                                  in_=p_sb[:, i, kc * 128:(kc + 1) * 128],
                                                                  identity=ident[:]),
                     reads=[pb, ib], writes=[psBb[j]])
            P.op("act", lambda a, i=i, j=j: a.activation(out=pT[:, :, i * 128:(i + 1) * 128],
                                                          in_=psB[j][:, 0:256].rearrange("p (k t) -> p k t", k=2),
                                                          func=AF.Copy), reads=[psBb[j]], writes=[pTb[i]])
        sgp = [C.sb([128, 512], F32, f"sgp{s}", st) for s in range(2)]
        sgpb = [Buf() for _ in range(2)]
        gfin = C.sb([128, 1024], F32, "gfin", st)
        gfinb = Buf()
        if last:
            P.dma("sp", gfin[:], gfin_d.partition_broadcast(128), writes=[gfinb])
        ssf = C.sb([128, NT], F32, "ssf", st)
        rf = C.sb([128, NT], F32, "rf", st)
        junkf = C.sb([128, 1024], BF16, "junkf", st)
        junkfb = Buf()
        ssfb = [Buf() for _ in range(NT)]
        rfb = [Buf() for _ in range(NT)]
        ob = [C.sb([128, 1024], F32, f"ob{s}", st) for s in range(2)]
        obb = [Buf() for _ in range(2)]
        out_v = out_d.rearrange("(n p) d -> p n d", p=128)
        cnt = 0
        for i in range(NT):
            for nh in range(2):
                jg = (cnt % 2)
                jp = 2 + (cnt % 2)
                sj = cnt % 2
                cnt += 1
                for kc in range(8):
                    P.op("pe", lambda t, i=i, kc=kc, nh=nh, jg=jg: t.matmul(
                        out=psB[jg][:], lhsT=hT[:, kc, i * 128:(i + 1) * 128],
                        rhs=wpg[:, kc, nh * 512:(nh + 1) * 512], start=(kc == 0), stop=(kc == 7)),
                        reads=[hTb[i], wpgb], writes=[psBb[jg]])
                for kc in range(2):
                    P.op("pe", lambda t, i=i, kc=kc, nh=nh, jp=jp: t.matmul(
                        out=psB[jp][:], lhsT=pT[:, kc, i * 128:(i + 1) * 128],
                        rhs=wpp[:, kc, nh * 512:(nh + 1) * 512], start=(kc == 0), stop=(kc == 1)),
                        reads=[pTb[i], wppb], writes=[psBb[jp]])
                P.op("act", lambda a, jg=jg, sj=sj: a.activation(out=sgp[sj][:], in_=psB[jg][:], func=AF.Sigmoid),
                     reads=[psBb[jg]], writes=[sgpb[sj]])
                P.op("dve", lambda v, jp=jp, sj=sj: v.tensor_tensor(out=sgp[sj][:], in0=sgp[sj][:], in1=psB[jp][:],
                                                                     op=ALU.mult),
                     reads=[sgpb[sj], psBb[jp]], writes=[sgpb[sj]])
                P.op("dve", lambda v, i=i, nh=nh, sj=sj: v.tensor_tensor(
                    out=x_sb[:, i, nh * 512:(nh + 1) * 512], in0=x_sb[:, i, nh * 512:(nh + 1) * 512],
                    in1=sgp[sj][:], op=ALU.add), reads=[sgpb[sj], xb[i]], writes=[xb[i]])
            if last:
                o = i % 2
                P.op("pool", lambda g, i=i: g.memset(ssf[:, i:i + 1], 0.0), writes=[ssfb[i]])
                P.op("act", lambda a, i=i: a.activation(out=junkf[:], in_=x_sb[:, i, :], func=AF.Square,
                                                         accum_out=ssf[:, i:i + 1]),
                     reads=[xb[i]], writes=[junkfb, ssfb[i]])
                P.op("act", lambda a, i=i: a.activation(out=rf[:, i:i + 1], in_=ssf[:, i:i + 1], func=AF.Sqrt,
                                                         scale=1.0 / 1024.0, bias=EPS),
                     reads=[ssfb[i]], writes=[rfb[i]])
                P.op("dve", lambda v, i=i: v.reciprocal(out=rf[:, i:i + 1], in_=rf[:, i:i + 1]), reads=[rfb[i]],
                     writes=[rfb[i]])
                P.op("dve", lambda v, i=i, o=o: v.scalar_tensor_tensor(
                    out=ob[o][:], in0=x_sb[:, i, :], scalar=rf[:, i:i + 1], in1=gfin[:], op0=ALU.mult,
                    op1=ALU.mult), reads=[xb[i], rfb[i], gfinb], writes=[obb[o]])
                P.dma("sp", out_v[:, i, :], ob[o][:], reads=[obb[o]], is_output=True)
            else:
                P.dma("sp", out_v[:, i, :], x_sb[:, i, :], reads=[xb[i]], writes=out_writes, is_output=out_is_output)
        if standalone:
            P.final_wait()
    if "h_dst" in fz:
        h_rows = fz["h_dst"]
        with C.phase() as st:
            hTn = C.sb([128, 8, T], BF16, "hTn", st)
            hTnb = [Buf() for _ in range(NT)]
            gn, gnb = load_gcol32(C, gnext_d, "nxt")
            rms_to_hT(C, x_sb, xb, NT, gn, gnb, ident, ib, hTn, hTnb, psA, psAb, tag="n")
            for k0 in range(0, 8, 2):
                hap, hB_ = h_rows[k0 // 2]
                P.dma("sp", hap.rearrange("(k p) t -> p k t", p=128), hTn[:, k0:k0 + 2, :], reads=hTnb, writes=[hB_])
    if standalone:
        P.flush()
    return C


def finalize_heads(C, hacc, haccb, gt, gtb, NT, identb, ibb, pbank, outT, outTb, tag):
    P = C.P
    ssq = C.sb([128, NT * 2], F32, "fssq" + tag)
    ssqb = Buf()
    on = C.sb([128, NT, 128], BF16, "fon" + tag)
    onb = Buf()
    P.op("dve", lambda v: v.tensor_tensor(out=on[:], in0=hacc[:], in1=hacc[:], op=ALU.mult), reads=haccb, writes=[onb])
    P.op("dve", lambda v: v.tensor_reduce(out=ssq[:], in_=on[:].rearrange("p n (h e) -> p (n h) e", h=2), axis=AX.X,
                                          op=ALU.add), reads=[onb], writes=[ssqb])
    P.op("act", lambda a: a.activation(out=ssq[:], in_=ssq[:], func=AF.Sqrt, scale=1.0 / 64.0, bias=EPS),
         reads=[ssqb], writes=[ssqb])
    P.op("dve", lambda v: v.reciprocal(out=ssq[:], in_=ssq[:]), reads=[ssqb], writes=[ssqb])
    P.op("dve", lambda v: v.tensor_tensor(out=hacc[:].rearrange("p n (h e) -> p (n h) e", h=2),
                                          in0=hacc[:].rearrange("p n (h e) -> p (n h) e", h=2),
                                          in1=bc(ssq[:], 2, [128, NT * 2, 64]), op=ALU.mult),
         reads=haccb + [ssqb], writes=haccb)
    P.op("dve", lambda v: v.tensor_tensor(out=on[:], in0=hacc[:], in1=gt[:], op=ALU.mult), reads=haccb + gtb,
         writes=[onb])
    GRP = min(4, NT)
    pb1 = Buf()
    for g0 in range(0, NT, GRP):
        for k in range(GRP):
            P.op("pe", lambda t, g0=g0, k=k: t.transpose(out=pbank[:, k * 128:(k + 1) * 128], in_=on[:, g0 + k, :],
                                                         identity=identb[:]),
                 reads=[onb, ibb], writes=[pb1])
        ob_ = Buf()
        outTb.append(ob_)
        P.op("dve", lambda a, g0=g0: a.tensor_copy(out=outT[:, g0 * 128:(g0 + GRP) * 128], in_=pbank[:, 0:GRP * 128]),
             reads=[pb1], writes=[ob_])


def build_k1(T, phases="AGM", stop_at=None, C=None, sfx="", hsrc=None, mix_dst=None, after_A=None):
    standalone = C is None
    if C is None:
        C = Ctx()
        C.P.stop_at = stop_at
    nc, P = C.nc, C.P
    NT = T // 128
    NCH = T // 64
    TB = min(512, T)
    NTB = T // TB
    SUB = TB // 128

    if hsrc is None:
        x_d = C.din("x" + sfx, [T, 1024], F32)
        gmix_d = C.din("norm_mix_g" + sfx, [1024], F32)
    watt_d = C.din("w_att" + sfx, [1024, 448], F32)
    wglaf_d = C.din("w_gla_f" + sfx, [1024, 256], F32)
    wglat_d = C.din("w_gla_t" + sfx, [1024, 384], F32)
    wglr_d = C.din("w_glr" + sfx, [1024, 32], F32)
    wdb_d = C.din("wdb" + sfx, [2, 17, 128], F32)
    wmlf_d = C.din("w_ml_f" + sfx, [1024, 256], F32)
    wmlt_d = C.din("w_ml_t" + sfx, [1024, 264], F32)
    cw_d = C.din("cw" + sfx, [256, 4], F32)
    gatesb_d = C.din("gates_b" + sfx, [8], F32)
    g6_d = C.din("g6" + sfx, [384], F32)
    glag_d = C.din("gla_g" + sfx, [128], F32)
    mlg_d = C.din("ml_g" + sfx, [128], F32)
    rope_d = C.din("rope", [T, 128], F32)
    cf_d = C.din("cf", [2, 128, 128], F32)
    cs_d = C.din("cs", [6, 128, 128], F32)
    hm_d = C.din("hm", [128, 2], F32)
    if mix_dst is None:
        mixT_d = C.dout("mixT", [512, T], BF16)
        mix_rows = [(mixT_d[k * 128:(k + 1) * 128, :], []) for k in range(4)]
        mix_is_output = True
    else:
        mix_rows = [(ap_, [b_]) for ap_, b_ in mix_dst]
        mix_is_output = False

    hT = C.sb([128, 8, T], BF16, "hT")
    hTb = [Buf() for _ in range(NT)]
    ident, ib = load_ident(C)
    identb, ibb = load_ident(C, BF16)
    cf = C.sb([128, 2, 128], F32, "cf")
    cfb = Buf()
    P.dma("sp", cf[:], cf_d.rearrange("c p n -> p c n"), writes=[cfb])
    ones = C.sb([128, 128], F32, "ones")
    onesb = Buf()
    P.op("pool", lambda g: g.memset(ones[:], 1.0), writes=[onesb])

    def hT_blk(tb):
        return hTb[tb * SUB:(tb + 1) * SUB]

    if hsrc is not None:
        T2_ = T // 2
        for r_ in range(2):
            for k0 in range(0, 8, 2):
                hap, hgB = hsrc[(r_, k0)]
                P.dma("sp", hT[:, k0:k0 + 2, r_ * T2_:(r_ + 1) * T2_], hap.rearrange("(k p) t -> p k t", p=128),
                      reads=[hgB], writes=hTb[r_ * (NT // 2):(r_ + 1) * (NT // 2)])
    for _once in ([] if hsrc is not None else [0]):
      with C.phase():
        PS = [C.sb, None]
        ps2 = [C.ps([128, 1024], F32, f"p0_{j}") for j in range(2)]
        ps2b = [Buf() for _ in range(2)]
        g, gb = load_gcol32(C, gmix_d, "mix")
        xg = [C.sb([128, 4, 1024], F32, f"xg{s}") for s in range(2)]
        xgb = [[Buf() for _ in range(4)] for _ in range(2)]
        ss = C.sb([128, NT], F32, "ss")
        r = C.sb([128, NT], F32, "r")
        ssb = [Buf() for _ in range(NT)]
        rb = [Buf() for _ in range(NT)]
        junk = C.sb([128, 1024], BF16, "junk")
        junkb = Buf()
        xs = [C.sb([128, 1024], F32, f"xs{j}") for j in range(2)]
        xsb = [Buf() for _ in range(2)]
        x_v = x_d.rearrange("(n p) d -> p n d", p=128)
        P.op("pool", lambda g_: g_.memset(ss[:], 0.0), writes=ssb)
        def p0_tile(i, j):
            gi, k = divmod(i, 4)
            s = gi % 2
            if k == 0:
                n = min(4, NT - i)
                P.dma("sp", xg[s][:, 0:n, :], x_v[:, i:i + n, :], writes=xgb[s][0:n])
            P.op("act", lambda a, i=i, s=s, k=k: a.activation(out=junk[:], in_=xg[s][:, k, :], func=AF.Square,
                                                              accum_out=ss[:, i:i + 1]),
                 reads=[xgb[s][k]], writes=[junkb, ssb[i]])
            yield
            P.op("act", lambda a, i=i: a.activation(out=r[:, i:i + 1], in_=ss[:, i:i + 1], func=AF.Sqrt,
                                                     scale=1.0 / 1024.0, bias=EPS), reads=[ssb[i]], writes=[rb[i]])
            yield
            P.op("dve", lambda v, i=i: v.reciprocal(out=r[:, i:i + 1], in_=r[:, i:i + 1]), reads=[rb[i]],
                 writes=[rb[i]])
            yield
            P.op("act", lambda a, i=i, s=s, k=k, j=j: a.activation(out=xs[j][:], in_=xg[s][:, k, :], func=AF.Copy,
                                                                   scale=r[:, i:i + 1]),
                 reads=[xgb[s][k], rb[i]], writes=[xsb[j]])
            yield
            for kc in range(8):
                P.op("pe", lambda t, j=j, kc=kc: t.transpose(out=ps2[j][:, kc * 128:(kc + 1) * 128],
                                                             in_=xs[j][:, kc * 128:(kc + 1) * 128],
                                                             identity=ident[:]),
                     reads=[xsb[j], ib], writes=[ps2b[j]])
            yield
            P.op("dve", lambda v, i=i, j=j: v.tensor_tensor(out=hT[:, :, i * 128:(i + 1) * 128],
                                                             in0=ps2[j][:].rearrange("p (k t) -> p k t", k=8),
                                                             in1=bc(g[:], 2, [128, 8, 128]), op=ALU.mult),
                 reads=[ps2b[j], gb], writes=[hTb[i]])
        for i0 in range(0, NT, 2):
            gens = [p0_tile(i0 + k_, k_) for k_ in range(min(2, NT - i0))]
            live = [True] * len(gens)
            while any(live):
                for k_ in range(len(gens)):
                    if live[k_]:
                        try:
                            next(gens[k_])
                        except StopIteration:
                            live[k_] = False

    def psum_banks(n, dt=F32, cols=512):
        ts = [C.cur.enter_context(nc.psum_tensor(f"pb{C._n}_{k}", [128, cols], dt)) for k in range(n)]
        C._n += 1
        return ts, [Buf() for _ in range(n)]

    if "A" in phases:
        with C.phase():
            zps, zpsb = psum_banks(2)
            tps, tpsb = psum_banks(1, BF16, 1024)
            sps, spsb = psum_banks(3)
            ops_, opsb = psum_banks(2)
            watt = C.sb([128, 8, 448], BF16, "watt")
            wattb = Buf()
            P.dma("pool", watt[:], watt_d.rearrange("(k p) n -> p k n", p=128), writes=[wattb])
            rope = C.sb([128, NT, 128], F32, "rope")
            ropeb = Buf()
            P.dma("sp", rope[:], rope_d.rearrange("(n p) c -> p n c", p=128), writes=[ropeb])
            g6 = C.sb([128, 384], F32, "g6")
            g6b = Buf()
            P.dma("sp", g6[:], g6_d.partition_broadcast(128), writes=[g6b])
            qkT = C.sb([128, 3, T], BF16, "qkT")
            qkTb = [Buf() for _ in range(NT)]
            va0 = C.sb([128, NT, 65], BF16, "va0")
            va1 = C.sb([128, NT, 128], BF16, "va1")
            vab = [Buf() for _ in range(NT)]
            mixA = C.sb([128, 2, T], BF16, "mixA")
            mixAb = Buf()
            P.op("pool", lambda g_: g_.memset(va0[:], 1.0), writes=vab)
            P.op("pool", lambda g_: g_.memset(va1[:], 0.0), writes=vab)
            P.op("pool", lambda g_: g_.memset(va1[:, :, 0:1], 1.0), writes=vab)
            amx = C.sb([128, 2], F32, "amx")
            amxb = Buf()
            nb = C.sb([128, 1], F32, "nb")
            nbb = Buf()
            P.op("dve", lambda v: v.tensor_reduce(out=amx[:, 0:1], in_=g6[:, 0:64], axis=AX.X, op=ALU.max,
                                                  apply_absolute_value=True), reads=[g6b], writes=[amxb])
            P.op("dve", lambda v: v.tensor_reduce(out=amx[:, 1:2], in_=g6[:, 256:320], axis=AX.X, op=ALU.max,
                                                  apply_absolute_value=True), reads=[g6b, amxb], writes=[amxb])
            P.op("dve", lambda v: v.tensor_tensor(out=nb[:], in0=amx[:, 0:1], in1=amx[:, 1:2], op=ALU.mult),
                 reads=[amxb], writes=[nbb])
            P.op("dve", lambda v: v.tensor_scalar(out=nb[:], in0=nb[:], scalar1=-8.0, scalar2=None, op0=ALU.mult),
                 reads=[nbb], writes=[nbb])

            def T2(nm, shape, dt=F32):
                return [C.sb(shape, dt, f"{nm}{j}") for j in range(2)], [Buf() for _ in range(2)]
            zsb_, zsbb = T2("zsb", [128, 384])
            sq_, sqb_ = T2("asq", [128, 384])
            ssq_, ssqb_ = T2("assq", [128, 6])
            qn_, qnb_ = T2("qn", [128, 384])
            t1_, t1b_ = T2("t1", [128, 384])
            t2_, t2b_ = T2("t2", [128, 384])
            qr_, qrb_ = T2("qr", [128, 384], BF16)
            def a_tile(i, j):
                for kc in range(8):
                    P.op("pe", lambda t, i=i, kc=kc, j=j: t.matmul(out=zps[j][:, 0:448], lhsT=hT[:, kc, i * 128:(i + 1) * 128],
                                                                  rhs=watt[:, kc, :], start=(kc == 0), stop=(kc == 7)),
                         reads=[hTb[i], wattb], writes=[zpsb[j]])
                yield
                P.op("act", lambda a, j=j: a.activation(out=zsb_[j][:], in_=zps[j][:, 0:384], func=AF.Copy),
                     reads=[zpsb[j]], writes=[zsbb[j]])
                yield
                P.op("act", lambda a, i=i, j=j: a.activation(out=va0[:, i, 0:64], in_=zps[j][:, 384:448], func=AF.Copy),
                     reads=[zpsb[j]], writes=[vab[i]])
                yield
                P.op("act", lambda a, i=i, j=j: a.activation(out=va1[:, i, 64:128], in_=zps[j][:, 384:448], func=AF.Copy),
                     reads=[zpsb[j]], writes=[vab[i]])
                yield
                P.op("dve", lambda v, j=j: v.tensor_tensor(out=sq_[j][:], in0=zsb_[j][:], in1=zsb_[j][:], op=ALU.mult),
                     reads=[zsbb[j]], writes=[sqb_[j]])
                yield
                P.op("dve", lambda v, j=j: v.tensor_reduce(out=ssq_[j][:], in_=sq_[j][:].rearrange("p (h e) -> p h e", h=6),
                                                            axis=AX.X, op=ALU.add), reads=[sqb_[j]], writes=[ssqb_[j]])
                yield
                P.op("act", lambda a, j=j: a.activation(out=ssq_[j][:], in_=ssq_[j][:], func=AF.Sqrt, scale=1.0 / 64.0,
                                                         bias=EPS), reads=[ssqb_[j]], writes=[ssqb_[j]])
                yield
                P.op("dve", lambda v, j=j: v.reciprocal(out=ssq_[j][:], in_=ssq_[j][:]), reads=[ssqb_[j]],
                     writes=[ssqb_[j]])
                yield
                P.op("dve", lambda v, j=j: v.tensor_tensor(out=qn_[j][:].rearrange("p (h e) -> p h e", h=6),
                                                            in0=zsb_[j][:].rearrange("p (h e) -> p h e", h=6),
                                                            in1=bc(ssq_[j][:], 2, [128, 6, 64]), op=ALU.mult),
                     reads=[zsbb[j], ssqb_[j]], writes=[qnb_[j]])
                yield
                P.op("dve", lambda v, j=j: v.tensor_tensor(out=qn_[j][:], in0=qn_[j][:], in1=g6[:], op=ALU.mult),
                     reads=[qnb_[j], g6b], writes=[qnb_[j]])
                yield
                P.op("dve", lambda v, i=i, j=j: v.tensor_tensor(out=t1_[j][:].rearrange("p (h e) -> p h e", h=6),
                                                                 in0=qn_[j][:].rearrange("p (h e) -> p h e", h=6),
                                                                 in1=bc(rope[:, i, 0:64], 1, [128, 6, 64]), op=ALU.mult),
                     reads=[qnb_[j], ropeb], writes=[t1b_[j]])
                yield
                for w in range(2):
                    P.op("dve", lambda v, i=i, j=j, w=w: v.tensor_tensor(
                        out=t2_[j][:].rearrange("p (h a w e) -> p h a w e", h=6, a=2, w=2)[:, :, :, w, :],
                        in0=qn_[j][:].rearrange("p (h a w e) -> p h a w e", h=6, a=2, w=2)[:, :, :, 1 - w, :],
                        in1=bc(rope[:, i, 64:128].rearrange("p (a w e) -> p a w e", a=2, w=2)[:, :, w, :], 1,
                               [128, 6, 2, 16]), op=ALU.mult),
                        reads=[qnb_[j], ropeb], writes=[t2b_[j]])
                yield
                P.op("dve", lambda v, j=j: v.tensor_tensor(out=qr_[j][:], in0=t1_[j][:], in1=t2_[j][:], op=ALU.add),
                     reads=[t1b_[j], t2b_[j]], writes=[qrb_[j]])
                yield
                for k in range(3):
                    P.op("pe", lambda t, j=j, k=k: t.transpose(out=tps[0][:, k * 128:(k + 1) * 128],
                                                               in_=qr_[j][:, k * 128:(k + 1) * 128], identity=identb[:]),
                         reads=[qrb_[j], ibb], writes=[tpsb[0]])
                P.op("act", lambda a, i=i: a.activation(out=qkT[:, :, i * 128:(i + 1) * 128],
                                                         in_=tps[0][:, 0:384].rearrange("p (k t) -> p k t", k=3),
                                                         func=AF.Copy), reads=[tpsb[0]], writes=[qkTb[i]])

            for i0 in range(0, NT, 2):
                gens = [a_tile(i0 + k_, k_) for k_ in range(min(2, NT - i0))]
                live = [True] * len(gens)
                while any(live):
                    for k_ in range(len(gens)):
                        if live[k_]:
                            try:
                                next(gens[k_])
                            except StopIteration:
                                live[k_] = False

            pe_ = [C.sb([128, TB], BF16, f"pexp{s}") for s in range(3)]
            peb = [Buf() for _ in range(3)]
            denr = C.sb([128, TB], F32, "denr")
            denrb = Buf()
            bcs = C.sb([128, TB], F32, "bcs")
            bcsb = Buf()
            pe_.append(C.sb([128, TB], BF16, "pexp3"))
            peb.append(Buf())
            slots = [(sps[0], spsb[0]), (sps[1], spsb[1]), (sps[2], spsb[2]), (zps[0], zpsb[0])]
            steps = [(pr, qb, kt) for pr in range(2) for qb in range(NTB) for kt in range(NT)]

            def emit_S(j):
                pr, qb, kt = steps[j]
                qsl = slice(qb * TB, (qb + 1) * TB)
                for hh in range(2):
                    s = (2 * j + hh) % 4
                    rows = slice(hh * 64, (hh + 1) * 64)
                    P.op("pe", lambda t, s=s, rows=rows: t.matmul(out=slots[s][0][:, 0:TB],
                                                                  lhsT=qkT[rows, 2, kt * 128:(kt + 1) * 128],
                                                                  rhs=qkT[rows, pr, qsl], start=True, stop=True),
                         reads=[qkTb[kt]] + qkTb[qb * SUB:(qb + 1) * SUB], writes=[slots[s][1]])
                for hh in range(2):
                    s = (2 * j + hh) % 4
                    P.op("act", lambda a, s=s: a.activation(out=pe_[s][:], in_=slots[s][0][:, 0:TB], func=AF.Exp,
                                                            bias=nb[:, 0:1], scale=0.125),
                         reads=[slots[s][1], nbb], writes=[peb[s]])

            def emit_O(j):
                pr, qb, kt = steps[j]
                qsl = slice(qb * TB, (qb + 1) * TB)
                s0, s1 = (2 * j) % 4, (2 * j + 1) % 4
                P.op("pe", lambda t: t.matmul(out=ops_[0][0:65, 0:TB], lhsT=va0[:, kt, :], rhs=pe_[s0][:],
                                              start=(kt == 0), stop=(kt == NT - 1)),
                     reads=[vab[kt], peb[s0]], writes=[opsb[0]])
                P.op("pe", lambda t: t.matmul(out=ops_[1][:, 0:TB], lhsT=va1[:, kt, :], rhs=pe_[s1][:],
                                              start=(kt == 0), stop=(kt == NT - 1)),
                     reads=[vab[kt], peb[s1]], writes=[opsb[1]])
                if kt == NT - 1:
                    for h2 in range(2):
                        fin(pr, qsl, h2)

            def fin(pr, qsl, hh):
                dr = slice(64, 65) if hh == 0 else slice(0, 1)
                rows = slice(hh * 64, (hh + 1) * 64)
                P.op("dve", lambda v: v.reciprocal(out=denr[dr, :], in_=ops_[hh][dr, 0:TB]),
                     reads=[opsb[hh]], writes=[denrb])
                if hh == 0:
                    P.op("pe", lambda t: t.matmul(out=zps[1][0:64, 0:TB], lhsT=ones[dr, 0:64], rhs=denr[dr, :],
                                                  start=True, stop=True), reads=[denrb, onesb], writes=[zpsb[1]])
                else:
                    P.op("pe", lambda t: t.matmul(out=zps[1][:, 0:TB], lhsT=ones[dr, :], rhs=denr[dr, :],
                                                  start=True, stop=True), reads=[denrb, onesb], writes=[zpsb[1]])
                P.op("dve", lambda v: v.tensor_copy(out=bcs[rows, :], in_=zps[1][rows, 0:TB]),
                     reads=[zpsb[1]], writes=[bcsb])
                P.op("dve", lambda v: v.tensor_tensor(out=mixA[rows, pr, qsl], in0=ops_[hh][rows, 0:TB],
                                                      in1=bcs[rows, :], op=ALU.mult),
                     reads=[opsb[hh], bcsb], writes=[mixAb])

            for j in range(len(steps) + 1):
                if j < len(steps):
                    emit_S(j)
                if j >= 1:
                    emit_O(j - 1)
            for pr in range(2):
                P.dma("sp", mix_rows[pr][0], mixA[:, pr, :], reads=[mixAb], writes=mix_rows[pr][1], is_output=mix_is_output)

    if after_A is not None:
        after_A()
    if "G" in phases:
        with C.phase():
            pz, pzb = psum_banks(2)
            pc, pcb = psum_banks(2)
            pa, pab = psum_banks(2)
            C._n += 1
            po_ctx = [nc.psum_tensor(f"po{C._n}_{k}", [128, 512], F32) for k in range(2)]
            po = [c_.__enter__() for c_ in po_ctx]
            cs = C.sb([128, 6, 128], BF16, "cs")
            csb = Buf()
            P.dma("pool", cs[:], cs_d.rearrange("c p n -> p c n"), writes=[csb])
            wf = C.sb([128, 8, 256], BF16, "wglaf")
            wfb = Buf()
            P.dma("pool", wf[:], wglaf_d.rearrange("(k p) n -> p k n", p=128), writes=[wfb])
            wt = C.sb([128, 8, 384], BF16, "wglat")
            wtb = Buf()
            P.dma("pool", wt[:], wglat_d.rearrange("(k p) n -> p k n", p=128), writes=[wtb])
            wl = C.sb([128, 8, 32], BF16, "wglr")
            wlb = Buf()
            P.dma("pool", wl[:], wglr_d.rearrange("(k p) n -> p k n", p=128), writes=[wlb])
            wdb = C.sb([17, 2, 128], F32, "wdb")
            wdbb = Buf()
            P.dma("sp", wdb[:], wdb_d.rearrange("d r n -> r d n"), writes=[wdbb])
            gg_ = C.sb([128, 128], F32, "glag")
            ggb = Buf()
            P.dma("sp", gg_[:], glag_d.partition_broadcast(128), writes=[ggb])
            qgT = C.sb([128, T], BF16, "qgT")
            kgT = C.sb([128, T], BF16, "kgT")
            qgTb = [Buf() for _ in range(NTB)]
            kgTb = [Buf() for _ in range(NTB)]
            ktm = C.sb([128, NT, 128], BF16, "ktm")
            vtm = C.sb([128, NT, 128], BF16, "vtm")
            gate = C.sb([128, NT, 128], F32, "ggate")
            tmb = [Buf() for _ in range(NT)]
            la = [C.sb([128, NT, 128], BF16, f"la{d}") for d in range(2)]
            lab = [[Buf() for _ in range(NT)] for _ in range(2)]
            lrT = [C.sb([17, TB], F32, f"lrT{d}") for d in range(2)]
            lrTb = [Buf() for _ in range(2)]
            oacc = C.sb([128, NT, 128], F32, "oacc")
            oaccb = [Buf() for _ in range(NT)]
            mixG = C.sb([128, T], BF16, "mixG")
            mixGb = []
            for d in range(2):
                P.op("pool", lambda g_, d=d: g_.memset(lrT[d][:], 1.0), writes=[lrTb[d]])
            etmp = C.sb([128, 128], F32, "etmp")
            etmpb = Buf()
            for tb in range(NTB):
                tsl = slice(tb * TB, (tb + 1) * TB)
                for qk in range(2):
                    j = qk
                    for kc in range(8):
                        P.op("pe", lambda t, kc=kc, qk=qk, j=j, tsl=tsl: t.matmul(
                            out=pz[j][:, 0:TB], lhsT=wf[:, kc, qk * 128:(qk + 1) * 128], rhs=hT[:, kc, tsl],
                            start=(kc == 0), stop=(kc == 7)), reads=[wfb] + hT_blk(tb), writes=[pzb[j]])
                    if qk == 0:
                        P.op("act", lambda a, j=j, tsl=tsl: a.activation(out=qgT[:, tsl], in_=pz[j][:, 0:TB], func=AF.Copy,
                                                                         scale=0.125), reads=[pzb[j]], writes=[qgTb[tb]])
                    else:
                        P.op("act", lambda a, j=j, tsl=tsl: a.activation(out=kgT[:, tsl], in_=pz[j][:, 0:TB], func=AF.Copy),
                             reads=[pzb[j]], writes=[kgTb[tb]])
                for d in range(2):
                    j = d
                    for kc in range(8):
                        P.op("pe", lambda t, kc=kc, d=d, j=j, tsl=tsl: t.matmul(
                            out=pc[j][0:16, 0:TB], lhsT=wl[:, kc, d * 16:(d + 1) * 16], rhs=hT[:, kc, tsl],
                            start=(kc == 0), stop=(kc == 7)), reads=[wlb] + hT_blk(tb), writes=[pcb[j]])
                    P.op("dve", lambda v, d=d, j=j: v.tensor_copy(out=lrT[d][0:16, :], in_=pc[j][0:16, 0:TB]),
                         reads=[pcb[j]], writes=[lrTb[d]])
                for ts in range(SUB):
                    i = tb * SUB + ts
                    j = i % 2
                    for kc in range(8):
                        P.op("pe", lambda t, i=i, kc=kc, j=j: t.matmul(out=pz[j][:, 0:384], lhsT=hT[:, kc, i * 128:(i + 1) * 128],
                                                                      rhs=wt[:, kc, :], start=(kc == 0), stop=(kc == 7)),
                             reads=[hTb[i], wtb], writes=[pzb[j]])
                    P.op("act", lambda a, i=i, j=j: a.activation(out=ktm[:, i, :], in_=pz[j][:, 0:128], func=AF.Copy),
                         reads=[pzb[j]], writes=[tmb[i]])
                    P.op("act", lambda a, i=i, j=j: a.activation(out=vtm[:, i, :], in_=pz[j][:, 128:256], func=AF.Copy),
                         reads=[pzb[j]], writes=[tmb[i]])
                    P.op("act", lambda a, i=i, j=j: a.activation(out=gate[:, i, :], in_=pz[j][:, 256:384], func=AF.Silu),
                         reads=[pzb[j]], writes=[tmb[i]])
                    P.op("dve", lambda v, i=i: v.tensor_tensor(out=gate[:, i, :], in0=gate[:, i, :], in1=gg_[:], op=ALU.mult),
                         reads=[tmb[i], ggb], writes=[tmb[i]])
                    for d in range(2):
                        jj = d
                        P.op("pe", lambda t, d=d, jj=jj, ts=ts: t.matmul(out=pa[jj][:, 0:128],
                                                                        lhsT=lrT[d][0:17, ts * 128:(ts + 1) * 128],
                                                                        rhs=wdb[0:17, d, :], start=True, stop=True),
                             reads=[lrTb[d], wdbb], writes=[pab[jj]])
                        P.op("act", lambda a, jj=jj: a.activation(out=etmp[:], in_=pa[jj][:, 0:128], func=AF.Exp, scale=-1.0),
                             reads=[pab[jj]], writes=[etmpb])
                        P.op("act", lambda a, d=d, i=i: a.activation(out=la[d][:, i, :], in_=etmp[:], func=AF.Ln, bias=1.0),
                             reads=[etmpb], writes=[lab[d][i]])

            def G2(nm, shape, dt):
                return [C.sb(shape, dt, f"{nm}{j}") for j in range(2)], [Buf() for _ in range(2)]
            ebm_, ebmb = G2("ebm", [128, 128], F32)
            enbm_, enbmb = G2("enbm", [128, 128], F32)
            eb_, ebb = G2("eb", [128, 128], F32)
            ebl_, eblb = G2("ebl", [128, 128], F32)
            qd_, qdb = G2("qd", [128, 128], BF16)
            kd_, kdb = G2("kd", [128, 128], BF16)
            qbz_, qbzb = G2("qbz", [128, 2, 128], BF16)
            kl_, klb = G2("kl", [128, 128], BF16)
            at_, atb = G2("at", [128, 2, 128], BF16)
            for j in range(2):
                P.op("pool", lambda g_, j=j: g_.memset(qbz_[j][:], 0.0), writes=[qbzb[j]])
            S32d = [C.sb([128, 128], F32, f"S32_{d}") for d in range(2)]
            S32db = [Buf() for _ in range(2)]
            Sbfd = [[C.sb([128, 128], BF16, f"Sbf{d}_{j}") for j in range(2)] for d in range(2)]
            Sbfdb = [[Buf() for _ in range(2)] for _ in range(2)]
            for d in range(2):
                P.op("pool", lambda g_, d=d: g_.memset(S32d[d][:], 0.0), writes=[S32db[d]])
                for j in range(2):
                    P.op("pool", lambda g_, d=d, j=j: g_.memset(Sbfd[d][j][:], 0.0), writes=[Sbfdb[d][j]])
            it = 0
            scurd = [0, 0]
            seen = set()
            order = []
            for st_ in range(NT):
                order += [(0, st_), (1, NT - 1 - st_)]
            def g_body(d, i, j):
                chunks = (0, 1) if d == 0 else (1, 0)
                for _one in (0,):
                    tb = i // SUB
                    tsl = slice(i * 128, (i + 1) * 128)
                    P.op("pe", lambda t, d=d, i=i, j=j: t.matmul(out=pc[j][:, 0:128], lhsT=la[d][:, i, :], rhs=cs[:, 2 + d, :],
                                                                start=True, stop=True), reads=[lab[d][i], csb], writes=[pcb[j]])
                    P.op("pe", lambda t, d=d, i=i, j=j: t.matmul(out=pc[j][:, 128:256], lhsT=la[d][:, i, :], rhs=cs[:, 0 + d, :],
                                                                start=True, stop=True), reads=[lab[d][i], csb], writes=[pcb[j]])
                    P.op("pe", lambda t, d=d, i=i, j=j: t.matmul(out=pc[j][:, 256:384], lhsT=cs[:, 4 + d, :], rhs=la[d][:, i, :],
                                                                start=True, stop=True), reads=[lab[d][i], csb], writes=[pcb[j]])
                    yield
                    P.op("act", lambda a, j=j: a.activation(out=ebm_[j][:], in_=pc[j][:, 0:128], func=AF.Exp), reads=[pcb[j]],
                         writes=[ebmb[j]])
                    P.op("act", lambda a, j=j: a.activation(out=enbm_[j][:], in_=pc[j][:, 0:128], func=AF.Exp, scale=-1.0),
                         reads=[pcb[j]], writes=[enbmb[j]])
                    P.op("act", lambda a, j=j: a.activation(out=eb_[j][:], in_=pc[j][:, 128:256], func=AF.Exp), reads=[pcb[j]],
                         writes=[ebb[j]])
                    P.op("act", lambda a, j=j: a.activation(out=ebl_[j][:], in_=pc[j][:, 256:384], func=AF.Exp), reads=[pcb[j]],
                         writes=[eblb[j]])
                    yield
                    P.op("dve", lambda v, j=j, tsl=tsl: v.tensor_tensor(out=qd_[j][:], in0=qgT[:, tsl], in1=ebm_[j][:], op=ALU.mult),
                         reads=[qgTb[tb], ebmb[j]], writes=[qdb[j]])
                    P.op("dve", lambda v, j=j, tsl=tsl: v.tensor_tensor(out=kd_[j][:], in0=kgT[:, tsl], in1=enbm_[j][:], op=ALU.mult),
                         reads=[kgTb[tb], enbmb[j]], writes=[kdb[j]])
                    for c in range(2):
                        P.op("dve", lambda v, j=j, c=c, i=i: v.tensor_tensor(
                            out=qbz_[j][:, c, c * 64:(c + 1) * 64], in0=qgT[:, i * 128 + c * 64:i * 128 + (c + 1) * 64],
                            in1=eb_[j][:, c * 64:(c + 1) * 64], op=ALU.mult), reads=[qgTb[tb], ebb[j]], writes=[qbzb[j]])
                    P.op("dve", lambda v, j=j, i=i: v.tensor_tensor(out=kl_[j][:], in0=ktm[:, i, :], in1=ebl_[j][:], op=ALU.mult),
                         reads=[tmb[i], eblb[j]], writes=[klb[j]])
                    yield
                    S32, S32b, Sbf, Sbfb, scur = S32d[d], S32db[d], Sbfd[d], Sbfdb[d], scurd[d]
                    rb_ = [(pa[j], pab[j]), (pz[j], pzb[j])]
                    for h in range(2):
                        rows = slice(h * 64, (h + 1) * 64)
                        P.op("pe", lambda t, j=j, h=h, rows=rows, rb_=rb_: t.matmul(out=rb_[h][0][:, 0:128], lhsT=kd_[j][rows, :],
                                                                           rhs=qd_[j][rows, :], start=True, stop=True),
                             reads=[kdb[j], qdb[j]], writes=[rb_[h][1]])
                        P.op("dve", lambda v, j=j, d=d, h=h, rb_=rb_: v.tensor_tensor(out=at_[j][:, h, :], in0=rb_[h][0][:, 0:128],
                                                                         in1=cf[:, d, :], op=ALU.mult),
                             reads=[rb_[h][1], cfb], writes=[atb[j]])
                    for c in range(2):
                        crow = slice(c * 64, (c + 1) * 64)
                        P.op("pe", lambda t, j=j, c=c, crow=crow, i=i, rb_=rb_: t.matmul(out=rb_[c][0][:, 128:256],
                                                                                lhsT=kl_[j][crow, :], rhs=vtm[crow, i, :],
                                                                                start=True, stop=True),
                             reads=[klb[j], tmb[i]], writes=[rb_[c][1]])
                    yield
                    first = True
                    for c in chunks:
                        P.op("pe", lambda t, j=j, c=c, scur=scur, first=first, Sbf=Sbf: t.matmul(
                            out=po[j][:, 0:128], lhsT=qbz_[j][:, c, :], rhs=Sbf[scur][:], start=first, stop=False),
                            reads=[qbzb[j], Sbfb[scur]], writes=[poB[j]])
                        yield
                        first = False
                        dcol = (c * 64 + 63) if d == 0 else (c * 64)
                        for h in range(2):
                            rows = slice(h * 64, (h + 1) * 64)
                            P.op("dve", lambda v, j=j, c=c, h=h, rows=rows, dcol=dcol, rb_=rb_, S32=S32: v.scalar_tensor_tensor(
                                out=S32[rows, h * 64:(h + 1) * 64], in0=S32[rows, h * 64:(h + 1) * 64],
                                scalar=eb_[j][rows, dcol:dcol + 1],
                                in1=rb_[c][0][rows, 128 + h * 64:128 + (h + 1) * 64], op0=ALU.mult, op1=ALU.add),
                                reads=[S32b, ebb[j], rb_[c][1]], writes=[S32b])
                        yield
                        scur = 1 - scur
                        P.op("pool", lambda g_, scur=scur, Sbf=Sbf, S32=S32: g_.tensor_copy(out=Sbf[scur][:], in_=S32[:]),
                             reads=[S32b], writes=[Sbfb[scur]])
                        yield
                    for h in range(2):
                        P.op("pe", lambda t, j=j, h=h, i=i: t.matmul(out=po[j][:, h * 64:(h + 1) * 64], lhsT=at_[j][:, h, :],
                                                                     rhs=vtm[:, i, h * 64:(h + 1) * 64], start=False, stop=(h == 1)),
                             reads=[atb[j], tmb[i]], writes=[poB[j]])
                    yield
                    if i not in seen:
                        seen.add(i)
                        P.op("act", lambda a, i=i, j=j: a.activation(out=oacc[:, i, :], in_=po[j][:, 0:128], func=AF.Copy),
                             reads=[poB[j]], writes=[oaccb[i]])
                    else:
                        P.op("dve", lambda v, i=i, j=j: v.tensor_tensor(out=oacc[:, i, :], in0=oacc[:, i, :], in1=po[j][:, 0:128],
                                                                    op=ALU.add), reads=[poB[j], oaccb[i]], writes=[oaccb[i]])
                    scurd[d] = scur

            poB = [Buf(), Buf()]
            for p_ in range(NT):
                gens = [g_body(order[2 * p_][0], order[2 * p_][1], 0), g_body(order[2 * p_ + 1][0], order[2 * p_ + 1][1], 1)]
                live = [True, True]
                while any(live):
                    for k_ in range(2):
                        if live[k_]:
                            try:
                                next(gens[k_])
                            except StopIteration:
                                live[k_] = False
            P.barrier()
            P.flush()
            for c_ in reversed(po_ctx):
                c_.__exit__(None, None, None)
            pt, ptb = psum_banks(1, BF16, 1024)
            finalize_heads(C, oacc, oaccb, gate, tmb, NT, identb, ibb, pt[0], mixG, mixGb, "g")
            P.dma("sp", mix_rows[2][0], mixG[:], reads=mixGb, writes=mix_rows[2][1], is_output=mix_is_output)

    if "M" in phases:
        with C.phase():
            pz, pzb = psum_banks(2)
            prp, prpb = psum_banks(1)
            pst, pstb = psum_banks(2)
            pk, pkb = psum_banks(1)
            C._n += 1
            pt_ctx0 = nc.psum_tensor(f"ptm{C._n}", [128, 1024], BF16)
            pt = [pt_ctx0.__enter__()]
            wmf = C.sb([128, 8, 256], BF16, "wmlf")
            wmfb = Buf()
            P.dma("pool", wmf[:], wmlf_d.rearrange("(k p) n -> p k n", p=128), writes=[wmfb])
            wmt = C.sb([128, 8, 264], BF16, "wmlt")
            wmtb = Buf()
            P.dma("pool", wmt[:], wmlt_d.rearrange("(k p) n -> p k n", p=128), writes=[wmtb])
            cw = C.sb([128, 2, 4], F32, "cw")
            cwb = Buf()
            P.dma("sp", cw[:], cw_d.rearrange("(a p) c -> p a c", p=128), writes=[cwb])
            gb8 = C.sb([128, 8], F32, "gb8")
            gb8b = Buf()
            P.dma("sp", gb8[:], gatesb_d.partition_broadcast(128), writes=[gb8b])
            mlg = C.sb([128, 128], F32, "mlg")
            mlgb = Buf()
            P.dma("sp", mlg[:], mlg_d.partition_broadcast(128), writes=[mlgb])
            hm = C.sb([128, 2], F32, "hm")
            hmb = Buf()
            P.dma("sp", hm[:], hm_d, writes=[hmb])
            mqT = C.sb([128, T], BF16, "mqT")
            mkT = C.sb([128, T], BF16, "mkT")
            mqTb, mkTb = Buf(), Buf()
            vaug = C.sb([128, NT, 2, 65], BF16, "mvaug")
            smo = C.sb([128, NT, 128], F32, "smo")
            gts = C.sb([128, NT, 8], F32, "gts")
            tmb = [Buf() for _ in range(NT)]
            P.op("pool", lambda g_: g_.memset(vaug[:], 1.0), writes=tmb)
            with C.phase():
                raw = C.sb([128, T + 2], F32, "raw")
                rawb = Buf()
                y = C.sb([128, T], F32, "convy")
                yb = Buf()
                for qk in range(2):
                    P.op("pool", lambda g_: g_.memset(raw[:, 0:1], 0.0), writes=[rawb])
                    P.op("pool", lambda g_: g_.memset(raw[:, T + 1:T + 2], 0.0), writes=[rawb])
                    for tb in range(NTB):
                        j = tb % 2
                        tsl = slice(tb * TB, (tb + 1) * TB)
                        for kc in range(8):
                            P.op("pe", lambda t, kc=kc, qk=qk, j=j, tsl=tsl: t.matmul(
                                out=pz[j][:, 0:TB], lhsT=wmf[:, kc, qk * 128:(qk + 1) * 128], rhs=hT[:, kc, tsl],
                                start=(kc == 0), stop=(kc == 7)), reads=[wmfb] + hT_blk(tb), writes=[pzb[j]])
                        P.op("act", lambda a, j=j, tb=tb: a.activation(out=raw[:, 1 + tb * TB:1 + (tb + 1) * TB],
                                                                       in_=pz[j][:, 0:TB], func=AF.Copy),
                             reads=[pzb[j]], writes=[rawb])
                    P.op("dve", lambda v, qk=qk: v.tensor_scalar(out=y[:], in0=raw[:, 0:T], scalar1=cw[:, qk, 0:1],
                                                                  scalar2=cw[:, qk, 3:4], op0=ALU.mult, op1=ALU.add),
                         reads=[rawb, cwb], writes=[yb])
                    P.op("dve", lambda v, qk=qk: v.scalar_tensor_tensor(out=y[:], in0=raw[:, 1:T + 1], scalar=cw[:, qk, 1:2],
                                                                         in1=y[:], op0=ALU.mult, op1=ALU.add),
                         reads=[rawb, cwb, yb], writes=[yb])
                    P.op("dve", lambda v, qk=qk: v.scalar_tensor_tensor(out=y[:], in0=raw[:, 2:T + 2], scalar=cw[:, qk, 2:3],
                                                                         in1=y[:], op0=ALU.mult, op1=ALU.add),
                         reads=[rawb, cwb, yb], writes=[yb])
                    if qk == 0:
                        P.op("act", lambda a: a.activation(out=mqT[:], in_=y[:], func=AF.Silu), reads=[yb], writes=[mqTb])
                    else:
                        P.op("act", lambda a: a.activation(out=y[:], in_=y[:], func=AF.Silu), reads=[yb], writes=[yb])
                        P.op("pool", lambda g_: g_.tensor_scalar(out=mkT[:], in0=y[:], scalar1=0.125, scalar2=None,
                                                                 op0=ALU.mult), reads=[yb], writes=[mkTb])
            for i in range(NT):
                j = i % 2
                for kc in range(8):
                    P.op("pe", lambda t, i=i, kc=kc, j=j: t.matmul(out=pz[j][:, 0:264], lhsT=hT[:, kc, i * 128:(i + 1) * 128],
                                                                  rhs=wmt[:, kc, :], start=(kc == 0), stop=(kc == 7)),
                         reads=[hTb[i], wmtb], writes=[pzb[j]])
                P.op("act", lambda a, i=i, j=j: a.activation(out=vaug[:, i, :, 0:64],
                                                              in_=pz[j][:, 0:128].rearrange("p (h e) -> p h e", h=2),
                                                              func=AF.Copy), reads=[pzb[j]], writes=[tmb[i]])
                P.op("act", lambda a, i=i, j=j: a.activation(out=smo[:, i, :], in_=pz[j][:, 128:256], func=AF.Sigmoid),
                     reads=[pzb[j]], writes=[tmb[i]])
                P.op("dve", lambda v, i=i: v.tensor_tensor(out=smo[:, i, :], in0=smo[:, i, :], in1=mlg[:], op=ALU.mult),
                     reads=[tmb[i], mlgb], writes=[tmb[i]])
                P.op("dve", lambda v, i=i, j=j: v.tensor_tensor(out=gts[:, i, :], in0=pz[j][:, 256:264], in1=gb8[:], op=ALU.add),
                     reads=[pzb[j], gb8b], writes=[tmb[i]])

            def S(shape, nm, dt=F32):
                return C.sb(shape, dt, nm), Buf(nm)
            lf, lfb = S([128, NT, 4], "lf")
            cums, cumsb = S([128, NT, 4], "cums")
            aa, aab = S([128, NT, 4], "aa")
            xcat, xcatb = S([128, NT, 8], "xcat")
            xm, xmb = S([128, NT, 2, 8], "xm")
            LSE, LSEb = S([128, NCH, 4], "LSE")
            bl, blb = S([128, NCH, 4], "bl")
            ein, einb = S([128, NCH + 1, 4], "ein")
            einB = [Buf(), Buf()]
            tmx = [C.sb([128, 2], F32, f"tmx{d}") for d in range(2)]
            tmxb = [Buf(), Buf()]
            Mp, Mpb = S([128, NCH, 4], "Mp")
            wpv, wpvb = S([128, NCH, 4], "wpv")
            Mtm, Mtmb = S([128, NT, 4], "Mtm")
            wptm, wptmb = S([128, NT, 4], "wptm")
            es, esb = S([128, NT, 4], "es")
            fden, fdenb = S([128, NT, 4], "fden")
            P.op("act", lambda a: a.activation(out=lf[:], in_=gts[:, :, 4:8], func=AF.Exp, scale=-1.0), reads=tmb, writes=[lfb])
            P.op("act", lambda a: a.activation(out=lf[:], in_=lf[:], func=AF.Ln, bias=1.0), reads=[lfb], writes=[lfb])
            for d in range(2):
                P.op("pe", lambda t, d=d: t.matmul(out=prp[0][:, d * NT * 2:(d + 1) * NT * 2], lhsT=cf[:, d, :],
                                                   rhs=lf[:, :, d * 2:(d + 1) * 2], start=True, stop=True),
                     reads=[cfb, lfb], writes=[prpb[0]])
            for d in range(2):
                P.op("dve", lambda v, d=d: v.tensor_copy(out=cums[:, :, d * 2:(d + 1) * 2],
                                                         in_=prp[0][:, d * NT * 2:(d + 1) * NT * 2].rearrange("p (n s) -> p n s", s=2)),
                     reads=[prpb[0]], writes=[cumsb])
            P.op("dve", lambda v: v.tensor_tensor(out=aa[:], in0=gts[:, :, 0:4], in1=cums[:], op=ALU.add), reads=tmb + [cumsb],
                 writes=[aab])
            P.op("act", lambda a: a.activation(out=xcat[:, :, 0:4], in_=aa[:], func=AF.Exp), reads=[aab], writes=[xcatb])
            P.op("dve", lambda g_: g_.tensor_copy(out=xcat[:, :, 4:8], in_=lf[:]), reads=[lfb, xcatb], writes=[xcatb])
            for jh in range(2):
                P.op("dve", lambda v, jh=jh: v.tensor_scalar(out=xm[:, :, jh, :], in0=xcat[:], scalar1=hm[:, jh:jh + 1],
                                                              scalar2=None, op0=ALU.mult), reads=[xcatb, hmb], writes=[xmb])
            P.op("pe", lambda t: t.matmul(out=prp[0][:, 0:NT * 16], lhsT=ones[:],
                                          rhs=xm[:].rearrange("p n j s -> p (n j s)"), start=True, stop=True),
                 reads=[onesb, xmb, cumsb], writes=[prpb[0]])
            P.op("act", lambda a: a.activation(out=LSE[:],
                                               in_=prp[0][:, 0:NCH * 8].rearrange("p (n s) -> p n s", s=8)[:, :, 0:4],
                                               func=AF.Ln), reads=[prpb[0]], writes=[LSEb])
            P.op("act", lambda a: a.activation(out=bl[:],
                                               in_=prp[0][:, 0:NCH * 8].rearrange("p (n s) -> p n s", s=8)[:, :, 4:8],
                                               func=AF.Copy), reads=[prpb[0]], writes=[blb])
            P.op("pool", lambda g_: g_.memset(ein[:], -1e30), writes=[einb] + einB)
            for n in range(NCH):
                P.op("dve", lambda v, n=n: v.tensor_tensor(out=tmx[0][:], in0=LSE[:, n, 0:2], in1=ein[:, n, 0:2], op=ALU.max),
                     reads=[LSEb, einB[0]], writes=[tmxb[0]])
                P.op("dve", lambda v, n=n: v.tensor_tensor(out=ein[:, n + 1, 0:2], in0=tmx[0][:], in1=bl[:, n, 0:2],
                                                            op=ALU.subtract), reads=[tmxb[0], blb], writes=[einB[0]])
                m = NCH - 1 - n
                P.op("dve", lambda g_, m=m: g_.tensor_tensor(out=tmx[1][:], in0=LSE[:, m, 2:4], in1=ein[:, m + 1, 2:4],
                                                              op=ALU.max), reads=[LSEb, einB[1]], writes=[tmxb[1]])
                P.op("dve", lambda g_, m=m: g_.tensor_tensor(out=ein[:, m, 2:4], in0=tmx[1][:], in1=bl[:, m, 2:4],
                                                              op=ALU.subtract), reads=[tmxb[1], blb], writes=[einB[1]])
            P.op("dve", lambda v: v.tensor_tensor(out=Mp[:, :, 0:2], in0=LSE[:, :, 0:2], in1=ein[:, 0:NCH, 0:2], op=ALU.max),
                 reads=[LSEb] + einB, writes=[Mpb])
            P.op("dve", lambda v: v.tensor_tensor(out=Mp[:, :, 2:4], in0=LSE[:, :, 2:4], in1=ein[:, 1:NCH + 1, 2:4], op=ALU.max),
                 reads=[LSEb] + einB, writes=[Mpb])
            P.op("dve", lambda v: v.tensor_tensor(out=wpv[:, :, 0:2], in0=ein[:, 0:NCH, 0:2], in1=Mp[:, :, 0:2], op=ALU.subtract),
                 reads=[Mpb] + einB, writes=[wpvb])
            P.op("dve", lambda v: v.tensor_tensor(out=wpv[:, :, 2:4], in0=ein[:, 1:NCH + 1, 2:4], in1=Mp[:, :, 2:4], op=ALU.subtract),
                 reads=[Mpb] + einB, writes=[wpvb])
            P.op("act", lambda a: a.activation(out=wpv[:], in_=wpv[:], func=AF.Exp), reads=[wpvb], writes=[wpvb])
            for hf in range(2):
                rows = slice(hf * 64, (hf + 1) * 64)
                P.op("dve", lambda v, hf=hf, rows=rows: v.tensor_copy(
                    out=Mtm[rows, :, :], in_=Mp[rows, :, :].rearrange("p (i c) s -> p i c s", c=2)[:, :, hf, :]),
                    reads=[Mpb], writes=[Mtmb])
                P.op("dve", lambda v, hf=hf, rows=rows: v.tensor_copy(
                    out=wptm[rows, :, :], in_=wpv[rows, :, :].rearrange("p (i c) s -> p i c s", c=2)[:, :, hf, :]),
                    reads=[wpvb], writes=[wptmb])
            P.op("dve", lambda v: v.tensor_tensor(out=es[:], in0=aa[:], in1=Mtm[:], op=ALU.subtract), reads=[aab, Mtmb], writes=[esb])
            P.op("act", lambda a: a.activation(out=es[:], in_=es[:], func=AF.Exp), reads=[esb], writes=[esb])
            P.op("dve", lambda v: v.tensor_tensor(out=fden[:], in0=cums[:], in1=Mtm[:], op=ALU.subtract), reads=[cumsb, Mtmb],
                 writes=[fdenb])
            P.op("act", lambda a: a.activation(out=fden[:], in_=fden[:], func=AF.Exp), reads=[fdenb], writes=[fdenb])

            ktm = C.sb([128, NT, 128], BF16, "mktm")
            ktmb = [Buf() for _ in range(NT)]
            ptb1 = Buf()
            GRP = min(4, NT)
            for g0 in range(0, NT, GRP):
                for k in range(GRP):
                    P.op("pe", lambda t, g0=g0, k=k: t.transpose(out=pt[0][:, k * 128:(k + 1) * 128],
                                                                 in_=mkT[:, (g0 + k) * 128:(g0 + k + 1) * 128],
                                                                 identity=identb[:]), reads=[mkTb, ibb], writes=[ptb1])
                P.op("dve", lambda a, g0=g0: a.tensor_copy(out=ktm[:, g0:g0 + GRP, :],
                                                            in_=pt[0][:, 0:GRP * 128].rearrange("p (k t) -> p k t", k=GRP)),
                     reads=[ptb1], writes=ktmb[g0:g0 + GRP])
            qz = C.sb([128, NT, 2, 128], BF16, "qz")
            qzb = Buf()
            P.op("pool", lambda g_: g_.memset(qz[:], 0.0), writes=[qzb])
            for c in range(2):
                P.op("pool", lambda g_, c=c: g_.tensor_copy(out=qz[:, :, c, c * 64:(c + 1) * 64],
                                                            in_=mqT[:].rearrange("p (n c e) -> p n c e", c=2, e=64)[:, :, c, :]),
                     reads=[mqTb, qzb], writes=[qzb])
            hacc = C.sb([128, NT, 128], F32, "hacc")
            haccb = [Buf() for _ in range(NT)]
            mixM = C.sb([128, T], BF16, "mixM")
            mixMb = []

            def M2(nm, shape, dt):
                return [C.sb(shape, dt, f"{nm}{j}") for j in range(2)], [Buf() for _ in range(2)]
            st_, stb = M2("mst", [128, 2, 128], BF16)
            kw_, kwb = M2("mkw", [128, 128], BF16)
            isb_, isbb = M2("misb", [128, 130], F32)
            res_, resb = M2("mres", [128, 130], F32)
            den_, denb = M2("mden", [128, 2], F32)
            htmp_, htmpb = M2("mhtmp", [128, 128], F32)
            C32d = [C.sb([128, 130], F32, f"C32_{d}") for d in range(2)]
            C32db = [Buf(), Buf()]
            Cbfd = [[C.sb([128, 130], BF16, f"Cbf{d}_{j}") for j in range(2)] for d in range(2)]
            Cbfdb = [[Buf(), Buf()], [Buf(), Buf()]]
            kwz = [C.sb([128, 2, 128], BF16, f"kwz{j}") for j in range(2)]
            kwzb = [Buf(), Buf()]
            for d in range(2):
                P.op("pool", lambda g_, d=d: g_.memset(C32d[d][:], 0.0), writes=[C32db[d]])
                P.op("pool", lambda g_, d=d: g_.memset(kwz[d][:], 0.0), writes=[kwzb[d]])
                for j in range(2):
                    P.op("pool", lambda g_, d=d, j=j: g_.memset(Cbfd[d][j][:], 0.0), writes=[Cbfdb[d][j]])
            P.barrier()
            P.flush()
            pt_ctx0.__exit__(None, None, None)
            C._n += 1
            pi_ctx = [nc.psum_tensor(f"pi{C._n}_{k}", [128, 512], F32) for k in range(2)]
            pi2 = [c_.__enter__() for c_ in pi_ctx]
            pi2b = [Buf(), Buf()]
            kvb = [(pk[0], pkb[0]), (prp[0], prpb[0])]
            scurd = [0, 0]
            seen = set()

            def m_body(d, i, j):
                chunks = (0, 1) if d == 0 else (1, 0)
                tsl = slice(i * 128, (i + 1) * 128)
                sb_ = [(pst[j], pstb[j]), (pz[j], pzb[j])]
                kvt, kvtb = kvb[j]
                pit, pitb = pi2[j], pi2b[j]
                for h in range(2):
                    rows = slice(h * 64, (h + 1) * 64)
                    P.op("pe", lambda t, h=h, rows=rows: t.matmul(out=sb_[h][0][:, 0:128], lhsT=mkT[rows, tsl],
                                                                  rhs=mqT[rows, tsl], start=True, stop=True),
                         reads=[mkTb, mqTb], writes=[sb_[h][1]])
                for h in range(2):
                    sc = d * 2 + h
                    for c in range(2):
                        crow = slice(c * 64, (c + 1) * 64)
                        P.op("act", lambda a, h=h, sc=sc, c=c, crow=crow: a.activation(
                            out=kwz[j][crow, c, h * 64:(h + 1) * 64], in_=ktm[crow, i, h * 64:(h + 1) * 64], func=AF.Copy,
                            scale=es[crow, i, sc:sc + 1]), reads=[ktmb[i], esb], writes=[kwzb[j]])
                yield
                for h in range(2):
                    sc = d * 2 + h
                    P.op("dve", lambda v, h=h, sc=sc: v.scalar_tensor_tensor(
                        out=st_[j][:, h, :], in0=sb_[h][0][:, 0:128], scalar=es[:, i, sc:sc + 1],
                        in1=cf[:, d, :], op0=ALU.mult, op1=ALU.mult), reads=[sb_[h][1], esb, cfb], writes=[stb[j]])
                for c in range(2):
                    P.op("pe", lambda t, c=c: t.matmul(out=kvt[:, c * 130:(c + 1) * 130], lhsT=kwz[j][:, c, :],
                                                       rhs=vaug[:, i, :, :].rearrange("p h e -> p (h e)"),
                                                       start=True, stop=True),
                         reads=[kwzb[j], tmb[i]], writes=[kvtb])
                yield
                for h in range(2):
                    P.op("pe", lambda t, h=h: t.matmul(out=pit[:, h * 65:(h + 1) * 65], lhsT=st_[j][:, h, :],
                                                       rhs=vaug[:, i, h, :], start=True, stop=True),
                         reads=[stb[j], tmb[i]], writes=[pitb])
                yield
                C32, C32b, Cbf, Cbfb, scur = C32d[d], C32db[d], Cbfd[d], Cbfdb[d], scurd[d]
                first = True
                for ci, c in enumerate(chunks):
                    n = 2 * i + c
                    P.op("pe", lambda t, c=c, scur=scur, first=first, ci=ci: t.matmul(
                        out=pit[:, 130:260], lhsT=qz[:, i, c, :], rhs=Cbf[scur][:], start=first, stop=(ci == 1)),
                        reads=[qzb, Cbfb[scur]], writes=[pitb])
                    first = False
                    yield
                    for h in range(2):
                        rows = slice(h * 64, (h + 1) * 64)
                        sc = d * 2 + h
                        P.op("dve", lambda v, c=c, h=h, rows=rows, sc=sc, n=n: v.scalar_tensor_tensor(
                            out=C32[rows, h * 65:(h + 1) * 65], in0=C32[rows, h * 65:(h + 1) * 65],
                            scalar=wpv[rows, n, sc:sc + 1], in1=kvt[rows, c * 130 + h * 65:c * 130 + (h + 1) * 65],
                            op0=ALU.mult, op1=ALU.add), reads=[C32b, wpvb, kvtb], writes=[C32b])
                    yield
                    scur = 1 - scur
                    P.op("act", lambda a, scur=scur: a.activation(out=Cbf[scur][:], in_=C32[:], func=AF.Copy),
                         reads=[C32b], writes=[Cbfb[scur]])
                    yield
                scurd[d] = scur
                P.op("act", lambda a: a.activation(out=isb_[j][:], in_=pit[:, 0:130], func=AF.Copy),
                     reads=[pitb], writes=[isbb[j]])
                yield
                for h in range(2):
                    sc = d * 2 + h
                    P.op("dve", lambda v, h=h, sc=sc: v.scalar_tensor_tensor(
                        out=res_[j][:, h * 65:(h + 1) * 65], in0=pit[:, 130 + h * 65:130 + (h + 1) * 65],
                        scalar=wptm[:, i, sc:sc + 1], in1=isb_[j][:, h * 65:(h + 1) * 65], op0=ALU.mult, op1=ALU.add),
                        reads=[pitb, wptmb, isbb[j]], writes=[resb[j]])
                P.op("dve", lambda v: v.scalar_tensor_tensor(
                    out=den_[j][:], in0=res_[j][:].rearrange("p (h e) -> p h e", h=2)[:, :, 64], scalar=-1.0,
                    in1=res_[j][:].rearrange("p (h e) -> p h e", h=2)[:, :, 64], op0=ALU.mult, op1=ALU.max),
                    reads=[resb[j]], writes=[denb[j]])
                yield
                P.op("dve", lambda v: v.tensor_tensor(out=den_[j][:], in0=den_[j][:], in1=fden[:, i, d * 2:(d + 1) * 2],
                                                      op=ALU.max), reads=[denb[j], fdenb], writes=[denb[j]])
                P.op("dve", lambda v: v.reciprocal(out=den_[j][:], in_=den_[j][:]), reads=[denb[j]], writes=[denb[j]])
                yield
                if i not in seen:
                    seen.add(i)
                    P.op("dve", lambda v: v.tensor_tensor(
                        out=hacc[:, i, :].rearrange("p (h e) -> p h e", h=2),
                        in0=res_[j][:].rearrange("p (h e) -> p h e", h=2)[:, :, 0:64],
                        in1=bc(den_[j][:], 2, [128, 2, 64]), op=ALU.mult), reads=[resb[j], denb[j]], writes=[haccb[i]])
                else:
                    P.op("dve", lambda v: v.tensor_tensor(
                        out=htmp_[j][:].rearrange("p (h e) -> p h e", h=2),
                        in0=res_[j][:].rearrange("p (h e) -> p h e", h=2)[:, :, 0:64],
                        in1=bc(den_[j][:], 2, [128, 2, 64]), op=ALU.mult), reads=[resb[j], denb[j]], writes=[htmpb[j]])
                    P.op("pool", lambda g_: g_.tensor_tensor(out=hacc[:, i, :], in0=hacc[:, i, :], in1=htmp_[j][:],
                                                             op=ALU.add), reads=[htmpb[j], haccb[i]], writes=[haccb[i]])

            for p_ in range(NT):
                gens = [m_body(0, p_, 0), m_body(1, NT - 1 - p_, 1)]
                live = [True, True]
                while any(live):
                    for k_ in range(2):
                        if live[k_]:
                            try:
                                next(gens[k_])
                            except StopIteration:
                                live[k_] = False
            P.barrier()
            P.flush()
            for c_ in reversed(pi_ctx):
                c_.__exit__(None, None, None)
            pt, ptb = psum_banks(1, BF16, 1024)
            finalize_heads(C, hacc, haccb, smo, tmb, NT, identb, ibb, pt[0], mixM, mixMb, "m")
            P.dma("sp", mix_rows[3][0], mixM[:], reads=mixMb, writes=mix_rows[3][1], is_output=mix_is_output)
    if standalone:
        P.final_wait()
        P.flush()
    return C


PAIRS = [[0, 1], [2, 3], [4, 5], [6, 7]]


def build_fused(T):
    C = Ctx()
    nc, P = C.nc, C.P
    T2 = T // 2
    sel_d = C.din("sel", [128, 2], F32)
    sel = C.sb([128, 2], F32, "sel")
    selB = Buf()
    P.dma("sp", sel[:], sel_d, writes=[selB])
    mixb = [[nc.dram_tensor(f"mixb{l}_{c}", [256, T], BF16).ap() for c in range(2)] for l in range(2)]
    mixg = [[nc.dram_tensor(f"mixg{l}_{c}", [512, T], BF16).ap() for c in range(2)] for l in range(2)]
    hb = [nc.dram_tensor(f"hb_{c}", [512, T2], BF16).ap() for c in range(2)]
    hg = [nc.dram_tensor(f"hg_{c}", [1024, T2], BF16).ap() for c in range(2)]
    xs = nc.dram_tensor("xs", [T2, 1024], F32).ap()
    mixbB = [[Buf(), Buf()] for _ in range(2)]
    mixgB = [[Buf(), Buf()] for _ in range(2)]
    hbB, hgB, xsB = [Buf(), Buf()], [Buf(), Buf()], Buf()

    def stage(fn):
        ph = C.phase()
        ph.__enter__()
        fn()
        ph.__exit__(None, None, None)

    def mix_dst(l):
        return [(mixb[l][q // 2][(q % 2) * 128:(q % 2) * 128 + 128, :], mixbB[l][q // 2]) for q in range(4)]

    def mix_src(l):
        out = []
        for kc in range(8):
            r, q = kc // 4, kc % 4
            off = r * 256 + (q % 2) * 128
            out.append((mixg[l][q // 2][off:off + 128, :], mixgB[l][q // 2]))
        return out

    def gather_mix(l, c):
        P.collective("AllGather", mixb[l][c], mixg[l][c], PAIRS, reads=[mixbB[l][c]], writes=[mixgB[l][c]])

    h_dst = [(hb[k0 // 4][(k0 % 4) * 128:(k0 % 4) * 128 + 256, :], hbB[k0 // 4]) for k0 in range(0, 8, 2)]
    h_src = {(r, k0): (hg[k0 // 4][r * 512 + (k0 % 4) * 128:r * 512 + (k0 % 4) * 128 + 256, :], hgB[k0 // 4])
             for r in range(2) for k0 in range(0, 8, 2)}

    stage(lambda: build_k1(T, C=C, sfx="_l0", mix_dst=mix_dst(0), after_A=lambda: gather_mix(0, 0)))
    gather_mix(0, 1)
    stage(lambda: build_k2(T2, False, C=C, sfx="_l0", fz=dict(mixg=mix_src(0), sel=(sel, selB),
                                                              x_dst=(xs, xsB), h_dst=h_dst)))
    for c in range(2):
        P.collective("AllGather", hb[c], hg[c], PAIRS, reads=[hbB[c]], writes=[hgB[c]])
    stage(lambda: build_k1(T, C=C, sfx="_l1", hsrc=h_src, mix_dst=mix_dst(1), after_A=lambda: gather_mix(1, 0)))
    gather_mix(1, 1)
    stage(lambda: build_k2(T2, True, C=C, sfx="_l1", fz=dict(x_src=(xs, xsB), mixg=mix_src(1), sel=(sel, selB))))
    P.final_wait()
    P.flush()
    return C


_OFF = {}
_o = 0
for _n, _s in zip(("aq", "ak", "av", "gq", "gk", "gv", "gg", "glr", "mq", "mk", "mv", "mo", "mg"),
                  (512, 128, 128, 256, 256, 256, 256, 32, 256, 256, 256, 256, 16)):
    _OFF[_n] = (_o, _o + _s)
    _o += _s


def _consts(T):
    t = np.arange(T)
    row = (t // 64).astype(np.float32)
    col = (t % 64).astype(np.float32)
    inv = (np.float32(10000.0) ** (-np.arange(0, 32, 2, dtype=np.float32) / np.float32(32))).astype(np.float32)
    ar = row[:, None] * inv[None, :]
    ac = col[:, None] * inv[None, :]
    cr, sr, cc, sc = np.cos(ar), np.sin(ar), np.cos(ac), np.sin(ac)
    rope = np.concatenate([cr, cr, cc, cc, -sr, sr, -sc, sc], axis=1).astype(np.float32)
    s = np.arange(128)[:, None]
    u = np.arange(128)[None, :]
    same = (s // 64) == (u // 64)
    tri_f = (same & (s <= u)).astype(np.float32)
    tri_b = (same & (s >= u)).astype(np.float32)
    cstart = (np.arange(128) // 64) * 64
    mid_f = tri_f - tri_f[:, cstart + 31]
    mid_b = tri_b - tri_b[:, cstart + 32]
    last_f = (same & (s > u)).astype(np.float32)
    last_b = (same & (s < u)).astype(np.float32)
    cf = np.stack([tri_f, tri_b]).astype(np.float32)
    cs = (np.stack([tri_f, tri_b, mid_f, mid_b, last_f, last_b]) * np.float32(-1.0 / 16.0)).astype(np.float32)
    hm = np.stack([(np.arange(128) // 64) == 0, (np.arange(128) // 64) == 1], axis=1).astype(np.float32)
    return dict(rope=rope, cf=cf, cs=cs, hm=hm)


def k1_inputs(xb, W, li, hf, consts):
    w_in = W["w_in"][li]

    def cols(name, lo, hi):
        a, _ = _OFF[name]
        return w_in[:, a + lo:a + hi]
    ak = cols("ak", hf * 64, hf * 64 + 64)
    h0, h1 = 2 * hf, 2 * hf + 1
    mg = w_in[:, _OFF["mg"][0]:_OFF["mg"][1]]
    gidx = [0 + h0, 0 + h1, 8 + h0, 8 + h1, 4 + h0, 4 + h1, 12 + h0, 12 + h1]
    bi, bf = W["mlstm_b_input"][li], W["mlstm_b_forget"][li]
    gates_b = np.array([bi[0][h0], bi[0][h1], bi[1][h0], bi[1][h1], bf[0][h0], bf[0][h1], bf[1][h0], bf[1][h1]],
                       np.float32)
    sl = slice(hf * 128, hf * 128 + 128)
    wdb = np.zeros((2, 17, 128), np.float32)
    wdb[:, 0:16, :] = W["gla_w_decay"][li][:, :, sl]
    wdb[:, 16, :] = W["gla_b_decay"][li][:, sl]
    cwv, cbv = W["mlstm_conv_w"][li], W["mlstm_conv_b"][li]
    ch = np.concatenate([np.arange(hf * 128, hf * 128 + 128), 256 + np.arange(hf * 128, hf * 128 + 128)])
    cw = np.concatenate([cwv[:, ch].T, cbv[ch][:, None]], axis=1).astype(np.float32)
    qg, kg = W["attn_q_norm_g"][li], W["attn_k_norm_g"][li]
    d = dict(
        x=np.ascontiguousarray(xb), norm_mix_g=W["norm_mix_g"][li],
        w_att=np.ascontiguousarray(np.concatenate([cols("aq", hf * 256, hf * 256 + 256), ak, ak,
                                                   cols("av", hf * 64, hf * 64 + 64)], axis=1)),
        w_gla_f=np.ascontiguousarray(np.concatenate([cols("gq", sl.start, sl.stop), cols("gk", sl.start, sl.stop)], 1)),
        w_gla_t=np.ascontiguousarray(np.concatenate([cols("gk", sl.start, sl.stop), cols("gv", sl.start, sl.stop),
                                                     cols("gg", sl.start, sl.stop)], 1)),
        w_glr=np.ascontiguousarray(cols("glr", 0, 32)), wdb=wdb,
        w_ml_f=np.ascontiguousarray(np.concatenate([cols("mq", sl.start, sl.stop), cols("mk", sl.start, sl.stop)], 1)),
        w_ml_t=np.ascontiguousarray(np.concatenate([cols("mv", sl.start, sl.stop), cols("mo", sl.start, sl.stop),
                                                    mg[:, gidx]], 1)),
        cw=cw, gates_b=gates_b, g6=np.concatenate([qg, qg, qg, qg, kg, kg]).astype(np.float32),
        gla_g=np.tile(W["gla_out_norm_g"][li], 2).astype(np.float32),
        ml_g=np.tile(W["mlstm_out_norm_g"][li], 2).astype(np.float32),
    )
    d.update(consts)
    return d


_PROGS = {}


def _prog(key, fn):
    if key not in _PROGS:
        _PROGS[key] = fn()
    return _PROGS[key]


def _k2_inputs(W, li, last, fused):
    d = dict(
        norm_ffn_g=W["norm_ffn_g"][li],
        w_gr=np.ascontiguousarray(np.concatenate([W["w_group"][li], W["w_router"][li]], axis=1)),
        b_gr=np.concatenate([W["b_group"][li], W["b_router"][li]]),
        w_gate=W["w_expert_gate"][li], w_up=W["w_expert_up"][li], w_down=W["w_expert_down"][li],
        norm_ple_g=W["norm_ple_g"][li], w_ple_gate=W["w_ple_gate"][li], w_ple_proj=W["w_ple_proj"][li])
    if fused:
        perm = np.concatenate([np.concatenate([np.arange(r * 256, (r + 1) * 256), 512 + np.arange(r * 128, (r + 1) * 128),
                                               768 + np.arange(r * 128, (r + 1) * 128)]) for r in range(2)])
        d["w_out"] = np.ascontiguousarray(W["w_out"][li][perm])
    else:
        d["w_out"] = W["w_out"][li]
    if last:
        d["final_norm_g"] = W["final_norm_g"]
    return d


def kernel_unfused(**inputs):
    W = {k: np.asarray(v) for k, v in inputs.items()}
    x = np.ascontiguousarray(W["x"], dtype=np.float32)
    B, T, D = x.shape
    TH = T // 2
    consts = _consts(T)
    cores = list(range(8))
    for li in range(2):
        k1 = _prog(("k1", T), lambda: build_k1(T))
        in_maps = [k1_inputs(x[c // 2], W, li, c % 2, consts) for c in cores]
        res = run_bass_kernel_spmd(k1.nc, in_maps, core_ids=cores).results
        mixT = np.zeros((B, 1024, T), dtype=ml_dtypes.bfloat16)
        for c in cores:
            b, hf = c // 2, c % 2
            r = np.asarray(res[c]["mixT"])
            mixT[b, hf * 256:(hf + 1) * 256] = r[0:256]
            mixT[b, 512 + hf * 128:512 + (hf + 1) * 128] = r[256:384]
            mixT[b, 768 + hf * 128:768 + (hf + 1) * 128] = r[384:512]
        last = (li == 1)
        k2 = _prog(("k2", TH, last), lambda: build_k2(TH, last))
        in_maps = []
        for c in cores:
            b, half = c // 2, c % 2
            th = slice(half * TH, (half + 1) * TH)
            d = _k2_inputs(W, li, last, False)
            d.update(xh=np.ascontiguousarray(x[b, th]), mixT=np.ascontiguousarray(mixT[b][:, th]),
                     p=np.ascontiguousarray(W["p"][li, b, th]))
            in_maps.append(d)
        res = run_bass_kernel_spmd(k2.nc, in_maps, core_ids=cores).results
        xn = np.empty_like(x)
        for c in cores:
            b, half = c // 2, c % 2
            xn[b, half * TH:(half + 1) * TH] = np.asarray(res[c]["out"])
        x = xn
    return x.astype(np.float32)


def kernel(**inputs):
    W = {k: np.asarray(v) for k, v in inputs.items()}
    x = np.ascontiguousarray(W["x"], dtype=np.float32)
    B, T, D = x.shape
    TH = T // 2
    consts = _consts(T)
    cores = list(range(8))
    prog = _prog(("fused", T), lambda: build_fused(T))
    k2w = [_k2_inputs(W, li, li == 1, True) for li in range(2)]
    in_maps = []
    for c in cores:
        b, half = c // 2, c % 2
        th = slice(half * TH, (half + 1) * TH)
        d = {}
        for li in range(2):
            k1 = k1_inputs(x[b], W, li, half, consts)
            if li == 1:
                k1.pop("x")
                k1.pop("norm_mix_g")
            for k, v in k1.items():
                if k in consts:
                    d[k] = v
                else:
                    d[f"{k}_l{li}"] = v
            for k, v in k2w[li].items():
                d[f"{k}_l{li}"] = v
            d[f"p_l{li}"] = np.ascontiguousarray(W["p"][li, b, th])
        d["xh_l0"] = np.ascontiguousarray(x[b, th])
        d["norm_mix_g_next_l0"] = W["norm_mix_g"][1]
        d["sel"] = np.tile(np.array([[1.0 - half, float(half)]], np.float32), (128, 1))
        in_maps.append(d)
    res = run_bass_kernel_spmd(prog.nc, in_maps, core_ids=cores).results
    out = np.empty_like(x)
    for c in cores:
        b, half = c // 2, c % 2
        out[b, half * TH:(half + 1) * TH] = np.asarray(res[c]["out"])
    return out.astype(np.float32)
```

```python
import numpy as np
import ml_dtypes
from contextlib import ExitStack
import concourse.bass as bass
import concourse.mybir as mybir
from concourse.bass_utils import run_bass_kernel_spmd

F32 = mybir.dt.float32
BF16 = mybir.dt.bfloat16
AF = mybir.ActivationFunctionType
ALU = mybir.AluOpType
AX = mybir.AxisListType

EPS = 1e-6
EPOCH = 30000
_DBG = {}


class Buf:
    __slots__ = ("w", "rs", "name")

    def __init__(self, name=""):
        self.w = None
        self.rs = {}
        self.name = name


class _Q:
    def __init__(self, name):
        self.name = name
        self.ops = []
        self.n = 0
        self.known = {}
        self.shared = False
        self.maxep = {}


class Prog:
    ENG = ["pe", "act", "dve", "pool", "sp"]

    def __init__(self, nc, stack, n_dma_sems=24):
        self.nc = nc
        self.stack = stack
        self.q = {e: _Q(e) for e in self.ENG}
        self.esems = {e: [] for e in self.ENG}
        self.dsems = [stack.enter_context(nc.semaphore(f"dma{i}")) for i in range(n_dma_sems)]
        self.dcnt = [0] * n_dma_sems
        self.drr = 0
        self.snap = {}
        self.out_toks = []
        self.count = 0
        self.stop_at = None
        self.csems = []

    def _esem(self, e, epoch):
        while len(self.esems[e]) <= epoch:
            k = len(self.esems[e])
            self.esems[e].append(self.stack.enter_context(self.nc.semaphore(f"s_{e}{k}")))
        return self.esems[e][epoch]

    def _sem_of(self, key):
        if key[0] == "d":
            return self.dsems[key[1]] if key[1] < 1000 else self.csems[key[1] - 1000]
        return self._esem(key[0], key[1])

    def _is_known(self, q, key, val):
        if q.known.get(key, 0) >= val:
            return True
        if key[0] != "d" and q.maxep.get(key[0], -1) > key[1]:
            return True
        return False

    def _learn1(self, q, key, val):
        if q.known.get(key, 0) < val:
            if q.shared:
                q.known = dict(q.known)
                q.shared = False
            q.known[key] = val
        if key[0] != "d" and q.maxep.get(key[0], -1) < key[1]:
            q.maxep[key[0]] = key[1]

    def _wait(self, q, tok):
        key, val = tok
        if self._is_known(q, key, val):
            return
        sem = self._sem_of(key)
        q.ops.append(lambda eng, sem=sem, val=val: eng.wait_ge(sem, val))
        self._learn1(q, key, val)
        sn = self.snap.get(tok)
        if sn:
            for k, v in sn.items():
                self._learn1(q, k, v)

    def _deps(self, q, e, reads, writes):
        deps = {}
        for b in reads:
            if b.w is not None:
                deps[b.w] = 1
        for b in writes:
            if b.w is not None:
                deps[b.w] = 1
            for k, v in b.rs.items():
                deps[(k, v)] = 1
        for tok in deps:
            if e == "pe" and tok[0][0] == "pe":
                continue
            self._wait(q, tok)

    def _mark(self, tok, q, reads, writes):
        self.snap[tok] = q.known
        q.shared = True
        key, val = tok
        for b in reads:
            if b.rs.get(key, 0) < val:
                b.rs[key] = val
        for b in writes:
            b.w = tok
            b.rs = {}

    def op(self, e, fn, reads=(), writes=()):
        self.count += 1
        if self.stop_at is not None and self.count > self.stop_at:
            return None
        q = self.q[e]
        self._deps(q, e, reads, writes)
        epoch, cnt = divmod(q.n, EPOCH)
        cnt += 1
        q.n += 1
        sem = self._esem(e, epoch)
        q.ops.append(lambda eng, sem=sem, fn=fn: fn(eng).then_inc(sem, 1))
        tok = ((e, epoch), cnt)
        self._mark(tok, q, reads, writes)
        return tok

    def dma(self, e, out, in_, reads=(), writes=(), is_output=False, slow=False):
        self.count += 1
        if self.stop_at is not None and self.count > self.stop_at:
            return None
        q = self.q[e]
        i = self.drr
        self.drr = (self.drr + 1) % len(self.dsems)
        if self.dcnt[i] > 0:
            self._wait(q, (("d", i), self.dcnt[i]))
        self._deps(q, e, reads, writes)
        self.dcnt[i] += 16
        sem = self.dsems[i]
        kw = dict(allow_slow_non_contiguous=True) if slow else {}
        q.ops.append(lambda eng, sem=sem, out=out, in_=in_, kw=kw: eng.dma_start(out=out, in_=in_, **kw).then_inc(sem, 16))
        tok = (("d", i), self.dcnt[i])
        self._mark(tok, q, reads, writes)
        if is_output:
            self.out_toks.append(tok)
        return tok

    def collective(self, kind, in_ap, out_ap, groups, reads=(), writes=()):
        if _DBG.get("no_cc"):
            return None
        q = self.q["pool"]
        self._deps(q, "pool", reads, writes)
        k = len(self.csems)
        sem = self.stack.enter_context(self.nc.semaphore(f"cc{k}"))
        self.csems.append(sem)
        q.ops.append(lambda eng, sem=sem: eng.collective_compute(kind, ALU.bypass, replica_groups=groups,
                                                                 ins=[in_ap.opt()], outs=[out_ap.opt()]).then_inc(sem, 1))
        tok = (("d", 1000 + k), 1)
        self._mark(tok, q, reads, writes)
        return tok

    def barrier(self, bufs=()):
        toks = []
        for e in self.ENG:
            q = self.q[e]
            if q.n > 0:
                epoch, cnt = divmod(q.n - 1, EPOCH)
                toks.append(((e, epoch), cnt + 1))
        for i, c in enumerate(self.dcnt):
            if c > 0:
                toks.append((("d", i), c))
        for k in range(len(self.csems)):
            toks.append((("d", 1000 + k), 1))
        for e in self.ENG:
            for tok in toks:
                if tok[0][0] == e:
                    continue
                self._wait(self.q[e], tok)

    def final_wait(self):
        q = self.q["sp"]
        for tok in self.out_toks:
            self._wait(q, tok)

    def flush(self):
        qs = {e: list(self.q[e].ops) for e in self.ENG}
        for e in self.ENG:
            self.q[e].ops = []
        if not any(qs.values()):
            return
        with self.nc.Block() as block:
            @block.tensor
            def _(eng):
                for f in qs["pe"]:
                    f(eng)

            @block.scalar
            def _(eng):
                for f in qs["act"]:
                    f(eng)

            @block.vector
            def _(eng):
                for f in qs["dve"]:
                    f(eng)

            @block.gpsimd
            def _(eng):
                for f in qs["pool"]:
                    f(eng)

            @block.sync
            def _(eng):
                for f in qs["sp"]:
                    f(eng)


class Ctx:
    def __init__(self):
        self.nc = bass.Bass("TRN2", target_bir_lowering=False)
        self.stack = ExitStack()
        self.P = Prog(self.nc, self.stack)
        self._n = 0
        self.cur = self.stack

    def phase(self):
        C = self

        class _Ph:
            def __enter__(s2):
                s2.prev = C.cur
                s2.st = ExitStack()
                C.cur = s2.st
                return s2.st

            def __exit__(s2, *a):
                if a[0] is None:
                    C.P.barrier()
                    C.P.flush()
                C.cur = s2.prev
                s2.st.close()
                return False
        return _Ph()

    def sb(self, shape, dt, name=None, stack=None):
        self._n += 1
        t = (stack or self.cur).enter_context(self.nc.sbuf_tensor(f"{name or 't'}_{self._n}", list(shape), dt))
        return t

    def ps(self, shape, dt=F32, name=None):
        self._n += 1
        return self.cur.enter_context(self.nc.psum_tensor(f"{name or 'p'}_{self._n}", list(shape), dt))

    def din(self, name, shape, dt):
        if not hasattr(self, "_dins"):
            self._dins = {}
        if name not in self._dins:
            self._dins[name] = self.nc.dram_tensor(name, list(shape), dt, kind="ExternalInput").ap()
        return self._dins[name]

    def dout(self, name, shape, dt):
        return self.nc.dram_tensor(name, list(shape), dt, kind="ExternalOutput").ap()


def bc(ap, axis, shape):
    return ap.unsqueeze(axis).to_broadcast(list(shape))


def load_ident(C, dt=F32):
    P = C.P
    ident = C.sb([128, 128], dt, "ident")
    b = Buf("ident")
    P.op("pool", lambda g: g.memset(ident[:], 1.0), writes=[b])
    P.op("pool", lambda g: g.affine_select(out=ident[:], in_=ident[:], pattern=[[-1, 128]],
                                           compare_op=ALU.is_equal, fill=0.0, base=0, channel_multiplier=1),
         reads=[b], writes=[b])
    return ident, b


def rms_to_hT(C, x_sb, xb, NT, g32col, gb, ident, ib, hT, hTb, ps2, ps2b, fp32_cb=None, tag=""):
    P, nc = C.P, C.nc
    ss = C.sb([128, NT], F32, "ss" + tag)
    ssb = Buf("ss")
    r = C.sb([128, NT], F32, "r" + tag)
    rb = Buf("r")
    junk = C.sb([128, 1024], BF16, "junk" + tag)
    junkb = Buf("junk")
    P.op("pool", lambda g: g.memset(ss[:], 0.0), writes=[ssb])
    for i in range(NT):
        P.op("act", lambda a, i=i: a.activation(out=junk[:], in_=x_sb[:, i, :], func=AF.Square,
                                                 accum_out=ss[:, i:i + 1]),
             reads=[xb[i]], writes=[junkb, ssb])
    P.op("act", lambda a: a.activation(out=r[:], in_=ss[:], func=AF.Sqrt, scale=1.0 / 1024.0, bias=EPS),
         reads=[ssb], writes=[rb])
    P.op("dve", lambda v: v.reciprocal(out=r[:], in_=r[:]), reads=[rb], writes=[rb])
    xs = [C.sb([128, 1024], F32, f"xs{tag}{j}") for j in range(2)]
    xsb = [Buf("xs") for _ in range(2)]
    hTf = [C.sb([128, 8, 128], F32, f"hTf{tag}{j}") for j in range(2)]
    hTfb = [Buf("hTf") for _ in range(2)]
    for i in range(NT):
        j = i % 2
        P.op("act", lambda a, i=i, j=j: a.activation(out=xs[j][:], in_=x_sb[:, i, :], func=AF.Copy,
                                                      scale=r[:, i:i + 1]),
             reads=[xb[i], rb], writes=[xsb[j]])
        for kc in range(8):
            P.op("pe", lambda t, j=j, kc=kc: t.transpose(out=ps2[j][:, kc * 128:(kc + 1) * 128],
                                                         in_=xs[j][:, kc * 128:(kc + 1) * 128],
                                                         identity=ident[:]),
                 reads=[xsb[j], ib], writes=[ps2b[j]])
        if fp32_cb is not None:
            P.op("dve", lambda v, j=j: v.tensor_tensor(out=hTf[j][:], in0=ps2[j][:].rearrange("p (k t) -> p k t", k=8),
                                                        in1=bc(g32col[:], 2, [128, 8, 128]), op=ALU.mult),
                 reads=[ps2b[j], gb], writes=[hTfb[j]])
            P.op("pool", lambda g, i=i, j=j: g.tensor_copy(out=hT[:, :, i * 128:(i + 1) * 128], in_=hTf[j][:]),
                 reads=[hTfb[j]], writes=[hTb[i]])
            fp32_cb(i, hTf[j], hTfb[j])
        else:
            P.op("dve", lambda v, i=i, j=j: v.tensor_tensor(out=hT[:, :, i * 128:(i + 1) * 128],
                                                             in0=ps2[j][:].rearrange("p (k t) -> p k t", k=8),
                                                             in1=bc(g32col[:], 2, [128, 8, 128]), op=ALU.mult),
                 reads=[ps2b[j], gb], writes=[hTb[i]])


def load_gcol32(C, g_d, tag):
    P = C.P
    graw = C.sb([128, 8], F32, "graw" + tag)
    g32 = C.sb([128, 8], F32, "g32" + tag)
    b0, b1 = Buf(), Buf()
    P.dma("sp", graw[:], g_d.rearrange("(k p) -> p k", p=128), writes=[b0], slow=True)
    return graw, b0


def build_k2(T, last, C=None, sfx="", fz=None):
    standalone = C is None
    fz = fz or {}
    if C is None:
        C = Ctx()
    nc, P = C.nc, C.P
    NT = T // 128
    TB = min(512, T)
    NTB = T // TB
    SUB = TB // 128

    if "x_src" in fz:
        x_d, x_srcb = fz["x_src"]
        x_reads = [x_srcb]
    else:
        x_d = C.din("xh" + sfx, [T, 1024], F32)
        x_reads = []
    mixT_d = None if "mixg" in fz else C.din("mixT" + sfx, [1024, T], BF16)
    p_d = C.din("p" + sfx, [T, 256], F32)
    wout_d = C.din("w_out" + sfx, [1024, 1024], F32)
    gffn_d = C.din("norm_ffn_g" + sfx, [1024], F32)
    wr_d = C.din("w_gr" + sfx, [1024, 20], F32)
    br_d = C.din("b_gr" + sfx, [20], F32)
    wg_d = C.din("w_gate" + sfx, [16, 1024, 512], F32)
    wu_d = C.din("w_up" + sfx, [16, 1024, 512], F32)
    wd_d = C.din("w_down" + sfx, [16, 512, 1024], F32)
    gple_d = C.din("norm_ple_g" + sfx, [1024], F32)
    wpg_d = C.din("w_ple_gate" + sfx, [1024, 1024], F32)
    wpp_d = C.din("w_ple_proj" + sfx, [256, 1024], F32)
    gfin_d = C.din("final_norm_g" + sfx, [1024], F32) if last else None
    if "x_dst" in fz:
        out_d, out_dstb = fz["x_dst"]
        out_writes, out_is_output = [out_dstb], False
    else:
        out_d = C.dout("out", [T, 1024], F32)
        out_writes, out_is_output = [], True
    gnext_d = C.din("norm_mix_g_next" + sfx, [1024], F32) if "h_dst" in fz else None

    x_sb = C.sb([128, NT, 1024], F32, "x")
    xb = [Buf(f"x{i}") for i in range(NT)]
    ident, ib = load_ident(C)
    psA = [C.ps([128, 1024], F32, f"psA{j}") for j in range(2)]
    psAb = [Buf() for _ in range(2)]
    psB = [C.ps([128, 512], F32, f"psB{j}") for j in range(4)]
    psBb = [Buf() for _ in range(4)]

    x_v = x_d.rearrange("(n p) d -> p n d", p=128)
    for i0 in range(0, NT, 4):
        n = min(4, NT - i0)
        P.dma("sp", x_sb[:, i0:i0 + n, :], x_v[:, i0:i0 + n, :], reads=x_reads, writes=xb[i0:i0 + n])

    with C.phase() as st:
        mixT = C.sb([128, 8, T], BF16, "mixT", st)
        mb = Buf()
        wout = C.sb([128, 8, 1024], BF16, "wout", st)
        wb = Buf()
        if "mixg" in fz:
            mixg_rows = fz["mixg"]
            sel, selB = fz["sel"]
            cand = [[C.sb([128, T], BF16, f"cand{h}{s_}", st) for s_ in range(2)] for h in range(2)]
            candb = [[Buf() for _ in range(2)] for _ in range(2)]
            for kc in range(8):
                s_ = kc % 2
                for h in range(2):
                    P.dma("sp", cand[h][s_][:], mixg_rows[kc][0][:, h * T:(h + 1) * T], reads=[mixg_rows[kc][1]],
                          writes=[candb[h][s_]])
                P.op("dve", lambda v, kc=kc, s_=s_: v.tensor_scalar(out=mixT[:, kc, :], in0=cand[0][s_][:],
                                                                     scalar1=sel[:, 0:1], scalar2=None, op0=ALU.mult),
                     reads=[candb[0][s_], selB], writes=[mb])
                P.op("dve", lambda v, kc=kc, s_=s_: v.scalar_tensor_tensor(out=mixT[:, kc, :], in0=cand[1][s_][:],
                                                                            scalar=sel[:, 1:2], in1=mixT[:, kc, :],
                                                                            op0=ALU.mult, op1=ALU.add),
                     reads=[candb[1][s_], selB, mb], writes=[mb])
        else:
            P.dma("sp", mixT[:], mixT_d.rearrange("(k p) t -> p k t", p=128), writes=[mb])
        P.dma("pool", wout[:], wout_d.rearrange("(k p) n -> p k n", p=128), writes=[wb])
        for i in range(NT):
            for nh in range(2):
                j = (i * 2 + nh) % 4
                for kc in range(8):
                    P.op("pe", lambda t, i=i, nh=nh, kc=kc, j=j: t.matmul(
                        out=psB[j][:], lhsT=mixT[:, kc, i * 128:(i + 1) * 128],
                        rhs=wout[:, kc, nh * 512:(nh + 1) * 512], start=(kc == 0), stop=(kc == 7)),
                        reads=[mb, wb], writes=[psBb[j]])
                P.op("dve", lambda v, i=i, nh=nh, j=j: v.tensor_tensor(
                    out=x_sb[:, i, nh * 512:(nh + 1) * 512], in0=x_sb[:, i, nh * 512:(nh + 1) * 512],
                    in1=psB[j][:], op=ALU.add), reads=[psBb[j], xb[i]], writes=[xb[i]])

    with C.phase() as st:
        hT = C.sb([128, 8, T], BF16, "hT", st)
        hTb = [Buf() for _ in range(NT)]
        g32, g32b = load_gcol32(C, gffn_d, "ffn")
        wr = C.sb([128, 8, 20], F32, "wr", st)
        wrb = Buf()
        P.dma("sp", wr[:], wr_d.rearrange("(k p) n -> p k n", p=128), writes=[wrb])
        brb_t = C.sb([128, 20], F32, "brb", st)
        brb = Buf()
        P.dma("sp", brb_t[:], br_d.partition_broadcast(128), writes=[brb])
        L = C.sb([128, NT, 20], F32, "logits", st)
        Lb = Buf()
        NS = 2
        wgs = [C.sb([128, 8, 512], BF16, f"wg{s}", st) for s in range(NS)]
        wus = [C.sb([128, 8, 512], BF16, f"wu{s}", st) for s in range(NS)]
        wds = [C.sb([128, 4, 1024], BF16, f"wd{s}", st) for s in range(NS)]
        wgb = [Buf() for _ in range(NS)]
        wub = [Buf() for _ in range(NS)]
        wdb = [Buf() for _ in range(NS)]
        def load_expert(e):
            s = e % NS
            P.dma("pool", wgs[s][:], wg_d[e].rearrange("(k p) n -> p k n", p=128), writes=[wgb[s]])
            P.dma("pool", wus[s][:], wu_d[e].rearrange("(k p) n -> p k n", p=128), writes=[wub[s]])
            P.dma("pool", wds[s][:], wd_d[e].rearrange("(k p) n -> p k n", p=128), writes=[wdb[s]])

        load_expert(0)

        def router_cb(i, hTf, hTfb):
            j = 2 + (i % 2)
            for kc in range(8):
                P.op("pe", lambda t, kc=kc, j=j: t.matmul(out=psB[j][:, 0:20], lhsT=hTf[:, kc, :],
                                                          rhs=wr[:, kc, :], start=(kc == 0), stop=(kc == 7)),
                     reads=[hTfb, wrb], writes=[psBb[j]])
            P.op("dve", lambda v, i=i, j=j: v.tensor_tensor(out=L[:, i, :], in0=psB[j][:, 0:20], in1=brb_t[:],
                                                             op=ALU.add), reads=[psBb[j], brb], writes=[Lb])

        rms_to_hT(C, x_sb, xb, NT, g32, g32b, ident, ib, hT, hTb, psA, psAb, fp32_cb=router_cb, tag="f")

        def S(shape, nm):
            return C.sb(shape, F32, nm, st), Buf(nm)
        gmax, gmaxb = S([128, NT], "gmax")
        gm, gmb = S([128, NT, 4], "gm")
        ge, geb = S([128, NT, 4], "ge")
        gsum, gsumb = S([128, NT], "gsum")
        gprob, gprobb = S([128, NT], "gprob")
        tmp4, tmp4b = S([128, NT, 4, 4], "tmp4")
        els, elsb = S([128, NT, 4], "els")
        m1, m1b = S([128, NT], "m1")
        mk1, mk1b = S([128, NT, 4], "mk1")
        el2, el2b = S([128, NT, 4], "el2")
        m2, m2b = S([128, NT], "m2")
        mk2, mk2b = S([128, NT, 4], "mk2")
        dd, ddb = S([128, NT], "dd")
        e2, e2b = S([128, NT], "e2")
        w1, w1b = S([128, NT], "w1")
        w2, w2b = S([128, NT], "w2")
        ws, wsb = S([128, NT, 4], "ws")
        ws2, ws2b = S([128, NT, 4], "ws2")
        comb, combb = S([128, NT, 4, 4], "comb")
        gl = L[:, :, 0:4]
        el = L[:, :, 4:20].rearrange("p n (g e) -> p n g e", g=4)
        V = lambda fn, r, w: P.op("dve", fn, reads=r, writes=w)
        V(lambda v: v.tensor_reduce(out=gmax[:], in_=gl, axis=AX.X, op=ALU.max), [Lb], [gmaxb])
        V(lambda v: v.tensor_tensor(out=gm[:], in0=gl, in1=bc(gmax[:], 2, [128, NT, 4]), op=ALU.is_equal),
          [Lb, gmaxb], [gmb])
        V(lambda v: v.tensor_tensor(out=ge[:], in0=gl, in1=bc(gmax[:], 2, [128, NT, 4]), op=ALU.subtract),
          [Lb, gmaxb], [geb])
        P.op("act", lambda a: a.activation(out=ge[:], in_=ge[:], func=AF.Exp), reads=[geb], writes=[geb])
        V(lambda v: v.tensor_reduce(out=gsum[:], in_=ge[:], axis=AX.X, op=ALU.add), [geb], [gsumb])
        V(lambda v: v.reciprocal(out=gprob[:], in_=gsum[:]), [gsumb], [gprobb])
        V(lambda v: v.tensor_tensor(out=tmp4[:], in0=el, in1=bc(gm[:], 3, [128, NT, 4, 4]), op=ALU.mult),
          [Lb, gmb], [tmp4b])
        V(lambda v: v.tensor_reduce(out=els[:], in_=tmp4[:].rearrange("p n g e -> p n e g"), axis=AX.X, op=ALU.add),
          [tmp4b], [elsb])
        V(lambda v: v.tensor_reduce(out=m1[:], in_=els[:], axis=AX.X, op=ALU.max), [elsb], [m1b])
        V(lambda v: v.tensor_tensor(out=mk1[:], in0=els[:], in1=bc(m1[:], 2, [128, NT, 4]), op=ALU.is_equal),
          [elsb, m1b], [mk1b])
        V(lambda v: v.scalar_tensor_tensor(out=el2[:].rearrange("p n e -> p (n e)"),
                                           in0=mk1[:].rearrange("p n e -> p (n e)"), scalar=-1e30,
                                           in1=els[:].rearrange("p n e -> p (n e)"), op0=ALU.mult, op1=ALU.add),
          [mk1b, elsb], [el2b])
        V(lambda v: v.tensor_reduce(out=m2[:], in_=el2[:], axis=AX.X, op=ALU.max), [el2b], [m2b])
        V(lambda v: v.tensor_tensor(out=mk2[:], in0=el2[:], in1=bc(m2[:], 2, [128, NT, 4]), op=ALU.is_equal),
          [el2b, m2b], [mk2b])
        V(lambda v: v.tensor_tensor(out=dd[:], in0=m2[:], in1=m1[:], op=ALU.subtract), [m1b, m2b], [ddb])
        P.op("act", lambda a: a.activation(out=e2[:], in_=dd[:], func=AF.Exp), reads=[ddb], writes=[e2b])
        V(lambda v: v.tensor_scalar(out=dd[:], in0=e2[:], scalar1=1.0, scalar2=None, op0=ALU.add), [e2b], [ddb])
        V(lambda v: v.reciprocal(out=w1[:], in_=dd[:]), [ddb], [w1b])
        V(lambda v: v.tensor_tensor(out=w1[:], in0=w1[:], in1=gprob[:], op=ALU.mult), [w1b, gprobb], [w1b])
        V(lambda v: v.tensor_tensor(out=w2[:], in0=w1[:], in1=e2[:], op=ALU.mult), [w1b, e2b], [w2b])
        V(lambda v: v.tensor_tensor(out=ws[:], in0=mk1[:], in1=bc(w1[:], 2, [128, NT, 4]), op=ALU.mult),
          [mk1b, w1b], [wsb])
        V(lambda v: v.tensor_tensor(out=ws2[:], in0=mk2[:], in1=bc(w2[:], 2, [128, NT, 4]), op=ALU.mult),
          [mk2b, w2b], [ws2b])
        V(lambda v: v.tensor_tensor(out=ws[:], in0=ws[:], in1=ws2[:], op=ALU.add), [wsb, ws2b], [wsb])
        V(lambda v: v.tensor_tensor(out=comb[:], in0=bc(gm[:], 3, [128, NT, 4, 4]),
                                    in1=bc(ws[:], 2, [128, NT, 4, 4]), op=ALU.mult), [gmb, wsb], [combb])
        combf = comb[:].rearrange("p n g e -> p n (g e)")

        aT = [C.sb([128, 4, TB], BF16, f"aT{s}", st) for s in range(2)]
        aTb = [Buf() for _ in range(2)]
        sg = [C.sb([128, TB], BF16, f"sg{s}", st) for s in range(2)]
        sgb = [Buf() for _ in range(2)]
        gu = [(psA[0], 0), (psA[0], 512), (psA[1], 0), (psA[1], 512)]
        gub = [Buf() for _ in range(4)]

        dcnt_ = [0]

        def moe_step(e, tb, a, s):
            if True:
                if True:
                    pass
                tsl = slice(tb * TB, (tb + 1) * TB)
                for c in range(4):
                    gq = (c % 2) * 2
                    (gt, go), (ut, uo) = gu[gq], gu[gq + 1]
                    for kc in range(8):
                        P.op("pe", lambda t, kc=kc, c=c, gt=gt, go=go, s=s, tsl=tsl: t.matmul(
                            out=gt[:, go:go + TB], lhsT=wgs[s][:, kc, c * 128:(c + 1) * 128], rhs=hT[:, kc, tsl],
                            start=(kc == 0), stop=(kc == 7)), reads=[wgb[s]] + hTb[tb * SUB:(tb + 1) * SUB],
                            writes=[gub[gq]])
                    for kc in range(8):
                        P.op("pe", lambda t, kc=kc, c=c, ut=ut, uo=uo, s=s, tsl=tsl: t.matmul(
                            out=ut[:, uo:uo + TB], lhsT=wus[s][:, kc, c * 128:(c + 1) * 128], rhs=hT[:, kc, tsl],
                            start=(kc == 0), stop=(kc == 7)), reads=[wub[s]] + hTb[tb * SUB:(tb + 1) * SUB],
                            writes=[gub[gq + 1]])
                    sj = c % 2
                    P.op("act", lambda ac, gt=gt, go=go, sj=sj: ac.activation(out=sg[sj][:], in_=gt[:, go:go + TB],
                                                                              func=AF.Silu),
                         reads=[gub[gq]], writes=[sgb[sj]])
                    P.op("dve", lambda v, ut=ut, uo=uo, sj=sj, a=a, c=c: v.tensor_tensor(
                        out=aT[a][:, c, :], in0=sg[sj][:], in1=ut[:, uo:uo + TB], op=ALU.mult),
                        reads=[sgb[sj], gub[gq + 1]], writes=[aTb[a]])
                yield
                for ts in range(SUB):
                    i = tb * SUB + ts
                    for nh in range(2):
                        j = dcnt_[0] % 4
                        dcnt_[0] += 1
                        for c in range(4):
                            P.op("pe", lambda t, c=c, j=j, a=a, ts=ts, nh=nh, s=s: t.matmul(
                                out=psB[j][:], lhsT=aT[a][:, c, ts * 128:(ts + 1) * 128],
                                rhs=wds[s][:, c, nh * 512:(nh + 1) * 512], start=(c == 0), stop=(c == 3)),
                                reads=[aTb[a], wdb[s]], writes=[psBb[j]])
                        P.op("dve", lambda v, i=i, nh=nh, j=j, e=e: v.scalar_tensor_tensor(
                            out=x_sb[:, i, nh * 512:(nh + 1) * 512], in0=psB[j][:], scalar=combf[:, i, e:e + 1],
                            in1=x_sb[:, i, nh * 512:(nh + 1) * 512], op0=ALU.mult, op1=ALU.add),
                            reads=[psBb[j], combb, xb[i]], writes=[xb[i]])

        msteps = [(e, tb) for e in range(16) for tb in range(NTB)]
        pend = None
        for k_, (e, tb) in enumerate(msteps):
            g_ = moe_step(e, tb, k_ % 2, e % NS)
            next(g_)
            if pend is not None:
                for _ in pend:
                    pass
            pend = g_
            if tb == 0 and e + 1 < 16:
                load_expert(e + 1)
        for _ in pend:
            pass

    with C.phase() as st:
        hT = C.sb([128, 8, T], BF16, "h2T", st)
        hTb = [Buf() for _ in range(NT)]
        g32, g32b = load_gcol32(C, gple_d, "ple")
        wpg = C.sb([128, 8, 1024], BF16, "wpg", st)
        wpgb = Buf()
        wpp = C.sb([128, 2, 1024], BF16, "wpp", st)
        wppb = Buf()
        P.dma("pool", wpg[:], wpg_d.rearrange("(k p) n -> p k n", p=128), writes=[wpgb])
        P.dma("pool", wpp[:], wpp_d.rearrange("(k p) n -> p k n", p=128), writes=[wppb])
        p_sb = C.sb([128, NT, 256], F32, "p", st)
        pb = Buf()
        P.dma("sp", p_sb[:], p_d.rearrange("(n p) d -> p n d", p=128), writes=[pb])
        pT = C.sb([128, 2, T], BF16, "pT", st)
        pTb = [Buf() for _ in range(NT)]
        rms_to_hT(C, x_sb, xb, NT, g32, g32b, ident, ib, hT, hTb, psA, psAb, tag="p")
        for i in range(NT):
            j = i % 2
            for kc in range(2):
                P.op("pe", lambda t, i=i, kc=kc, j=j: t.transpose(out=psB[j][:, kc * 128:(kc + 1) * 128],
                                                                  in_=p_sb[:, i, kc * 128:(kc + 1) * 128],
                                                                  identity=ident[:]),
                     reads=[pb, ib], writes=[psBb[j]])
            P.op("act", lambda a, i=i, j=j: a.activation(out=pT[:, :, i * 128:(i + 1) * 128],
                                                          in_=psB[j][:, 0:256].rearrange("p (k t) -> p k t", k=2),
                                                          func=AF.Copy), reads=[psBb[j]], writes=[pTb[i]])
        sgp = [C.sb([128, 512], F32, f"sgp{s}", st) for s in range(2)]
        sgpb = [Buf() for _ in range(2)]
        gfin = C.sb([128, 1024], F32, "gfin", st)
        gfinb = Buf()
        if last:
            P.dma("sp", gfin[:], gfin_d.partition_broadcast(128), writes=[gfinb])
        ssf = C.sb([128, NT], F32, "ssf", st)
        rf = C.sb([128, NT], F32, "rf", st)
        junkf = C.sb([128, 1024], BF16, "junkf", st)
        junkfb = Buf()
        ssfb = [Buf() for _ in range(NT)]
        rfb = [Buf() for _ in range(NT)]
        ob = [C.sb([128, 1024], F32, f"ob{s}", st) for s in range(2)]
        obb = [Buf() for _ in range(2)]
        out_v = out_d.rearrange("(n p) d -> p n d", p=128)
        cnt = 0
        for i in range(NT):
            for nh in range(2):
                jg = (cnt % 2)
                jp = 2 + (cnt % 2)
                sj = cnt % 2
                cnt += 1
                for kc in range(8):
                    P.op("pe", lambda t, i=i, kc=kc, nh=nh, jg=jg: t.matmul(
                        out=psB[jg][:], lhsT=hT[:, kc, i * 128:(i + 1) * 128],
                        rhs=wpg[:, kc, nh * 512:(nh + 1) * 512], start=(kc == 0), stop=(kc == 7)),
                        reads=[hTb[i], wpgb], writes=[psBb[jg]])
                for kc in range(2):
                    P.op("pe", lambda t, i=i, kc=kc, nh=nh, jp=jp: t.matmul(
                        out=psB[jp][:], lhsT=pT[:, kc, i * 128:(i + 1) * 128],
                        rhs=wpp[:, kc, nh * 512:(nh + 1) * 512], start=(kc == 0), stop=(kc == 1)),
                        reads=[pTb[i], wppb], writes=[psBb[jp]])
                P.op("act", lambda a, jg=jg, sj=sj: a.activation(out=sgp[sj][:], in_=psB[jg][:], func=AF.Sigmoid),
                     reads=[psBb[jg]], writes=[sgpb[sj]])
                P.op("dve", lambda v, jp=jp, sj=sj: v.tensor_tensor(out=sgp[sj][:], in0=sgp[sj][:], in1=psB[jp][:],
                                                                     op=ALU.mult),
                     reads=[sgpb[sj], psBb[jp]], writes=[sgpb[sj]])
                P.op("dve", lambda v, i=i, nh=nh, sj=sj: v.tensor_tensor(
                    out=x_sb[:, i, nh * 512:(nh + 1) * 512], in0=x_sb[:, i, nh * 512:(nh + 1) * 512],
                    in1=sgp[sj][:], op=ALU.add), reads=[sgpb[sj], xb[i]], writes=[xb[i]])
            if last:
                o = i % 2
                P.op("pool", lambda g, i=i: g.memset(ssf[:, i:i + 1], 0.0), writes=[ssfb[i]])
                P.op("act", lambda a, i=i: a.activation(out=junkf[:], in_=x_sb[:, i, :], func=AF.Square,
                                                         accum_out=ssf[:, i:i + 1]),
                     reads=[xb[i]], writes=[junkfb, ssfb[i]])
                P.op("act", lambda a, i=i: a.activation(out=rf[:, i:i + 1], in_=ssf[:, i:i + 1], func=AF.Sqrt,
                                                         scale=1.0 / 1024.0, bias=EPS),
                     reads=[ssfb[i]], writes=[rfb[i]])
                P.op("dve", lambda v, i=i: v.reciprocal(out=rf[:, i:i + 1], in_=rf[:, i:i + 1]), reads=[rfb[i]],
                     writes=[rfb[i]])
                P.op("dve", lambda v, i=i, o=o: v.scalar_tensor_tensor(
                    out=ob[o][:], in0=x_sb[:, i, :], scalar=rf[:, i:i + 1], in1=gfin[:], op0=ALU.mult,
                    op1=ALU.mult), reads=[xb[i], rfb[i], gfinb], writes=[obb[o]])
                P.dma("sp", out_v[:, i, :], ob[o][:], reads=[obb[o]], is_output=True)
            else:
                P.dma("sp", out_v[:, i, :], x_sb[:, i, :], reads=[xb[i]], writes=out_writes, is_output=out_is_output)
        if standalone:
            P.final_wait()
    if "h_dst" in fz:
        h_rows = fz["h_dst"]
        with C.phase() as st:
            hTn = C.sb([128, 8, T], BF16, "hTn", st)
            hTnb = [Buf() for _ in range(NT)]
            gn, gnb = load_gcol32(C, gnext_d, "nxt")
            rms_to_hT(C, x_sb, xb, NT, gn, gnb, ident, ib, hTn, hTnb, psA, psAb, tag="n")
            for k0 in range(0, 8, 2):
                hap, hB_ = h_rows[k0 // 2]
                P.dma("sp", hap.rearrange("(k p) t -> p k t", p=128), hTn[:, k0:k0 + 2, :], reads=hTnb, writes=[hB_])
    if standalone:
        P.flush()
    return C


def finalize_heads(C, hacc, haccb, gt, gtb, NT, identb, ibb, pbank, outT, outTb, tag):
    P = C.P
    ssq = C.sb([128, NT * 2], F32, "fssq" + tag)
    ssqb = Buf()
    on = C.sb([128, NT, 128], BF16, "fon" + tag)
    onb = Buf()
    P.op("dve", lambda v: v.tensor_tensor(out=on[:], in0=hacc[:], in1=hacc[:], op=ALU.mult), reads=haccb, writes=[onb])
    P.op("dve", lambda v: v.tensor_reduce(out=ssq[:], in_=on[:].rearrange("p n (h e) -> p (n h) e", h=2), axis=AX.X,
                                          op=ALU.add), reads=[onb], writes=[ssqb])
    P.op("act", lambda a: a.activation(out=ssq[:], in_=ssq[:], func=AF.Sqrt, scale=1.0 / 64.0, bias=EPS),
         reads=[ssqb], writes=[ssqb])
    P.op("dve", lambda v: v.reciprocal(out=ssq[:], in_=ssq[:]), reads=[ssqb], writes=[ssqb])
    P.op("dve", lambda v: v.tensor_tensor(out=hacc[:].rearrange("p n (h e) -> p (n h) e", h=2),
                                          in0=hacc[:].rearrange("p n (h e) -> p (n h) e", h=2),
                                          in1=bc(ssq[:], 2, [128, NT * 2, 64]), op=ALU.mult),
         reads=haccb + [ssqb], writes=haccb)
    P.op("dve", lambda v: v.tensor_tensor(out=on[:], in0=hacc[:], in1=gt[:], op=ALU.mult), reads=haccb + gtb,
         writes=[onb])
    GRP = min(4, NT)
    pb1 = Buf()
    for g0 in range(0, NT, GRP):
        for k in range(GRP):
            P.op("pe", lambda t, g0=g0, k=k: t.transpose(out=pbank[:, k * 128:(k + 1) * 128], in_=on[:, g0 + k, :],
                                                         identity=identb[:]),
                 reads=[onb, ibb], writes=[pb1])
        ob_ = Buf()
        outTb.append(ob_)
        P.op("dve", lambda a, g0=g0: a.tensor_copy(out=outT[:, g0 * 128:(g0 + GRP) * 128], in_=pbank[:, 0:GRP * 128]),
             reads=[pb1], writes=[ob_])


def build_k1(T, phases="AGM", stop_at=None, C=None, sfx="", hsrc=None, mix_dst=None, after_A=None):
    standalone = C is None
    if C is None:
        C = Ctx()
        C.P.stop_at = stop_at
    nc, P = C.nc, C.P
    NT = T // 128
    NCH = T // 64
    TB = min(512, T)
    NTB = T // TB
    SUB = TB // 128

    if hsrc is None:
        x_d = C.din("x" + sfx, [T, 1024], F32)
        gmix_d = C.din("norm_mix_g" + sfx, [1024], F32)
    watt_d = C.din("w_att" + sfx, [1024, 448], F32)
    wglaf_d = C.din("w_gla_f" + sfx, [1024, 256], F32)
    wglat_d = C.din("w_gla_t" + sfx, [1024, 384], F32)
    wglr_d = C.din("w_glr" + sfx, [1024, 32], F32)
    wdb_d = C.din("wdb" + sfx, [2, 17, 128], F32)
    wmlf_d = C.din("w_ml_f" + sfx, [1024, 256], F32)
    wmlt_d = C.din("w_ml_t" + sfx, [1024, 264], F32)
    cw_d = C.din("cw" + sfx, [256, 4], F32)
    gatesb_d = C.din("gates_b" + sfx, [8], F32)
    g6_d = C.din("g6" + sfx, [384], F32)
    glag_d = C.din("gla_g" + sfx, [128], F32)
    mlg_d = C.din("ml_g" + sfx, [128], F32)
    rope_d = C.din("rope", [T, 128], F32)
    cf_d = C.din("cf", [2, 128, 128], F32)
    cs_d = C.din("cs", [6, 128, 128], F32)
    hm_d = C.din("hm", [128, 2], F32)
    if mix_dst is None:
        mixT_d = C.dout("mixT", [512, T], BF16)
        mix_rows = [(mixT_d[k * 128:(k + 1) * 128, :], []) for k in range(4)]
        mix_is_output = True
    else:
        mix_rows = [(ap_, [b_]) for ap_, b_ in mix_dst]
        mix_is_output = False

    hT = C.sb([128, 8, T], BF16, "hT")
    hTb = [Buf() for _ in range(NT)]
    ident, ib = load_ident(C)
    identb, ibb = load_ident(C, BF16)
    cf = C.sb([128, 2, 128], F32, "cf")
    cfb = Buf()
    P.dma("sp", cf[:], cf_d.rearrange("c p n -> p c n"), writes=[cfb])
    ones = C.sb([128, 128], F32, "ones")
    onesb = Buf()
    P.op("pool", lambda g: g.memset(ones[:], 1.0), writes=[onesb])

    def hT_blk(tb):
        return hTb[tb * SUB:(tb + 1) * SUB]

    if hsrc is not None:
        T2_ = T // 2
        for r_ in range(2):
            for k0 in range(0, 8, 2):
                hap, hgB = hsrc[(r_, k0)]
                P.dma("sp", hT[:, k0:k0 + 2, r_ * T2_:(r_ + 1) * T2_], hap.rearrange("(k p) t -> p k t", p=128),
                      reads=[hgB], writes=hTb[r_ * (NT // 2):(r_ + 1) * (NT // 2)])
    for _once in ([] if hsrc is not None else [0]):
      with C.phase():
        PS = [C.sb, None]
        ps2 = [C.ps([128, 1024], F32, f"p0_{j}") for j in range(2)]
        ps2b = [Buf() for _ in range(2)]
        g, gb = load_gcol32(C, gmix_d, "mix")
        xg = [C.sb([128, 4, 1024], F32, f"xg{s}") for s in range(2)]
        xgb = [[Buf() for _ in range(4)] for _ in range(2)]
        ss = C.sb([128, NT], F32, "ss")
        r = C.sb([128, NT], F32, "r")
        ssb = [Buf() for _ in range(NT)]
        rb = [Buf() for _ in range(NT)]
        junk = C.sb([128, 1024], BF16, "junk")
        junkb = Buf()
        xs = [C.sb([128, 1024], F32, f"xs{j}") for j in range(2)]
        xsb = [Buf() for _ in range(2)]
        x_v = x_d.rearrange("(n p) d -> p n d", p=128)
        P.op("pool", lambda g_: g_.memset(ss[:], 0.0), writes=ssb)
        def p0_tile(i, j):
            gi, k = divmod(i, 4)
            s = gi % 2
            if k == 0:
                n = min(4, NT - i)
                P.dma("sp", xg[s][:, 0:n, :], x_v[:, i:i + n, :], writes=xgb[s][0:n])
            P.op("act", lambda a, i=i, s=s, k=k: a.activation(out=junk[:], in_=xg[s][:, k, :], func=AF.Square,
                                                              accum_out=ss[:, i:i + 1]),
                 reads=[xgb[s][k]], writes=[junkb, ssb[i]])
            yield
            P.op("act", lambda a, i=i: a.activation(out=r[:, i:i + 1], in_=ss[:, i:i + 1], func=AF.Sqrt,
                                                     scale=1.0 / 1024.0, bias=EPS), reads=[ssb[i]], writes=[rb[i]])
            yield
            P.op("dve", lambda v, i=i: v.reciprocal(out=r[:, i:i + 1], in_=r[:, i:i + 1]), reads=[rb[i]],
                 writes=[rb[i]])
            yield
            P.op("act", lambda a, i=i, s=s, k=k, j=j: a.activation(out=xs[j][:], in_=xg[s][:, k, :], func=AF.Copy,
                                                                   scale=r[:, i:i + 1]),
                 reads=[xgb[s][k], rb[i]], writes=[xsb[j]])
            yield
            for kc in range(8):
                P.op("pe", lambda t, j=j, kc=kc: t.transpose(out=ps2[j][:, kc * 128:(kc + 1) * 128],
                                                             in_=xs[j][:, kc * 128:(kc + 1) * 128],
                                                             identity=ident[:]),
                     reads=[xsb[j], ib], writes=[ps2b[j]])
            yield
            P.op("dve", lambda v, i=i, j=j: v.tensor_tensor(out=hT[:, :, i * 128:(i + 1) * 128],
                                                             in0=ps2[j][:].rearrange("p (k t) -> p k t", k=8),
                                                             in1=bc(g[:], 2, [128, 8, 128]), op=ALU.mult),
                 reads=[ps2b[j], gb], writes=[hTb[i]])
        for i0 in range(0, NT, 2):
            gens = [p0_tile(i0 + k_, k_) for k_ in range(min(2, NT - i0))]
            live = [True] * len(gens)
            while any(live):
                for k_ in range(len(gens)):
                    if live[k_]:
                        try:
                            next(gens[k_])
                        except StopIteration:
                            live[k_] = False

    def psum_banks(n, dt=F32, cols=512):
        ts = [C.cur.enter_context(nc.psum_tensor(f"pb{C._n}_{k}", [128, cols], dt)) for k in range(n)]
        C._n += 1
        return ts, [Buf() for _ in range(n)]

    if "A" in phases:
        with C.phase():
            watt = C.sb([128, 8, 448], BF16, "watt")
            wattb = Buf()
            P.dma("pool", watt[:], watt_d.rearrange("(k p) n -> p k n", p=128), writes=[wattb])
            rope = C.sb([128, NT, 128], F32, "rope")
            ropeb = Buf()
            P.dma("sp", rope[:], rope_d.rearrange("(n p) c -> p n c", p=128), writes=[ropeb])
            g6 = C.sb([128, 384], F32, "g6")
            g6b = Buf()
            P.dma("sp", g6[:], g6_d.partition_broadcast(128), writes=[g6b])
            qkT = C.sb([128, 3, T], BF16, "qkT")
            qkTb = [Buf() for _ in range(NT)]
            va0 = C.sb([128, NT, 65], BF16, "va0")
            va1 = C.sb([128, NT, 128], BF16, "va1")
            vab = [Buf() for _ in range(NT)]
            mixA = C.sb([128, 2, T], BF16, "mixA")
            mixAb = Buf()
            P.op("pool", lambda g_: g_.memset(va0[:], 1.0), writes=vab)
            P.op("pool", lambda g_: g_.memset(va1[:], 0.0), writes=vab)
            P.op("pool", lambda g_: g_.memset(va1[:, :, 0:1], 1.0), writes=vab)
            amx = C.sb([128, 2], F32, "amx")
            amxb = Buf()
            nb = C.sb([128, 1], F32, "nb")
            nbb = Buf()
            P.op("dve", lambda v: v.tensor_reduce(out=amx[:, 0:1], in_=g6[:, 0:64], axis=AX.X, op=ALU.max,
                                                  apply_absolute_value=True), reads=[g6b], writes=[amxb])
            P.op("dve", lambda v: v.tensor_reduce(out=amx[:, 1:2], in_=g6[:, 256:320], axis=AX.X, op=ALU.max,
                                                  apply_absolute_value=True), reads=[g6b, amxb], writes=[amxb])
            P.op("dve", lambda v: v.tensor_tensor(out=nb[:], in0=amx[:, 0:1], in1=amx[:, 1:2], op=ALU.mult),
                 reads=[amxb], writes=[nbb])
            P.op("dve", lambda v: v.tensor_scalar(out=nb[:], in0=nb[:], scalar1=-8.0, scalar2=None, op0=ALU.mult),
                 reads=[nbb], writes=[nbb])

            _projph = C.phase()
            _projph.__enter__()
            zps, zpsb = psum_banks(2)
            tps, tpsb = psum_banks(1, BF16, 1024)

            def T2(nm, shape, dt=F32):
                return [C.sb(shape, dt, f"{nm}{j}") for j in range(2)], [Buf() for _ in range(2)]
            zsb_, zsbb = T2("zsb", [128, 384])
            sq_, sqb_ = T2("asq", [128, 384])
            ssq_, ssqb_ = T2("assq", [128, 6])
            qn_, qnb_ = T2("qn", [128, 384])
            t1_, t1b_ = T2("t1", [128, 384])
            t2_, t2b_ = T2("t2", [128, 384])
            qr_, qrb_ = T2("qr", [128, 384], BF16)
            def a_tile(i, j):
                for kc in range(8):
                    P.op("pe", lambda t, i=i, kc=kc, j=j: t.matmul(out=zps[j][:, 0:448], lhsT=hT[:, kc, i * 128:(i + 1) * 128],
                                                                  rhs=watt[:, kc, :], start=(kc == 0), stop=(kc == 7)),
                         reads=[hTb[i], wattb], writes=[zpsb[j]])
                yield
                P.op("act", lambda a, j=j: a.activation(out=zsb_[j][:], in_=zps[j][:, 0:384], func=AF.Copy),
                     reads=[zpsb[j]], writes=[zsbb[j]])
                yield
                P.op("act", lambda a, i=i, j=j: a.activation(out=va0[:, i, 0:64], in_=zps[j][:, 384:448], func=AF.Copy),
                     reads=[zpsb[j]], writes=[vab[i]])
                yield
                P.op("act", lambda a, i=i, j=j: a.activation(out=va1[:, i, 64:128], in_=zps[j][:, 384:448], func=AF.Copy),
                     reads=[zpsb[j]], writes=[vab[i]])
                yield
                P.op("dve", lambda v, j=j: v.tensor_tensor(out=sq_[j][:], in0=zsb_[j][:], in1=zsb_[j][:], op=ALU.mult),
                     reads=[zsbb[j]], writes=[sqb_[j]])
                yield
                P.op("dve", lambda v, j=j: v.tensor_reduce(out=ssq_[j][:], in_=sq_[j][:].rearrange("p (h e) -> p h e", h=6),
                                                            axis=AX.X, op=ALU.add), reads=[sqb_[j]], writes=[ssqb_[j]])
                yield
                P.op("act", lambda a, j=j: a.activation(out=ssq_[j][:], in_=ssq_[j][:], func=AF.Sqrt, scale=1.0 / 64.0,
                                                         bias=EPS), reads=[ssqb_[j]], writes=[ssqb_[j]])
                yield
                P.op("dve", lambda v, j=j: v.reciprocal(out=ssq_[j][:], in_=ssq_[j][:]), reads=[ssqb_[j]],
                     writes=[ssqb_[j]])
                yield
                P.op("dve", lambda v, j=j: v.tensor_tensor(out=qn_[j][:].rearrange("p (h e) -> p h e", h=6),
                                                            in0=zsb_[j][:].rearrange("p (h e) -> p h e", h=6),
                                                            in1=bc(ssq_[j][:], 2, [128, 6, 64]), op=ALU.mult),
                     reads=[zsbb[j], ssqb_[j]], writes=[qnb_[j]])
                yield
                P.op("dve", lambda v, j=j: v.tensor_tensor(out=qn_[j][:], in0=qn_[j][:], in1=g6[:], op=ALU.mult),
                     reads=[qnb_[j], g6b], writes=[qnb_[j]])
                yield
                P.op("dve", lambda v, i=i, j=j: v.tensor_tensor(out=t1_[j][:].rearrange("p (h e) -> p h e", h=6),
                                                                 in0=qn_[j][:].rearrange("p (h e) -> p h e", h=6),
                                                                 in1=bc(rope[:, i, 0:64], 1, [128, 6, 64]), op=ALU.mult),
                     reads=[qnb_[j], ropeb], writes=[t1b_[j]])
                yield
                for w in range(2):
                    P.op("dve", lambda v, i=i, j=j, w=w: v.tensor_tensor(
                        out=t2_[j][:].rearrange("p (h a w e) -> p h a w e", h=6, a=2, w=2)[:, :, :, w, :],
                        in0=qn_[j][:].rearrange("p (h a w e) -> p h a w e", h=6, a=2, w=2)[:, :, :, 1 - w, :],
                        in1=bc(rope[:, i, 64:128].rearrange("p (a w e) -> p a w e", a=2, w=2)[:, :, w, :], 1,
                               [128, 6, 2, 16]), op=ALU.mult),
                        reads=[qnb_[j], ropeb], writes=[t2b_[j]])
                yield
                P.op("dve", lambda v, j=j: v.tensor_tensor(out=qr_[j][:], in0=t1_[j][:], in1=t2_[j][:], op=ALU.add),
                     reads=[t1b_[j], t2b_[j]], writes=[qrb_[j]])
                yield
                for k in range(3):
                    P.op("pe", lambda t, j=j, k=k: t.transpose(out=tps[0][:, k * 128:(k + 1) * 128],
                                                               in_=qr_[j][:, k * 128:(k + 1) * 128], identity=identb[:]),
                         reads=[qrb_[j], ibb], writes=[tpsb[0]])
                P.op("act", lambda a, i=i: a.activation(out=qkT[:, :, i * 128:(i + 1) * 128],
                                                         in_=tps[0][:, 0:384].rearrange("p (k t) -> p k t", k=3),
                                                         func=AF.Copy), reads=[tpsb[0]], writes=[qkTb[i]])

            for i0 in range(0, NT, 2):
                gens = [a_tile(i0 + k_, k_) for k_ in range(min(2, NT - i0))]
                live = [True] * len(gens)
                while any(live):
                    for k_ in range(len(gens)):
                        if live[k_]:
                            try:
                                next(gens[k_])
                            except StopIteration:
                                live[k_] = False

            _projph.__exit__(None, None, None)
            sps, spsb = psum_banks(6)
            ops_, opsb = psum_banks(2)
            NSL = 6
            pe_ = [C.sb([128, TB], BF16, f"pexp{s}") for s in range(NSL)]
            peb = [Buf() for _ in range(NSL)]
            denr = C.sb([128, TB], F32, "denr")
            denrb = Buf()
            bcs = C.sb([128, TB], F32, "bcs")
            bcsb = Buf()
            slots = [(sps[k_], spsb[k_]) for k_ in range(NSL)]
            steps = [(pr, qb, kt) for pr in range(2) for qb in range(NTB) for kt in range(NT)]

            def emit_S(j):
                pr, qb, kt = steps[j]
                qsl = slice(qb * TB, (qb + 1) * TB)
                for hh in range(2):
                    s = (2 * j + hh) % NSL
                    rows = slice(hh * 64, (hh + 1) * 64)
                    P.op("pe", lambda t, s=s, rows=rows: t.matmul(out=slots[s][0][:, 0:TB],
                                                                  lhsT=qkT[rows, 2, kt * 128:(kt + 1) * 128],
                                                                  rhs=qkT[rows, pr, qsl], start=True, stop=True),
                         reads=[qkTb[kt]] + qkTb[qb * SUB:(qb + 1) * SUB], writes=[slots[s][1]])
                for hh in range(2):
                    s = (2 * j + hh) % NSL
                    P.op("act", lambda a, s=s: a.activation(out=pe_[s][:], in_=slots[s][0][:, 0:TB], func=AF.Exp,
                                                            bias=nb[:, 0:1], scale=0.125),
                         reads=[slots[s][1], nbb], writes=[peb[s]])

            def emit_O(j):
                pr, qb, kt = steps[j]
                qsl = slice(qb * TB, (qb + 1) * TB)
                s0, s1 = (2 * j) % NSL, (2 * j + 1) % NSL
                P.op("pe", lambda t: t.matmul(out=ops_[0][0:65, 0:TB], lhsT=va0[:, kt, :], rhs=pe_[s0][:],
                                              start=(kt == 0), stop=(kt == NT - 1)),
                     reads=[vab[kt], peb[s0]], writes=[opsb[0]])
                P.op("pe", lambda t: t.matmul(out=ops_[1][:, 0:TB], lhsT=va1[:, kt, :], rhs=pe_[s1][:],
                                              start=(kt == 0), stop=(kt == NT - 1)),
                     reads=[vab[kt], peb[s1]], writes=[opsb[1]])
                if kt == NT - 1:
                    for h2 in range(2):
                        fin(pr, qsl, h2, (2 * j + h2) % NSL)

            def fin(pr, qsl, hh, fs):
                bk, bkb = slots[fs]
                dr = slice(64, 65) if hh == 0 else slice(0, 1)
                rows = slice(hh * 64, (hh + 1) * 64)
                P.op("dve", lambda v: v.reciprocal(out=denr[dr, :], in_=ops_[hh][dr, 0:TB]),
                     reads=[opsb[hh]], writes=[denrb])
                if hh == 0:
                    P.op("pe", lambda t: t.matmul(out=bk[0:64, 0:TB], lhsT=ones[dr, 0:64], rhs=denr[dr, :],
                                                  start=True, stop=True), reads=[denrb, onesb], writes=[bkb])
                else:
                    P.op("pe", lambda t: t.matmul(out=bk[:, 0:TB], lhsT=ones[dr, :], rhs=denr[dr, :],
                                                  start=True, stop=True), reads=[denrb, onesb], writes=[bkb])
                P.op("dve", lambda v: v.tensor_copy(out=bcs[rows, :], in_=bk[rows, 0:TB]),
                     reads=[bkb], writes=[bcsb])
                P.op("dve", lambda v: v.tensor_tensor(out=mixA[rows, pr, qsl], in0=ops_[hh][rows, 0:TB],
                                                      in1=bcs[rows, :], op=ALU.mult),
                     reads=[opsb[hh], bcsb], writes=[mixAb])

            LOOKP = 2
            for j in range(len(steps) + LOOKP):
                if j < len(steps):
                    emit_S(j)
                if j >= LOOKP:
                    emit_O(j - LOOKP)
            for pr in range(2):
                P.dma("sp", mix_rows[pr][0], mixA[:, pr, :], reads=[mixAb], writes=mix_rows[pr][1], is_output=mix_is_output)

    if after_A is not None:
        after_A()
    if "G" in phases:
        with C.phase():
            pz, pzb = psum_banks(2)
            pc, pcb = psum_banks(2)
            pa, pab = psum_banks(2)
            C._n += 1
            po_ctx = [nc.psum_tensor(f"po{C._n}_{k}", [128, 512], F32) for k in range(2)]
            po = [c_.__enter__() for c_ in po_ctx]
            cs = C.sb([128, 6, 128], BF16, "cs")
            csb = Buf()
            P.dma("pool", cs[:], cs_d.rearrange("c p n -> p c n"), writes=[csb])
            wf = C.sb([128, 8, 256], BF16, "wglaf")
            wfb = Buf()
            P.dma("pool", wf[:], wglaf_d.rearrange("(k p) n -> p k n", p=128), writes=[wfb])
            wt = C.sb([128, 8, 384], BF16, "wglat")
            wtb = Buf()
            P.dma("pool", wt[:], wglat_d.rearrange("(k p) n -> p k n", p=128), writes=[wtb])
            wl = C.sb([128, 8, 32], BF16, "wglr")
            wlb = Buf()
            P.dma("pool", wl[:], wglr_d.rearrange("(k p) n -> p k n", p=128), writes=[wlb])
            wdb = C.sb([17, 2, 128], F32, "wdb")
            wdbb = Buf()
            P.dma("sp", wdb[:], wdb_d.rearrange("d r n -> r d n"), writes=[wdbb])
            gg_ = C.sb([128, 128], F32, "glag")
            ggb = Buf()
            P.dma("sp", gg_[:], glag_d.partition_broadcast(128), writes=[ggb])
            qgT = C.sb([128, T], BF16, "qgT")
            kgT = C.sb([128, T], BF16, "kgT")
            qgTb = [Buf() for _ in range(NTB)]
            kgTb = [Buf() for _ in range(NTB)]
            ktm = C.sb([128, NT, 128], BF16, "ktm")
            vtm = C.sb([128, NT, 128], BF16, "vtm")
            gate = C.sb([128, NT, 128], F32, "ggate")
            tmb = [Buf() for _ in range(NT)]
            la = [C.sb([128, NT, 128], BF16, f"la{d}") for d in range(2)]
            lab = [[Buf() for _ in range(NT)] for _ in range(2)]
            lrT = [C.sb([17, TB], F32, f"lrT{d}") for d in range(2)]
            lrTb = [Buf() for _ in range(2)]
            oacc = C.sb([128, NT, 128], F32, "oacc")
            oaccb = [Buf() for _ in range(NT)]
            mixG = C.sb([128, T], BF16, "mixG")
            mixGb = []
            for d in range(2):
                P.op("pool", lambda g_, d=d: g_.memset(lrT[d][:], 1.0), writes=[lrTb[d]])
            etmp = C.sb([128, 128], F32, "etmp")
            etmpb = Buf()
            for tb in range(NTB):
                tsl = slice(tb * TB, (tb + 1) * TB)
                for qk in range(2):
                    j = qk
                    for kc in range(8):
                        P.op("pe", lambda t, kc=kc, qk=qk, j=j, tsl=tsl: t.matmul(
                            out=pz[j][:, 0:TB], lhsT=wf[:, kc, qk * 128:(qk + 1) * 128], rhs=hT[:, kc, tsl],
                            start=(kc == 0), stop=(kc == 7)), reads=[wfb] + hT_blk(tb), writes=[pzb[j]])
                    if qk == 0:
                        P.op("act", lambda a, j=j, tsl=tsl: a.activation(out=qgT[:, tsl], in_=pz[j][:, 0:TB], func=AF.Copy,
                                                                         scale=0.125), reads=[pzb[j]], writes=[qgTb[tb]])
                    else:
                        P.op("act", lambda a, j=j, tsl=tsl: a.activation(out=kgT[:, tsl], in_=pz[j][:, 0:TB], func=AF.Copy),
                             reads=[pzb[j]], writes=[kgTb[tb]])
                for d in range(2):
                    j = d
                    for kc in range(8):
                        P.op("pe", lambda t, kc=kc, d=d, j=j, tsl=tsl: t.matmul(
                            out=pc[j][0:16, 0:TB], lhsT=wl[:, kc, d * 16:(d + 1) * 16], rhs=hT[:, kc, tsl],
                            start=(kc == 0), stop=(kc == 7)), reads=[wlb] + hT_blk(tb), writes=[pcb[j]])
                    P.op("dve", lambda v, d=d, j=j: v.tensor_copy(out=lrT[d][0:16, :], in_=pc[j][0:16, 0:TB]),
                         reads=[pcb[j]], writes=[lrTb[d]])
                for ts in range(SUB):
                    i = tb * SUB + ts
                    j = i % 2
                    for kc in range(8):
                        P.op("pe", lambda t, i=i, kc=kc, j=j: t.matmul(out=pz[j][:, 0:384], lhsT=hT[:, kc, i * 128:(i + 1) * 128],
                                                                      rhs=wt[:, kc, :], start=(kc == 0), stop=(kc == 7)),
                             reads=[hTb[i], wtb], writes=[pzb[j]])
                    P.op("act", lambda a, i=i, j=j: a.activation(out=ktm[:, i, :], in_=pz[j][:, 0:128], func=AF.Copy),
                         reads=[pzb[j]], writes=[tmb[i]])
                    P.op("act", lambda a, i=i, j=j: a.activation(out=vtm[:, i, :], in_=pz[j][:, 128:256], func=AF.Copy),
                         reads=[pzb[j]], writes=[tmb[i]])
                    P.op("act", lambda a, i=i, j=j: a.activation(out=gate[:, i, :], in_=pz[j][:, 256:384], func=AF.Silu),
                         reads=[pzb[j]], writes=[tmb[i]])
                    P.op("dve", lambda v, i=i: v.tensor_tensor(out=gate[:, i, :], in0=gate[:, i, :], in1=gg_[:], op=ALU.mult),
                         reads=[tmb[i], ggb], writes=[tmb[i]])
                    for d in range(2):
                        jj = d
                        P.op("pe", lambda t, d=d, jj=jj, ts=ts: t.matmul(out=pa[jj][:, 0:128],
                                                                        lhsT=lrT[d][0:17, ts * 128:(ts + 1) * 128],
                                                                        rhs=wdb[0:17, d, :], start=True, stop=True),
                             reads=[lrTb[d], wdbb], writes=[pab[jj]])
                        P.op("act", lambda a, jj=jj: a.activation(out=etmp[:], in_=pa[jj][:, 0:128], func=AF.Exp, scale=-1.0),
                             reads=[pab[jj]], writes=[etmpb])
                        P.op("act", lambda a, d=d, i=i: a.activation(out=la[d][:, i, :], in_=etmp[:], func=AF.Ln, bias=1.0),
                             reads=[etmpb], writes=[lab[d][i]])

            def G2(nm, shape, dt):
                return [C.sb(shape, dt, f"{nm}{j}") for j in range(2)], [Buf() for _ in range(2)]
            ebm_, ebmb = G2("ebm", [128, 128], F32)
            enbm_, enbmb = G2("enbm", [128, 128], F32)
            eb_, ebb = G2("eb", [128, 128], F32)
            ebl_, eblb = G2("ebl", [128, 128], F32)
            qd_, qdb = G2("qd", [128, 128], BF16)
            kd_, kdb = G2("kd", [128, 128], BF16)
            qbz_, qbzb = G2("qbz", [128, 2, 128], BF16)
            kl_, klb = G2("kl", [128, 128], BF16)
            at_, atb = G2("at", [128, 2, 128], BF16)
            for j in range(2):
                P.op("pool", lambda g_, j=j: g_.memset(qbz_[j][:], 0.0), writes=[qbzb[j]])
            S32d = [C.sb([128, 128], F32, f"S32_{d}") for d in range(2)]
            S32db = [Buf() for _ in range(2)]
            Sbfd = [[C.sb([128, 128], BF16, f"Sbf{d}_{j}") for j in range(2)] for d in range(2)]
            Sbfdb = [[Buf() for _ in range(2)] for _ in range(2)]
            for d in range(2):
                P.op("pool", lambda g_, d=d: g_.memset(S32d[d][:], 0.0), writes=[S32db[d]])
                for j in range(2):
                    P.op("pool", lambda g_, d=d, j=j: g_.memset(Sbfd[d][j][:], 0.0), writes=[Sbfdb[d][j]])
            it = 0
            scurd = [0, 0]
            seen = set()
            order = []
            for st_ in range(NT):
                order += [(0, st_), (1, NT - 1 - st_)]
            def g_body(d, i, j):
                chunks = (0, 1) if d == 0 else (1, 0)
                for _one in (0,):
                    tb = i // SUB
                    tsl = slice(i * 128, (i + 1) * 128)
                    P.op("pe", lambda t, d=d, i=i, j=j: t.matmul(out=pc[j][:, 0:128], lhsT=la[d][:, i, :], rhs=cs[:, 2 + d, :],
                                                                start=True, stop=True), reads=[lab[d][i], csb], writes=[pcb[j]])
                    P.op("pe", lambda t, d=d, i=i, j=j: t.matmul(out=pc[j][:, 128:256], lhsT=la[d][:, i, :], rhs=cs[:, 0 + d, :],
                                                                start=True, stop=True), reads=[lab[d][i], csb], writes=[pcb[j]])
                    P.op("pe", lambda t, d=d, i=i, j=j: t.matmul(out=pc[j][:, 256:384], lhsT=cs[:, 4 + d, :], rhs=la[d][:, i, :],
                                                                start=True, stop=True), reads=[lab[d][i], csb], writes=[pcb[j]])
                    yield
                    P.op("act", lambda a, j=j: a.activation(out=ebm_[j][:], in_=pc[j][:, 0:128], func=AF.Exp), reads=[pcb[j]],
                         writes=[ebmb[j]])
                    P.op("act", lambda a, j=j: a.activation(out=enbm_[j][:], in_=pc[j][:, 0:128], func=AF.Exp, scale=-1.0),
                         reads=[pcb[j]], writes=[enbmb[j]])
                    P.op("act", lambda a, j=j: a.activation(out=eb_[j][:], in_=pc[j][:, 128:256], func=AF.Exp), reads=[pcb[j]],
                         writes=[ebb[j]])
                    P.op("act", lambda a, j=j: a.activation(out=ebl_[j][:], in_=pc[j][:, 256:384], func=AF.Exp), reads=[pcb[j]],
                         writes=[eblb[j]])
                    yield
                    P.op("dve", lambda v, j=j, tsl=tsl: v.tensor_tensor(out=qd_[j][:], in0=qgT[:, tsl], in1=ebm_[j][:], op=ALU.mult),
                         reads=[qgTb[tb], ebmb[j]], writes=[qdb[j]])
                    P.op("dve", lambda v, j=j, tsl=tsl: v.tensor_tensor(out=kd_[j][:], in0=kgT[:, tsl], in1=enbm_[j][:], op=ALU.mult),
                         reads=[kgTb[tb], enbmb[j]], writes=[kdb[j]])
                    for c in range(2):
                        P.op("dve", lambda v, j=j, c=c, i=i: v.tensor_tensor(
                            out=qbz_[j][:, c, c * 64:(c + 1) * 64], in0=qgT[:, i * 128 + c * 64:i * 128 + (c + 1) * 64],
                            in1=eb_[j][:, c * 64:(c + 1) * 64], op=ALU.mult), reads=[qgTb[tb], ebb[j]], writes=[qbzb[j]])
                    P.op("dve", lambda v, j=j, i=i: v.tensor_tensor(out=kl_[j][:], in0=ktm[:, i, :], in1=ebl_[j][:], op=ALU.mult),
                         reads=[tmb[i], eblb[j]], writes=[klb[j]])
                    yield
                    S32, S32b, Sbf, Sbfb, scur = S32d[d], S32db[d], Sbfd[d], Sbfdb[d], scurd[d]
                    rb_ = [(pa[j], pab[j]), (pz[j], pzb[j])]
                    for h in range(2):
                        rows = slice(h * 64, (h + 1) * 64)
                        P.op("pe", lambda t, j=j, h=h, rows=rows, rb_=rb_: t.matmul(out=rb_[h][0][:, 0:128], lhsT=kd_[j][rows, :],
                                                                           rhs=qd_[j][rows, :], start=True, stop=True),
                             reads=[kdb[j], qdb[j]], writes=[rb_[h][1]])
                        P.op("dve", lambda v, j=j, d=d, h=h, rb_=rb_: v.tensor_tensor(out=at_[j][:, h, :], in0=rb_[h][0][:, 0:128],
                                                                         in1=cf[:, d, :], op=ALU.mult),
                             reads=[rb_[h][1], cfb], writes=[atb[j]])
                    for c in range(2):
                        crow = slice(c * 64, (c + 1) * 64)
                        P.op("pe", lambda t, j=j, c=c, crow=crow, i=i, rb_=rb_: t.matmul(out=rb_[c][0][:, 128:256],
                                                                                lhsT=kl_[j][crow, :], rhs=vtm[crow, i, :],
                                                                                start=True, stop=True),
                             reads=[klb[j], tmb[i]], writes=[rb_[c][1]])
                    yield
                    first = True
                    for c in chunks:
                        P.op("pe", lambda t, j=j, c=c, scur=scur, first=first, Sbf=Sbf: t.matmul(
                            out=po[j][:, 0:128], lhsT=qbz_[j][:, c, :], rhs=Sbf[scur][:], start=first, stop=False),
                            reads=[qbzb[j], Sbfb[scur]], writes=[poB[j]])
                        yield
                        first = False
                        dcol = (c * 64 + 63) if d == 0 else (c * 64)
                        for h in range(2):
                            rows = slice(h * 64, (h + 1) * 64)
                            P.op("dve", lambda v, j=j, c=c, h=h, rows=rows, dcol=dcol, rb_=rb_, S32=S32: v.scalar_tensor_tensor(
                                out=S32[rows, h * 64:(h + 1) * 64], in0=S32[rows, h * 64:(h + 1) * 64],
                                scalar=eb_[j][rows, dcol:dcol + 1],
                                in1=rb_[c][0][rows, 128 + h * 64:128 + (h + 1) * 64], op0=ALU.mult, op1=ALU.add),
                                reads=[S32b, ebb[j], rb_[c][1]], writes=[S32b])
                        yield
                        scur = 1 - scur
                        P.op("pool", lambda g_, scur=scur, Sbf=Sbf, S32=S32: g_.tensor_copy(out=Sbf[scur][:], in_=S32[:]),
                             reads=[S32b], writes=[Sbfb[scur]])
                        yield
                    for h in range(2):
                        P.op("pe", lambda t, j=j, h=h, i=i: t.matmul(out=po[j][:, h * 64:(h + 1) * 64], lhsT=at_[j][:, h, :],
                                                                     rhs=vtm[:, i, h * 64:(h + 1) * 64], start=False, stop=(h == 1)),
                             reads=[atb[j], tmb[i]], writes=[poB[j]])
                    yield
                    if i not in seen:
                        seen.add(i)
                        P.op("act", lambda a, i=i, j=j: a.activation(out=oacc[:, i, :], in_=po[j][:, 0:128], func=AF.Copy),
                             reads=[poB[j]], writes=[oaccb[i]])
                    else:
                        P.op("dve", lambda v, i=i, j=j: v.tensor_tensor(out=oacc[:, i, :], in0=oacc[:, i, :], in1=po[j][:, 0:128],
                                                                    op=ALU.add), reads=[poB[j], oaccb[i]], writes=[oaccb[i]])
                    scurd[d] = scur

            poB = [Buf(), Buf()]
            for p_ in range(NT):
                gens = [g_body(order[2 * p_][0], order[2 * p_][1], 0), g_body(order[2 * p_ + 1][0], order[2 * p_ + 1][1], 1)]
                live = [True, True]
                while any(live):
                    for k_ in range(2):
                        if live[k_]:
                            try:
                                next(gens[k_])
                            except StopIteration:
                                live[k_] = False
            P.barrier()
            P.flush()
            for c_ in reversed(po_ctx):
                c_.__exit__(None, None, None)
            pt, ptb = psum_banks(1, BF16, 1024)
            finalize_heads(C, oacc, oaccb, gate, tmb, NT, identb, ibb, pt[0], mixG, mixGb, "g")
            P.dma("sp", mix_rows[2][0], mixG[:], reads=mixGb, writes=mix_rows[2][1], is_output=mix_is_output)

    if "M" in phases:
        with C.phase():
            pz, pzb = psum_banks(2)
            prp, prpb = psum_banks(1)
            pst, pstb = psum_banks(2)
            pk, pkb = psum_banks(1)
            C._n += 1
            pt_ctx0 = nc.psum_tensor(f"ptm{C._n}", [128, 1024], BF16)
            pt = [pt_ctx0.__enter__()]
            wmf = C.sb([128, 8, 256], BF16, "wmlf")
            wmfb = Buf()
            P.dma("pool", wmf[:], wmlf_d.rearrange("(k p) n -> p k n", p=128), writes=[wmfb])
            wmt = C.sb([128, 8, 264], BF16, "wmlt")
            wmtb = Buf()
            P.dma("pool", wmt[:], wmlt_d.rearrange("(k p) n -> p k n", p=128), writes=[wmtb])
            cw = C.sb([128, 2, 4], F32, "cw")
            cwb = Buf()
            P.dma("sp", cw[:], cw_d.rearrange("(a p) c -> p a c", p=128), writes=[cwb])
            gb8 = C.sb([128, 8], F32, "gb8")
            gb8b = Buf()
            P.dma("sp", gb8[:], gatesb_d.partition_broadcast(128), writes=[gb8b])
            mlg = C.sb([128, 128], F32, "mlg")
            mlgb = Buf()
            P.dma("sp", mlg[:], mlg_d.partition_broadcast(128), writes=[mlgb])
            hm = C.sb([128, 2], F32, "hm")
            hmb = Buf()
            P.dma("sp", hm[:], hm_d, writes=[hmb])
            mqT = C.sb([128, T], BF16, "mqT")
            mkT = C.sb([128, T], BF16, "mkT")
            mqTb, mkTb = Buf(), Buf()
            vaug = C.sb([128, NT, 2, 65], BF16, "mvaug")
            smo = C.sb([128, NT, 128], F32, "smo")
            gts = C.sb([128, NT, 8], F32, "gts")
            tmb = [Buf() for _ in range(NT)]
            P.op("pool", lambda g_: g_.memset(vaug[:], 1.0), writes=tmb)
            with C.phase():
                raw = C.sb([128, T + 2], F32, "raw")
                rawb = Buf()
                y = C.sb([128, T], F32, "convy")
                yb = Buf()
                for qk in range(2):
                    P.op("pool", lambda g_: g_.memset(raw[:, 0:1], 0.0), writes=[rawb])
                    P.op("pool", lambda g_: g_.memset(raw[:, T + 1:T + 2], 0.0), writes=[rawb])
                    for tb in range(NTB):
                        j = tb % 2
                        tsl = slice(tb * TB, (tb + 1) * TB)
                        for kc in range(8):
                            P.op("pe", lambda t, kc=kc, qk=qk, j=j, tsl=tsl: t.matmul(
                                out=pz[j][:, 0:TB], lhsT=wmf[:, kc, qk * 128:(qk + 1) * 128], rhs=hT[:, kc, tsl],
                                start=(kc == 0), stop=(kc == 7)), reads=[wmfb] + hT_blk(tb), writes=[pzb[j]])
                        P.op("act", lambda a, j=j, tb=tb: a.activation(out=raw[:, 1 + tb * TB:1 + (tb + 1) * TB],
                                                                       in_=pz[j][:, 0:TB], func=AF.Copy),
                             reads=[pzb[j]], writes=[rawb])
                    P.op("dve", lambda v, qk=qk: v.tensor_scalar(out=y[:], in0=raw[:, 0:T], scalar1=cw[:, qk, 0:1],
                                                                  scalar2=cw[:, qk, 3:4], op0=ALU.mult, op1=ALU.add),
                         reads=[rawb, cwb], writes=[yb])
                    P.op("dve", lambda v, qk=qk: v.scalar_tensor_tensor(out=y[:], in0=raw[:, 1:T + 1], scalar=cw[:, qk, 1:2],
                                                                         in1=y[:], op0=ALU.mult, op1=ALU.add),
                         reads=[rawb, cwb, yb], writes=[yb])
                    P.op("dve", lambda v, qk=qk: v.scalar_tensor_tensor(out=y[:], in0=raw[:, 2:T + 2], scalar=cw[:, qk, 2:3],
                                                                         in1=y[:], op0=ALU.mult, op1=ALU.add),
                         reads=[rawb, cwb, yb], writes=[yb])
                    if qk == 0:
                        P.op("act", lambda a: a.activation(out=mqT[:], in_=y[:], func=AF.Silu), reads=[yb], writes=[mqTb])
                    else:
                        P.op("act", lambda a: a.activation(out=y[:], in_=y[:], func=AF.Silu), reads=[yb], writes=[yb])
                        P.op("pool", lambda g_: g_.tensor_scalar(out=mkT[:], in0=y[:], scalar1=0.125, scalar2=None,
                                                                 op0=ALU.mult), reads=[yb], writes=[mkTb])
            for i in range(NT):
                j = i % 2
                for kc in range(8):
                    P.op("pe", lambda t, i=i, kc=kc, j=j: t.matmul(out=pz[j][:, 0:264], lhsT=hT[:, kc, i * 128:(i + 1) * 128],
                                                                  rhs=wmt[:, kc, :], start=(kc == 0), stop=(kc == 7)),
                         reads=[hTb[i], wmtb], writes=[pzb[j]])
                P.op("act", lambda a, i=i, j=j: a.activation(out=vaug[:, i, :, 0:64],
                                                              in_=pz[j][:, 0:128].rearrange("p (h e) -> p h e", h=2),
                                                              func=AF.Copy), reads=[pzb[j]], writes=[tmb[i]])
                P.op("act", lambda a, i=i, j=j: a.activation(out=smo[:, i, :], in_=pz[j][:, 128:256], func=AF.Sigmoid),
                     reads=[pzb[j]], writes=[tmb[i]])
                P.op("dve", lambda v, i=i: v.tensor_tensor(out=smo[:, i, :], in0=smo[:, i, :], in1=mlg[:], op=ALU.mult),
                     reads=[tmb[i], mlgb], writes=[tmb[i]])
                P.op("dve", lambda v, i=i, j=j: v.tensor_tensor(out=gts[:, i, :], in0=pz[j][:, 256:264], in1=gb8[:], op=ALU.add),
                     reads=[pzb[j], gb8b], writes=[tmb[i]])

            def S(shape, nm, dt=F32):
                return C.sb(shape, dt, nm), Buf(nm)
            lf, lfb = S([128, NT, 4], "lf")
            cums, cumsb = S([128, NT, 4], "cums")
            aa, aab = S([128, NT, 4], "aa")
            xcat, xcatb = S([128, NT, 8], "xcat")
            xm, xmb = S([128, NT, 2, 8], "xm")
            LSE, LSEb = S([128, NCH, 4], "LSE")
            bl, blb = S([128, NCH, 4], "bl")
            ein, einb = S([128, NCH + 1, 4], "ein")
            einB = [Buf(), Buf()]
            tmx = [C.sb([128, 2], F32, f"tmx{d}") for d in range(2)]
            tmxb = [Buf(), Buf()]
            Mp, Mpb = S([128, NCH, 4], "Mp")
            wpv, wpvb = S([128, NCH, 4], "wpv")
            Mtm, Mtmb = S([128, NT, 4], "Mtm")
            wptm, wptmb = S([128, NT, 4], "wptm")
            es, esb = S([128, NT, 4], "es")
            fden, fdenb = S([128, NT, 4], "fden")
            P.op("act", lambda a: a.activation(out=lf[:], in_=gts[:, :, 4:8], func=AF.Exp, scale=-1.0), reads=tmb, writes=[lfb])
            P.op("act", lambda a: a.activation(out=lf[:], in_=lf[:], func=AF.Ln, bias=1.0), reads=[lfb], writes=[lfb])
            for d in range(2):
                P.op("pe", lambda t, d=d: t.matmul(out=prp[0][:, d * NT * 2:(d + 1) * NT * 2], lhsT=cf[:, d, :],
                                                   rhs=lf[:, :, d * 2:(d + 1) * 2], start=True, stop=True),
                     reads=[cfb, lfb], writes=[prpb[0]])
            for d in range(2):
                P.op("dve", lambda v, d=d: v.tensor_copy(out=cums[:, :, d * 2:(d + 1) * 2],
                                                         in_=prp[0][:, d * NT * 2:(d + 1) * NT * 2].rearrange("p (n s) -> p n s", s=2)),
                     reads=[prpb[0]], writes=[cumsb])
            P.op("dve", lambda v: v.tensor_tensor(out=aa[:], in0=gts[:, :, 0:4], in1=cums[:], op=ALU.add), reads=tmb + [cumsb],
                 writes=[aab])
            P.op("act", lambda a: a.activation(out=xcat[:, :, 0:4], in_=aa[:], func=AF.Exp), reads=[aab], writes=[xcatb])
            P.op("dve", lambda g_: g_.tensor_copy(out=xcat[:, :, 4:8], in_=lf[:]), reads=[lfb, xcatb], writes=[xcatb])
            for jh in range(2):
                P.op("dve", lambda v, jh=jh: v.tensor_scalar(out=xm[:, :, jh, :], in0=xcat[:], scalar1=hm[:, jh:jh + 1],
                                                              scalar2=None, op0=ALU.mult), reads=[xcatb, hmb], writes=[xmb])
            P.op("pe", lambda t: t.matmul(out=prp[0][:, 0:NT * 16], lhsT=ones[:],
                                          rhs=xm[:].rearrange("p n j s -> p (n j s)"), start=True, stop=True),
                 reads=[onesb, xmb, cumsb], writes=[prpb[0]])
            P.op("act", lambda a: a.activation(out=LSE[:],
                                               in_=prp[0][:, 0:NCH * 8].rearrange("p (n s) -> p n s", s=8)[:, :, 0:4],
                                               func=AF.Ln), reads=[prpb[0]], writes=[LSEb])
            P.op("act", lambda a: a.activation(out=bl[:],
                                               in_=prp[0][:, 0:NCH * 8].rearrange("p (n s) -> p n s", s=8)[:, :, 4:8],
                                               func=AF.Copy), reads=[prpb[0]], writes=[blb])
            P.op("pool", lambda g_: g_.memset(ein[:], -1e30), writes=[einb] + einB)
            for n in range(NCH):
                P.op("dve", lambda v, n=n: v.tensor_tensor(out=tmx[0][:], in0=LSE[:, n, 0:2], in1=ein[:, n, 0:2], op=ALU.max),
                     reads=[LSEb, einB[0]], writes=[tmxb[0]])
                P.op("dve", lambda v, n=n: v.tensor_tensor(out=ein[:, n + 1, 0:2], in0=tmx[0][:], in1=bl[:, n, 0:2],
                                                            op=ALU.subtract), reads=[tmxb[0], blb], writes=[einB[0]])
                m = NCH - 1 - n
                P.op("dve", lambda g_, m=m: g_.tensor_tensor(out=tmx[1][:], in0=LSE[:, m, 2:4], in1=ein[:, m + 1, 2:4],
                                                              op=ALU.max), reads=[LSEb, einB[1]], writes=[tmxb[1]])
                P.op("dve", lambda g_, m=m: g_.tensor_tensor(out=ein[:, m, 2:4], in0=tmx[1][:], in1=bl[:, m, 2:4],
                                                              op=ALU.subtract), reads=[tmxb[1], blb], writes=[einB[1]])
            P.op("dve", lambda v: v.tensor_tensor(out=Mp[:, :, 0:2], in0=LSE[:, :, 0:2], in1=ein[:, 0:NCH, 0:2], op=ALU.max),
                 reads=[LSEb] + einB, writes=[Mpb])
            P.op("dve", lambda v: v.tensor_tensor(out=Mp[:, :, 2:4], in0=LSE[:, :, 2:4], in1=ein[:, 1:NCH + 1, 2:4], op=ALU.max),
                 reads=[LSEb] + einB, writes=[Mpb])
            P.op("dve", lambda v: v.tensor_tensor(out=wpv[:, :, 0:2], in0=ein[:, 0:NCH, 0:2], in1=Mp[:, :, 0:2], op=ALU.subtract),
                 reads=[Mpb] + einB, writes=[wpvb])
            P.op("dve", lambda v: v.tensor_tensor(out=wpv[:, :, 2:4], in0=ein[:, 1:NCH + 1, 2:4], in1=Mp[:, :, 2:4], op=ALU.subtract),
                 reads=[Mpb] + einB, writes=[wpvb])
            P.op("act", lambda a: a.activation(out=wpv[:], in_=wpv[:], func=AF.Exp), reads=[wpvb], writes=[wpvb])
            for hf in range(2):
                rows = slice(hf * 64, (hf + 1) * 64)
                P.op("dve", lambda v, hf=hf, rows=rows: v.tensor_copy(
                    out=Mtm[rows, :, :], in_=Mp[rows, :, :].rearrange("p (i c) s -> p i c s", c=2)[:, :, hf, :]),
                    reads=[Mpb], writes=[Mtmb])
                P.op("dve", lambda v, hf=hf, rows=rows: v.tensor_copy(
                    out=wptm[rows, :, :], in_=wpv[rows, :, :].rearrange("p (i c) s -> p i c s", c=2)[:, :, hf, :]),
                    reads=[wpvb], writes=[wptmb])
            P.op("dve", lambda v: v.tensor_tensor(out=es[:], in0=aa[:], in1=Mtm[:], op=ALU.subtract), reads=[aab, Mtmb], writes=[esb])
            P.op("act", lambda a: a.activation(out=es[:], in_=es[:], func=AF.Exp), reads=[esb], writes=[esb])
            P.op("dve", lambda v: v.tensor_tensor(out=fden[:], in0=cums[:], in1=Mtm[:], op=ALU.subtract), reads=[cumsb, Mtmb],
                 writes=[fdenb])
            P.op("act", lambda a: a.activation(out=fden[:], in_=fden[:], func=AF.Exp), reads=[fdenb], writes=[fdenb])

            ktm = C.sb([128, NT, 128], BF16, "mktm")
            ktmb = [Buf() for _ in range(NT)]
            ptb1 = Buf()
            GRP = min(4, NT)
            for g0 in range(0, NT, GRP):
                for k in range(GRP):
                    P.op("pe", lambda t, g0=g0, k=k: t.transpose(out=pt[0][:, k * 128:(k + 1) * 128],
                                                                 in_=mkT[:, (g0 + k) * 128:(g0 + k + 1) * 128],
                                                                 identity=identb[:]), reads=[mkTb, ibb], writes=[ptb1])
                P.op("dve", lambda a, g0=g0: a.tensor_copy(out=ktm[:, g0:g0 + GRP, :],
                                                            in_=pt[0][:, 0:GRP * 128].rearrange("p (k t) -> p k t", k=GRP)),
                     reads=[ptb1], writes=ktmb[g0:g0 + GRP])
            qz = C.sb([128, NT, 2, 128], BF16, "qz")
            qzb = Buf()
            P.op("pool", lambda g_: g_.memset(qz[:], 0.0), writes=[qzb])
            for c in range(2):
                P.op("pool", lambda g_, c=c: g_.tensor_copy(out=qz[:, :, c, c * 64:(c + 1) * 64],
                                                            in_=mqT[:].rearrange("p (n c e) -> p n c e", c=2, e=64)[:, :, c, :]),
                     reads=[mqTb, qzb], writes=[qzb])
            hacc = C.sb([128, NT, 128], F32, "hacc")
            haccb = [Buf() for _ in range(NT)]
            mixM = C.sb([128, T], BF16, "mixM")
            mixMb = []

            def M2(nm, shape, dt):
                return [C.sb(shape, dt, f"{nm}{j}") for j in range(2)], [Buf() for _ in range(2)]
            st_, stb = M2("mst", [128, 2, 128], BF16)
            kw_, kwb = M2("mkw", [128, 128], BF16)
            isb_, isbb = M2("misb", [128, 130], F32)
            res_, resb = M2("mres", [128, 130], F32)
            den_, denb = M2("mden", [128, 2], F32)
            htmp_, htmpb = M2("mhtmp", [128, 128], F32)
            C32d = [C.sb([128, 130], F32, f"C32_{d}") for d in range(2)]
            C32db = [Buf(), Buf()]
            Cbfd = [[C.sb([128, 130], BF16, f"Cbf{d}_{j}") for j in range(2)] for d in range(2)]
            Cbfdb = [[Buf(), Buf()], [Buf(), Buf()]]
            kwz = [C.sb([128, 2, 128], BF16, f"kwz{j}") for j in range(2)]
            kwzb = [Buf(), Buf()]
            for d in range(2):
                P.op("pool", lambda g_, d=d: g_.memset(C32d[d][:], 0.0), writes=[C32db[d]])
                P.op("pool", lambda g_, d=d: g_.memset(kwz[d][:], 0.0), writes=[kwzb[d]])
                for j in range(2):
                    P.op("pool", lambda g_, d=d, j=j: g_.memset(Cbfd[d][j][:], 0.0), writes=[Cbfdb[d][j]])
            P.barrier()
            P.flush()
            pt_ctx0.__exit__(None, None, None)
            C._n += 1
            pi_ctx = [nc.psum_tensor(f"pi{C._n}_{k}", [128, 512], F32) for k in range(2)]
            pi2 = [c_.__enter__() for c_ in pi_ctx]
            pi2b = [Buf(), Buf()]
            kvb = [(pk[0], pkb[0]), (prp[0], prpb[0])]
            scurd = [0, 0]
            seen = set()

            def m_body(d, i, j):
                chunks = (0, 1) if d == 0 else (1, 0)
                tsl = slice(i * 128, (i + 1) * 128)
                sb_ = [(pst[j], pstb[j]), (pz[j], pzb[j])]
                kvt, kvtb = kvb[j]
                pit, pitb = pi2[j], pi2b[j]
                for h in range(2):
                    rows = slice(h * 64, (h + 1) * 64)
                    P.op("pe", lambda t, h=h, rows=rows: t.matmul(out=sb_[h][0][:, 0:128], lhsT=mkT[rows, tsl],
                                                                  rhs=mqT[rows, tsl], start=True, stop=True),
                         reads=[mkTb, mqTb], writes=[sb_[h][1]])
                for h in range(2):
                    sc = d * 2 + h
                    for c in range(2):
                        crow = slice(c * 64, (c + 1) * 64)
                        P.op("act", lambda a, h=h, sc=sc, c=c, crow=crow: a.activation(
                            out=kwz[j][crow, c, h * 64:(h + 1) * 64], in_=ktm[crow, i, h * 64:(h + 1) * 64], func=AF.Copy,
                            scale=es[crow, i, sc:sc + 1]), reads=[ktmb[i], esb], writes=[kwzb[j]])
                yield
                for h in range(2):
                    sc = d * 2 + h
                    P.op("dve", lambda v, h=h, sc=sc: v.scalar_tensor_tensor(
                        out=st_[j][:, h, :], in0=sb_[h][0][:, 0:128], scalar=es[:, i, sc:sc + 1],
                        in1=cf[:, d, :], op0=ALU.mult, op1=ALU.mult), reads=[sb_[h][1], esb, cfb], writes=[stb[j]])
                for c in range(2):
                    P.op("pe", lambda t, c=c: t.matmul(out=kvt[:, c * 130:(c + 1) * 130], lhsT=kwz[j][:, c, :],
                                                       rhs=vaug[:, i, :, :].rearrange("p h e -> p (h e)"),
                                                       start=True, stop=True),
                         reads=[kwzb[j], tmb[i]], writes=[kvtb])
                yield
                for h in range(2):
                    P.op("pe", lambda t, h=h: t.matmul(out=pit[:, h * 65:(h + 1) * 65], lhsT=st_[j][:, h, :],
                                                       rhs=vaug[:, i, h, :], start=True, stop=True),
                         reads=[stb[j], tmb[i]], writes=[pitb])
                yield
                C32, C32b, Cbf, Cbfb, scur = C32d[d], C32db[d], Cbfd[d], Cbfdb[d], scurd[d]
                first = True
                for ci, c in enumerate(chunks):
                    n = 2 * i + c
                    P.op("pe", lambda t, c=c, scur=scur, first=first, ci=ci: t.matmul(
                        out=pit[:, 130:260], lhsT=qz[:, i, c, :], rhs=Cbf[scur][:], start=first, stop=(ci == 1)),
                        reads=[qzb, Cbfb[scur]], writes=[pitb])
                    first = False
                    yield
                    for h in range(2):
                        rows = slice(h * 64, (h + 1) * 64)
                        sc = d * 2 + h
                        P.op("dve", lambda v, c=c, h=h, rows=rows, sc=sc, n=n: v.scalar_tensor_tensor(
                            out=C32[rows, h * 65:(h + 1) * 65], in0=C32[rows, h * 65:(h + 1) * 65],
                            scalar=wpv[rows, n, sc:sc + 1], in1=kvt[rows, c * 130 + h * 65:c * 130 + (h + 1) * 65],
                            op0=ALU.mult, op1=ALU.add), reads=[C32b, wpvb, kvtb], writes=[C32b])
                    yield
                    scur = 1 - scur
                    P.op("act", lambda a, scur=scur: a.activation(out=Cbf[scur][:], in_=C32[:], func=AF.Copy),
                         reads=[C32b], writes=[Cbfb[scur]])
                    yield
                scurd[d] = scur
                P.op("act", lambda a: a.activation(out=isb_[j][:], in_=pit[:, 0:130], func=AF.Copy),
                     reads=[pitb], writes=[isbb[j]])
                yield
                for h in range(2):
                    sc = d * 2 + h
                    P.op("dve", lambda v, h=h, sc=sc: v.scalar_tensor_tensor(
                        out=res_[j][:, h * 65:(h + 1) * 65], in0=pit[:, 130 + h * 65:130 + (h + 1) * 65],
                        scalar=wptm[:, i, sc:sc + 1], in1=isb_[j][:, h * 65:(h + 1) * 65], op0=ALU.mult, op1=ALU.add),
                        reads=[pitb, wptmb, isbb[j]], writes=[resb[j]])
                P.op("dve", lambda v: v.scalar_tensor_tensor(
                    out=den_[j][:], in0=res_[j][:].rearrange("p (h e) -> p h e", h=2)[:, :, 64], scalar=-1.0,
                    in1=res_[j][:].rearrange("p (h e) -> p h e", h=2)[:, :, 64], op0=ALU.mult, op1=ALU.max),
                    reads=[resb[j]], writes=[denb[j]])
                yield
                P.op("dve", lambda v: v.tensor_tensor(out=den_[j][:], in0=den_[j][:], in1=fden[:, i, d * 2:(d + 1) * 2],
                                                      op=ALU.max), reads=[denb[j], fdenb], writes=[denb[j]])
                P.op("dve", lambda v: v.reciprocal(out=den_[j][:], in_=den_[j][:]), reads=[denb[j]], writes=[denb[j]])
                yield
                if i not in seen:
                    seen.add(i)
                    P.op("dve", lambda v: v.tensor_tensor(
                        out=hacc[:, i, :].rearrange("p (h e) -> p h e", h=2),
                        in0=res_[j][:].rearrange("p (h e) -> p h e", h=2)[:, :, 0:64],
                        in1=bc(den_[j][:], 2, [128, 2, 64]), op=ALU.mult), reads=[resb[j], denb[j]], writes=[haccb[i]])
                else:
                    P.op("dve", lambda v: v.tensor_tensor(
                        out=htmp_[j][:].rearrange("p (h e) -> p h e", h=2),
                        in0=res_[j][:].rearrange("p (h e) -> p h e", h=2)[:, :, 0:64],
                        in1=bc(den_[j][:], 2, [128, 2, 64]), op=ALU.mult), reads=[resb[j], denb[j]], writes=[htmpb[j]])
                    P.op("pool", lambda g_: g_.tensor_tensor(out=hacc[:, i, :], in0=hacc[:, i, :], in1=htmp_[j][:],
                                                             op=ALU.add), reads=[htmpb[j], haccb[i]], writes=[haccb[i]])

            for p_ in range(NT):
                gens = [m_body(0, p_, 0), m_body(1, NT - 1 - p_, 1)]
                live = [True, True]
                while any(live):
                    for k_ in range(2):
                        if live[k_]:
                            try:
                                next(gens[k_])
                            except StopIteration:
                                live[k_] = False
            P.barrier()
            P.flush()
            for c_ in reversed(pi_ctx):
                c_.__exit__(None, None, None)
            pt, ptb = psum_banks(1, BF16, 1024)
            finalize_heads(C, hacc, haccb, smo, tmb, NT, identb, ibb, pt[0], mixM, mixMb, "m")
            P.dma("sp", mix_rows[3][0], mixM[:], reads=mixMb, writes=mix_rows[3][1], is_output=mix_is_output)
    if standalone:
        P.final_wait()
        P.flush()
    return C


PAIRS = [[0, 1], [2, 3], [4, 5], [6, 7]]


def build_fused(T):
    C = Ctx()
    nc, P = C.nc, C.P
    T2 = T // 2
    sel_d = C.din("sel", [128, 2], F32)
    sel = C.sb([128, 2], F32, "sel")
    selB = Buf()
    P.dma("sp", sel[:], sel_d, writes=[selB])
    mixb = [[nc.dram_tensor(f"mixb{l}_{c}", [256, T], BF16).ap() for c in range(2)] for l in range(2)]
    mixg = [[nc.dram_tensor(f"mixg{l}_{c}", [512, T], BF16).ap() for c in range(2)] for l in range(2)]
    hb = [nc.dram_tensor(f"hb_{c}", [512, T2], BF16).ap() for c in range(2)]
    hg = [nc.dram_tensor(f"hg_{c}", [1024, T2], BF16).ap() for c in range(2)]
    xs = nc.dram_tensor("xs", [T2, 1024], F32).ap()
    mixbB = [[Buf(), Buf()] for _ in range(2)]
    mixgB = [[Buf(), Buf()] for _ in range(2)]
    hbB, hgB, xsB = [Buf(), Buf()], [Buf(), Buf()], Buf()

    def stage(fn):
        ph = C.phase()
        ph.__enter__()
        fn()
        ph.__exit__(None, None, None)

    def mix_dst(l):
        return [(mixb[l][q // 2][(q % 2) * 128:(q % 2) * 128 + 128, :], mixbB[l][q // 2]) for q in range(4)]

    def mix_src(l):
        out = []
        for kc in range(8):
            r, q = kc // 4, kc % 4
            off = r * 256 + (q % 2) * 128
            out.append((mixg[l][q // 2][off:off + 128, :], mixgB[l][q // 2]))
        return out

    def gather_mix(l, c):
        P.collective("AllGather", mixb[l][c], mixg[l][c], PAIRS, reads=[mixbB[l][c]], writes=[mixgB[l][c]])

    h_dst = [(hb[k0 // 4][(k0 % 4) * 128:(k0 % 4) * 128 + 256, :], hbB[k0 // 4]) for k0 in range(0, 8, 2)]
    h_src = {(r, k0): (hg[k0 // 4][r * 512 + (k0 % 4) * 128:r * 512 + (k0 % 4) * 128 + 256, :], hgB[k0 // 4])
             for r in range(2) for k0 in range(0, 8, 2)}

    stage(lambda: build_k1(T, C=C, sfx="_l0", mix_dst=mix_dst(0), after_A=lambda: gather_mix(0, 0)))
    gather_mix(0, 1)
    stage(lambda: build_k2(T2, False, C=C, sfx="_l0", fz=dict(mixg=mix_src(0), sel=(sel, selB),
                                                              x_dst=(xs, xsB), h_dst=h_dst)))
    for c in range(2):
        P.collective("AllGather", hb[c], hg[c], PAIRS, reads=[hbB[c]], writes=[hgB[c]])
    stage(lambda: build_k1(T, C=C, sfx="_l1", hsrc=h_src, mix_dst=mix_dst(1), after_A=lambda: gather_mix(1, 0)))
    gather_mix(1, 1)
    stage(lambda: build_k2(T2, True, C=C, sfx="_l1", fz=dict(x_src=(xs, xsB), mixg=mix_src(1), sel=(sel, selB))))
    P.final_wait()
    P.flush()
    return C


_OFF = {}
_o = 0
for _n, _s in zip(("aq", "ak", "av", "gq", "gk", "gv", "gg", "glr", "mq", "mk", "mv", "mo", "mg"),
                  (512, 128, 128, 256, 256, 256, 256, 32, 256, 256, 256, 256, 16)):
    _OFF[_n] = (_o, _o + _s)
    _o += _s


def _consts(T):
    t = np.arange(T)
    row = (t // 64).astype(np.float32)
    col = (t % 64).astype(np.float32)
    inv = (np.float32(10000.0) ** (-np.arange(0, 32, 2, dtype=np.float32) / np.float32(32))).astype(np.float32)
    ar = row[:, None] * inv[None, :]
    ac = col[:, None] * inv[None, :]
    cr, sr, cc, sc = np.cos(ar), np.sin(ar), np.cos(ac), np.sin(ac)
    rope = np.concatenate([cr, cr, cc, cc, -sr, sr, -sc, sc], axis=1).astype(np.float32)
    s = np.arange(128)[:, None]
    u = np.arange(128)[None, :]
    same = (s // 64) == (u // 64)
    tri_f = (same & (s <= u)).astype(np.float32)
    tri_b = (same & (s >= u)).astype(np.float32)
    cstart = (np.arange(128) // 64) * 64
    mid_f = tri_f - tri_f[:, cstart + 31]
    mid_b = tri_b - tri_b[:, cstart + 32]
    last_f = (same & (s > u)).astype(np.float32)
    last_b = (same & (s < u)).astype(np.float32)
    cf = np.stack([tri_f, tri_b]).astype(np.float32)
    cs = (np.stack([tri_f, tri_b, mid_f, mid_b, last_f, last_b]) * np.float32(-1.0 / 16.0)).astype(np.float32)
    hm = np.stack([(np.arange(128) // 64) == 0, (np.arange(128) // 64) == 1], axis=1).astype(np.float32)
    return dict(rope=rope, cf=cf, cs=cs, hm=hm)


def k1_inputs(xb, W, li, hf, consts):
    w_in = W["w_in"][li]

    def cols(name, lo, hi):
        a, _ = _OFF[name]
        return w_in[:, a + lo:a + hi]
    ak = cols("ak", hf * 64, hf * 64 + 64)
    h0, h1 = 2 * hf, 2 * hf + 1
    mg = w_in[:, _OFF["mg"][0]:_OFF["mg"][1]]
    gidx = [0 + h0, 0 + h1, 8 + h0, 8 + h1, 4 + h0, 4 + h1, 12 + h0, 12 + h1]
    bi, bf = W["mlstm_b_input"][li], W["mlstm_b_forget"][li]
    gates_b = np.array([bi[0][h0], bi[0][h1], bi[1][h0], bi[1][h1], bf[0][h0], bf[0][h1], bf[1][h0], bf[1][h1]],
                       np.float32)
    sl = slice(hf * 128, hf * 128 + 128)
    wdb = np.zeros((2, 17, 128), np.float32)
    wdb[:, 0:16, :] = W["gla_w_decay"][li][:, :, sl]
    wdb[:, 16, :] = W["gla_b_decay"][li][:, sl]
    cwv, cbv = W["mlstm_conv_w"][li], W["mlstm_conv_b"][li]
    ch = np.concatenate([np.arange(hf * 128, hf * 128 + 128), 256 + np.arange(hf * 128, hf * 128 + 128)])
    cw = np.concatenate([cwv[:, ch].T, cbv[ch][:, None]], axis=1).astype(np.float32)
    qg, kg = W["attn_q_norm_g"][li], W["attn_k_norm_g"][li]
    d = dict(
        x=np.ascontiguousarray(xb), norm_mix_g=W["norm_mix_g"][li],
        w_att=np.ascontiguousarray(np.concatenate([cols("aq", hf * 256, hf * 256 + 256), ak, ak,
                                                   cols("av", hf * 64, hf * 64 + 64)], axis=1)),
        w_gla_f=np.ascontiguousarray(np.concatenate([cols("gq", sl.start, sl.stop), cols("gk", sl.start, sl.stop)], 1)),
        w_gla_t=np.ascontiguousarray(np.concatenate([cols("gk", sl.start, sl.stop), cols("gv", sl.start, sl.stop),
                                                     cols("gg", sl.start, sl.stop)], 1)),
        w_glr=np.ascontiguousarray(cols("glr", 0, 32)), wdb=wdb,
        w_ml_f=np.ascontiguousarray(np.concatenate([cols("mq", sl.start, sl.stop), cols("mk", sl.start, sl.stop)], 1)),
        w_ml_t=np.ascontiguousarray(np.concatenate([cols("mv", sl.start, sl.stop), cols("mo", sl.start, sl.stop),
                                                    mg[:, gidx]], 1)),
        cw=cw, gates_b=gates_b, g6=np.concatenate([qg, qg, qg, qg, kg, kg]).astype(np.float32),
        gla_g=np.tile(W["gla_out_norm_g"][li], 2).astype(np.float32),
        ml_g=np.tile(W["mlstm_out_norm_g"][li], 2).astype(np.float32),
    )
    d.update(consts)
    return d


_PROGS = {}


def _prog(key, fn):
    if key not in _PROGS:
        _PROGS[key] = fn()
    return _PROGS[key]


def _k2_inputs(W, li, last, fused):
    d = dict(
        norm_ffn_g=W["norm_ffn_g"][li],
        w_gr=np.ascontiguousarray(np.concatenate([W["w_group"][li], W["w_router"][li]], axis=1)),
        b_gr=np.concatenate([W["b_group"][li], W["b_router"][li]]),
        w_gate=W["w_expert_gate"][li], w_up=W["w_expert_up"][li], w_down=W["w_expert_down"][li],
        norm_ple_g=W["norm_ple_g"][li], w_ple_gate=W["w_ple_gate"][li], w_ple_proj=W["w_ple_proj"][li])
    if fused:
        perm = np.concatenate([np.concatenate([np.arange(r * 256, (r + 1) * 256), 512 + np.arange(r * 128, (r + 1) * 128),
                                               768 + np.arange(r * 128, (r + 1) * 128)]) for r in range(2)])
        d["w_out"] = np.ascontiguousarray(W["w_out"][li][perm])
    else:
        d["w_out"] = W["w_out"][li]
    if last:
        d["final_norm_g"] = W["final_norm_g"]
    return d


def kernel_unfused(**inputs):
    W = {k: np.asarray(v) for k, v in inputs.items()}
    x = np.ascontiguousarray(W["x"], dtype=np.float32)
    B, T, D = x.shape
    TH = T // 2
    consts = _consts(T)
    cores = list(range(8))
    for li in range(2):
        k1 = _prog(("k1", T), lambda: build_k1(T))
        in_maps = [k1_inputs(x[c // 2], W, li, c % 2, consts) for c in cores]
        res = run_bass_kernel_spmd(k1.nc, in_maps, core_ids=cores).results
        mixT = np.zeros((B, 1024, T), dtype=ml_dtypes.bfloat16)
        for c in cores:
            b, hf = c // 2, c % 2
            r = np.asarray(res[c]["mixT"])
            mixT[b, hf * 256:(hf + 1) * 256] = r[0:256]
            mixT[b, 512 + hf * 128:512 + (hf + 1) * 128] = r[256:384]
            mixT[b, 768 + hf * 128:768 + (hf + 1) * 128] = r[384:512]
        last = (li == 1)
        k2 = _prog(("k2", TH, last), lambda: build_k2(TH, last))
        in_maps = []
        for c in cores:
            b, half = c // 2, c % 2
            th = slice(half * TH, (half + 1) * TH)
            d = _k2_inputs(W, li, last, False)
            d.update(xh=np.ascontiguousarray(x[b, th]), mixT=np.ascontiguousarray(mixT[b][:, th]),
                     p=np.ascontiguousarray(W["p"][li, b, th]))
            in_maps.append(d)
        res = run_bass_kernel_spmd(k2.nc, in_maps, core_ids=cores).results
        xn = np.empty_like(x)
        for c in cores:
            b, half = c // 2, c % 2
            xn[b, half * TH:(half + 1) * TH] = np.asarray(res[c]["out"])
        x = xn
    return x.astype(np.float32)


def kernel(**inputs):
    W = {k: np.asarray(v) for k, v in inputs.items()}
    x = np.ascontiguousarray(W["x"], dtype=np.float32)
    B, T, D = x.shape
    TH = T // 2
    consts = _consts(T)
    cores = list(range(8))
    prog = _prog(("fused", T), lambda: build_fused(T))
    k2w = [_k2_inputs(W, li, li == 1, True) for li in range(2)]
    in_maps = []
    for c in cores:
        b, half = c // 2, c % 2
        th = slice(half * TH, (half + 1) * TH)
        d = {}
        for li in range(2):
            k1 = k1_inputs(x[b], W, li, half, consts)
            if li == 1:
                k1.pop("x")
                k1.pop("norm_mix_g")
            for k, v in k1.items():
                if k in consts:
                    d[k] = v
                else:
                    d[f"{k}_l{li}"] = v
            for k, v in k2w[li].items():
                d[f"{k}_l{li}"] = v
            d[f"p_l{li}"] = np.ascontiguousarray(W["p"][li, b, th])
        d["xh_l0"] = np.ascontiguousarray(x[b, th])
        d["norm_mix_g_next_l0"] = W["norm_mix_g"][1]
        d["sel"] = np.tile(np.array([[1.0 - half, float(half)]], np.float32), (128, 1))
        in_maps.append(d)
    res = run_bass_kernel_spmd(prog.nc, in_maps, core_ids=cores).results
    out = np.empty_like(x)
    for c in cores:
        b, half = c // 2, c % 2
        out[b, half * TH:(half + 1) * TH] = np.asarray(res[c]["out"])
    return out.astype(np.float32)
```
